# Optimizing a Trainium2 kernel written in Bass

```python
import math
import jax, jax.numpy as jnp
from jax import lax
import numpy as np

D_MODEL = 1024
BATCH = 4
SEQ = 8192
DEPTH = 4

SSD_HEAD_DIM = 64
D_SSD = D_MODEL
SSD_HEADS = D_SSD // SSD_HEAD_DIM
SSD_GROUPS = 2
D_STATE = 128
CONV_W = 4
SSD_CHUNK = 128
CONV_DIM = D_SSD + 2 * SSD_GROUPS * D_STATE
DSA_HEADS = 8
DSA_HEAD_DIM = 64
D_DSA = DSA_HEADS * DSA_HEAD_DIM
D_LAT = 128
IDX_HEADS = 4
IDX_DIM = 64
TOPK_MAX = 256
Q_BLOCK = 128
MEM_LEN = 256
MEM_HEADS = 4
MEM_HEAD_DIM = 128
D_MEMG = MEM_HEADS * MEM_HEAD_DIM
D_MIX = D_SSD + D_DSA + D_MEMG
SPLIT_SIZES = (D_SSD,
               CONV_DIM,
               SSD_HEADS,
               DSA_HEADS * D_LAT,
               D_LAT,
               IDX_HEADS * IDX_DIM,
               IDX_DIM,
               IDX_HEADS,
               D_MEMG)
N_IN = sum(SPLIT_SIZES)
N_EXPERTS = 32
TOP_K = 4
D_FF = D_MODEL
SWIGLU_LIMIT = 7.0
SWIGLU_ALPHA = 1.702
EXPERT_BLOCK = 256
ALPHA = (2 * DEPTH) ** 0.25
BETA = (8 * DEPTH) ** -0.25
LN_EPS = 1e-5
RMS_EPS = 1e-6

kernel_name = "hymba_ssd_dsa_mem_moe_deepnorm"

F32 = jnp.float32


def layer_norm(x, g, b):
    xf = x.astype(F32)
    mu = jnp.mean(xf, -1, keepdims=True)
    xc = xf - mu
    var = jnp.mean(xc * xc, -1, keepdims=True)
    return (xc * lax.rsqrt(var + LN_EPS) * g + b).astype(x.dtype)


def rms_norm(x, g):
    xf = x.astype(F32)
    return (xf * lax.rsqrt(jnp.mean(xf * xf, -1, keepdims=True) + RMS_EPS) * g).astype(x.dtype)


def gated_rms_norm(y, z, g):
    b, s, dd = y.shape
    hf = (y * jax.nn.silu(z)).astype(F32).reshape(b, s, SSD_GROUPS, dd // SSD_GROUPS)
    hf = hf * lax.rsqrt(jnp.mean(hf * hf, -1, keepdims=True) + RMS_EPS)
    return (hf.reshape(b, s, dd) * g).astype(y.dtype)


def split_cols(p):
    outs, o = [], 0
    for n in SPLIT_SIZES:
        outs.append(p[..., o:o + n])
        o += n
    return outs


def alibi_slopes(n):
    return jnp.exp2(-8.0 * jnp.arange(1, n + 1, dtype=F32) / n)


def causal_depthwise_conv(u, w, bias):
    c = u.shape[-1]
    y = lax.conv_general_dilated(u, w[:, None, :].astype(u.dtype), window_strides=(1,),
                                 padding=[(CONV_W - 1, 0)],
                                 dimension_numbers=('NWC', 'WIO', 'NWC'),
                                 feature_group_count=c)
    return y + bias


def ssd_chunked_scan(xdt, adt, bmat, cmat):
    b, s, h, p = xdt.shape
    g, n = bmat.shape[-2:]
    k = h // g
    q = SSD_CHUNK
    nc = s // q
    to_chunks = lambda t: jnp.moveaxis(t.reshape((b, nc, q) + t.shape[2:]), 1, 0)
    xc = to_chunks(xdt.reshape(b, s, g, k, p))
    ac = to_chunks(adt.reshape(b, s, g, k))
    bc = to_chunks(bmat)
    cc = to_chunks(cmat)
    causal = jnp.tril(jnp.ones((q, q), bool))[None, :, :, None, None]

    def step(state, inp):
        x_, a_, b_, c_ = inp
        acum = jnp.cumsum(a_, axis=1)
        seg = acum[:, :, None] - acum[:, None]
        lmat = jnp.exp(jnp.where(causal, seg, -jnp.inf))
        cb = jnp.einsum('blgn,bsgn->blsg', c_, b_)
        y_diag = jnp.einsum('blsg,blsgk,bsgkp->blgkp', cb, lmat, x_)
        y_off = jnp.einsum('blgn,bgkpn->blgkp', c_, state) * jnp.exp(acum)[..., None]
        decay_end = jnp.exp(acum[:, -1:] - acum)
        new_state = state * jnp.exp(acum[:, -1])[..., None, None] + \
            jnp.einsum('bsgn,bsgk,bsgkp->bgkpn', b_, decay_end, x_)
        return new_state.astype(state.dtype), (y_diag + y_off).astype(x_.dtype)

    state0 = jnp.zeros((b, g, k, p, n), xdt.dtype)
    _, ys = lax.scan(step, state0, (xc, ac, bc, cc))
    return jnp.moveaxis(ys, 0, 1).reshape(b, s, h, p)


def ssd_group(z, xbc, dt_raw, conv_w, conv_b, dt_bias, a_log, d_skip, norm_g):
    b, s, _ = z.shape
    xbc = jax.nn.silu(causal_depthwise_conv(xbc, conv_w, conv_b))
    xs = xbc[..., :D_SSD].reshape(b, s, SSD_HEADS, SSD_HEAD_DIM)
    bm = xbc[..., D_SSD:D_SSD + SSD_GROUPS * D_STATE].reshape(b, s, SSD_GROUPS, D_STATE)
    cm = xbc[..., D_SSD + SSD_GROUPS * D_STATE:].reshape(b, s, SSD_GROUPS, D_STATE)
    dt = jax.nn.softplus(dt_raw + dt_bias)
    a = -jnp.exp(a_log)
    y = ssd_chunked_scan(xs * dt[..., None], dt * a, bm, cm)
    y = (y + d_skip[:, None] * xs).reshape(b, s, D_SSD)
    return gated_rms_norm(y, z, norm_g)


def dsa_group(q_lat, c_kv, q_idx, k_idx, w_idx, kv_norm_g, w_uv):
    b, s, _ = q_lat.shape
    q_lat = q_lat.reshape(b, s, DSA_HEADS, D_LAT)
    q_idx = q_idx.reshape(b, s, IDX_HEADS, IDX_DIM)
    c = rms_norm(c_kv, kv_norm_g)
    n_sel = min(TOPK_MAX, s // 4)
    nb = s // Q_BLOCK
    slopes = alibi_slopes(DSA_HEADS)
    key_pos = jnp.arange(s)
    scale = D_LAT ** -0.5

    def block(args):
        qb, qib, wb, t0 = args
        tq = t0 + jnp.arange(Q_BLOCK)
        idx_logits = jnp.einsum('bthd,bsd->bths', qib, k_idx)
        score = jnp.einsum('bth,bths->bts', wb, jax.nn.relu(idx_logits)).astype(F32)
        score = jnp.where(key_pos[None, None, :] <= tq[None, :, None], score, -jnp.inf)
        _, sel = lax.top_k(score, n_sel)
        c_sel = jax.vmap(lambda cc, ii: cc[ii])(c, sel)
        logits = jnp.einsum('bthc,btkc->bthk', qb, c_sel).astype(F32) * scale
        dist = (tq[None, :, None] - sel).astype(F32)
        logits = logits - slopes[None, None, :, None] * dist[:, :, None, :]
        logits = jnp.where((dist >= 0)[:, :, None, :], logits, -jnp.inf)
        p = jax.nn.softmax(logits, -1).astype(c_sel.dtype)
        return jnp.einsum('bthk,btkc->bthc', p, c_sel)

    blocks = lambda t: jnp.moveaxis(t.reshape((b, nb, Q_BLOCK) + t.shape[2:]), 1, 0)
    ctx = lax.map(block, (blocks(q_lat), blocks(q_idx), blocks(w_idx), jnp.arange(nb) * Q_BLOCK))
    ctx = jnp.moveaxis(ctx, 0, 1).reshape(b, s, DSA_HEADS, D_LAT)
    return jnp.einsum('bshc,hcd->bshd', ctx, w_uv).reshape(b, s, D_DSA)


def memory_group(q_mem, mem, w_mem_k, w_mem_v):
    b, s, _ = q_mem.shape
    m = mem.shape[1]
    q = q_mem.reshape(b, s, MEM_HEADS, MEM_HEAD_DIM)
    k = (mem @ w_mem_k).reshape(b, m, MEM_HEADS, MEM_HEAD_DIM)
    v = (mem @ w_mem_v).reshape(b, m, MEM_HEADS, MEM_HEAD_DIM)
    logits = jnp.einsum('bthd,bmhd->bhtm', q, k).astype(F32) * (MEM_HEAD_DIM ** -0.5)
    p = jax.nn.softmax(logits, -1).astype(v.dtype)
    return jnp.einsum('bhtm,bmhd->bthd', p, v).reshape(b, s, D_MEMG)


def hybrid_mixer(h, mem, w_in, conv_w, conv_b, dt_bias, a_log, d_skip, ssd_norm_g,
                 kv_norm_g, w_uv, w_mem_k, w_mem_v, w_out):
    proj = h @ w_in
    z, xbc, dt_raw, q_lat, c_kv, q_idx, k_idx, w_idx, q_mem = split_cols(proj)
    y_ssd = ssd_group(z, xbc, dt_raw, conv_w, conv_b, dt_bias, a_log, d_skip, ssd_norm_g)
    y_dsa = dsa_group(q_lat, c_kv, q_idx, k_idx, w_idx, kv_norm_g, w_uv)
    y_mem = memory_group(q_mem, mem, w_mem_k, w_mem_v)
    return jnp.concatenate([y_ssd, y_dsa, y_mem], -1) @ w_out


def clamped_swiglu(gu):
    glu, lin = jnp.split(gu, 2, axis=-1)
    glu = jnp.minimum(glu, SWIGLU_LIMIT)
    lin = jnp.clip(lin, -SWIGLU_LIMIT, SWIGLU_LIMIT)
    return glu * jax.nn.sigmoid(SWIGLU_ALPHA * glu) * (lin + 1.0)


def moe(h2, router_w, router_b, w_gu, b_gu, w_down, b_down):
    t, d = h2.shape
    logits = (h2 @ router_w + router_b).astype(F32)
    top_val, top_idx = lax.top_k(logits, TOP_K)
    gates = jax.nn.softmax(top_val, -1).astype(h2.dtype)
    flat_e = top_idx.reshape(-1)
    tk = flat_e.shape[0]
    order = jnp.argsort(flat_e)
    sorted_e = flat_e[order]
    sorted_tok = (order // TOP_K).astype(jnp.int32)
    counts = jnp.bincount(flat_e, length=N_EXPERTS)
    padded = (counts + EXPERT_BLOCK - 1) // EXPERT_BLOCK * EXPERT_BLOCK
    start_sorted = jnp.cumsum(counts) - counts
    pad_end = jnp.cumsum(padded)
    start_pad = pad_end - padded
    dest = (start_pad[sorted_e] + jnp.arange(tk) - start_sorted[sorted_e]).astype(jnp.int32)
    n_blk = -(-tk // EXPERT_BLOCK) + N_EXPERTS
    row_tok = jnp.zeros((n_blk * EXPERT_BLOCK,), jnp.int32).at[dest].set(sorted_tok)
    blk_e = jnp.minimum(jnp.searchsorted(pad_end, jnp.arange(n_blk) * EXPERT_BLOCK, side='right'),
                        N_EXPERTS - 1)

    def expert_block(args):
        toks, e = args
        xb = h2[toks]
        gu = xb @ w_gu[e] + b_gu[e]
        return clamped_swiglu(gu) @ w_down[e] + b_down[e]

    ys = lax.map(expert_block, (row_tok.reshape(n_blk, EXPERT_BLOCK), blk_e)).reshape(-1, d)
    dest_orig = jnp.zeros((tk,), jnp.int32).at[order].set(dest)
    y_sel = ys[dest_orig].reshape(t, TOP_K, d)
    return jnp.einsum('tk,tkd->td', gates, y_sel)


def setup_inputs(seed: int = 0) -> dict:
    key = jax.random.key(seed)
    ks = jax.random.split(key, 32)
    L = DEPTH
    nrm = lambda k, shape, sc: jax.random.normal(k, shape, F32) * sc
    dt = jnp.exp(jax.random.uniform(ks[5], (L, SSD_HEADS), F32, math.log(1e-3), math.log(1e-1)))
    return {
        "x": nrm(ks[0], (BATCH, SEQ, D_MODEL), 1.0),
        "mem": nrm(ks[1], (BATCH, MEM_LEN, D_MODEL), 1.0),
        "ln_in_g": 1.0 + nrm(ks[2], (D_MODEL,), 0.02),
        "ln_in_b": nrm(ks[3], (D_MODEL,), 0.02),
        "w_in": nrm(ks[4], (L, D_MODEL, N_IN), D_MODEL ** -0.5),
        "conv_w": nrm(ks[6], (L, CONV_W, CONV_DIM), CONV_W ** -0.5),
        "conv_b": nrm(ks[7], (L, CONV_DIM), 0.02),
        "dt_bias": dt + jnp.log(-jnp.expm1(-dt)),
        "a_log": jnp.log(jax.random.uniform(ks[8], (L, SSD_HEADS), F32, 1.0, 16.0)),
        "d_skip": 1.0 + nrm(ks[9], (L, SSD_HEADS), 0.02),
        "ssd_norm_g": 1.0 + nrm(ks[10], (L, D_SSD), 0.02),
        "kv_norm_g": 1.0 + nrm(ks[11], (L, D_LAT), 0.02),
        "w_uv": nrm(ks[12], (L, DSA_HEADS, D_LAT, DSA_HEAD_DIM), BETA * D_LAT ** -0.5),
        "w_mem_k": nrm(ks[13], (L, D_MODEL, D_MEMG), D_MODEL ** -0.5),
        "w_mem_v": nrm(ks[14], (L, D_MODEL, D_MEMG), BETA * D_MODEL ** -0.5),
        "w_out": nrm(ks[15], (L, D_MIX, D_MODEL), BETA * D_MIX ** -0.5),
        "ln1_g": 1.0 + nrm(ks[16], (L, D_MODEL), 0.02),
        "ln1_b": nrm(ks[17], (L, D_MODEL), 0.02),
        "router_w": nrm(ks[18], (L, D_MODEL, N_EXPERTS), D_MODEL ** -0.5),
        "router_b": nrm(ks[19], (L, N_EXPERTS), 0.01),
        "w_gu": nrm(ks[20], (L, N_EXPERTS, D_MODEL, 2 * D_FF), BETA * D_MODEL ** -0.5),
        "b_gu": nrm(ks[21], (L, N_EXPERTS, 2 * D_FF), 0.02),
        "w_down": nrm(ks[22], (L, N_EXPERTS, D_FF, D_MODEL), BETA * D_FF ** -0.5),
        "b_down": nrm(ks[23], (L, N_EXPERTS, D_MODEL), 0.02),
        "ln2_g": 1.0 + nrm(ks[24], (L, D_MODEL), 0.02),
        "ln2_b": nrm(ks[25], (L, D_MODEL), 0.02),
    }


def reference(x, mem, ln_in_g, ln_in_b, w_in, conv_w, conv_b, dt_bias, a_log, d_skip,
              ssd_norm_g, kv_norm_g, w_uv, w_mem_k, w_mem_v, w_out, ln1_g, ln1_b,
              router_w, router_b, w_gu, b_gu, w_down, b_down, ln2_g, ln2_b):
    b, s, d = x.shape
    h = layer_norm(x, ln_in_g, ln_in_b)
    for l in range(DEPTH):
        mix = hybrid_mixer(h, mem, w_in[l], conv_w[l], conv_b[l], dt_bias[l], a_log[l],
                           d_skip[l], ssd_norm_g[l], kv_norm_g[l], w_uv[l], w_mem_k[l],
                           w_mem_v[l], w_out[l])
        h = layer_norm(ALPHA * h + mix, ln1_g[l], ln1_b[l])
        ff = moe(h.reshape(b * s, d), router_w[l], router_b[l], w_gu[l], b_gu[l],
                 w_down[l], b_down[l]).reshape(b, s, d)
        h = layer_norm(ALPHA * h + ff, ln2_g[l], ln2_b[l])
    return h
```

```python
import math
import numpy as np
from contextlib import ExitStack
import concourse.bass as bass
import concourse.mybir as mybir
from concourse.bass_utils import run_bass_kernel_spmd

F32 = mybir.dt.float32
BF16 = mybir.dt.bfloat16
I32 = mybir.dt.int32
U32 = mybir.dt.uint32
AF = mybir.ActivationFunctionType
ALU = mybir.AluOpType
AX = mybir.AxisListType

NDS = 24
NDS_SW = 8


class Buf:
    def __init__(self, t, name):
        self.t = t
        self.name = name
        self.w = {}
        self.r = {}

    def __getitem__(self, idx):
        return self.t[idx]


class V:
    def __init__(self, buf, tag=None):
        self.buf = buf
        self.tag = tag


def _norm(x):
    if isinstance(x, V):
        return x.buf, x.tag
    return x, None


class FW:
    def __init__(self, nc, stack, same_engine_sync=True):
        self.nc = nc
        self.stack = stack
        self.eng = {"pe": nc.tensor, "dve": nc.vector, "act": nc.scalar,
                    "pool": nc.gpsimd, "sp": nc.sync}
        self.sem = {k: stack.enter_context(nc.semaphore("s_" + k)) for k in self.eng}
        self.cnt = {k: 0 for k in self.eng}
        self.waited = {k: {} for k in self.eng}
        self.dsem = [stack.enter_context(nc.semaphore("d%d" % i)) for i in range(NDS)]
        self.dcnt = 0
        self.dsem_sw = [stack.enter_context(nc.semaphore("w%d" % i)) for i in range(NDS_SW)]
        self.dcnt_sw = 0
        self.ses = same_engine_sync
        self.ninstr = 0

    def sb(self, name, shape, dt=F32):
        self.nalloc = getattr(self, "nalloc", 0) + 1
        name = "%s_%d" % (name, self.nalloc)
        return Buf(self.stack.enter_context(self.nc.sbuf_tensor(name, list(shape), dt)), name)

    def ps(self, name, shape, dt=F32):
        return Buf(self.stack.enter_context(self.nc.psum_tensor(name, list(shape), dt)), name)

    def dram(self, name, shape, dt=F32, kind="Internal"):
        return Buf(self.nc.dram_tensor(name, list(shape), dt, kind=kind).ap(), name)

    def _deps(self, reads, writes):
        deps = []
        for x in reads:
            b, tag = _norm(x)
            for tg, tok in b.w.items():
                if tag is None or tg is None or tg == tag:
                    deps.append(tok)
        for x in writes:
            b, tag = _norm(x)
            for tg, tok in b.w.items():
                if tag is None or tg is None or tg == tag:
                    deps.append(tok)
            for tg, toks in b.r.items():
                if tag is None or tg is None or tg == tag:
                    deps.extend(toks)
        return deps

    def _wait(self, ek, deps, skip_same=False):
        e = self.eng[ek]
        need = {}
        for (sem, val, src) in deps:
            if src == ek and (skip_same or not self.ses):
                continue
            key = id(sem)
            if self.waited[ek].get(key, 0) >= val:
                continue
            if key not in need or need[key][1] < val:
                need[key] = (sem, val)
        for key, (sem, val) in need.items():
            e.wait_ge(sem, val)
            self.waited[ek][key] = val
            self.ninstr += 1

    def _record(self, tok, reads, writes):
        for x in reads:
            b, tag = _norm(x)
            lst = b.r.setdefault(tag, [])
            lst[:] = [t for t in lst if t[0] is not tok[0]] + [tok]
        for x in writes:
            b, tag = _norm(x)
            if tag is None:
                b.w = {None: tok}
                b.r = {}
            else:
                b.w[tag] = tok
                b.r[tag] = []

    def op(self, ek, fn, reads=(), writes=(), skip_same=False):
        self._wait(ek, self._deps(reads, writes), skip_same=skip_same)
        ins = fn(self.eng[ek])
        self.cnt[ek] += 1
        ins.then_inc(self.sem[ek], 1)
        tok = (self.sem[ek], self.cnt[ek], ek)
        self._record(tok, reads, writes)
        self.ninstr += 1
        return tok

    def dma(self, qk, out, in_, reads=(), writes=(), indirect=None, **kw):
        self._wait(qk, self._deps(reads, writes))
        if qk == "pool":
            i = self.dcnt_sw % NDS_SW
            rnd = self.dcnt_sw // NDS_SW
            self.dcnt_sw += 1
            sem = self.dsem_sw[i]
        else:
            i = self.dcnt % NDS
            rnd = self.dcnt // NDS
            self.dcnt += 1
            sem = self.dsem[i]
        if rnd > 0:
            key = id(sem)
            if self.waited[qk].get(key, 0) < 16 * rnd:
                self.eng[qk].wait_ge(sem, 16 * rnd)
                self.waited[qk][key] = 16 * rnd
        if indirect is None:
            self.eng[qk].dma_start(out=out, in_=in_, **kw).then_inc(sem, 16)
        else:
            self.eng[qk].indirect_dma_start(out=out, in_=in_, **indirect).then_inc(sem, 16)
        tok = (sem, 16 * (rnd + 1), "dma")
        self._record(tok, reads, writes)
        self.ninstr += 1
        return tok

    def barrier(self):
        toks = [(self.sem[k], self.cnt[k], k) for k in self.eng if self.cnt[k] > 0]
        for i in range(NDS):
            n = (self.dcnt - i + NDS - 1) // NDS
            if n > 0:
                toks.append((self.dsem[i], 16 * n, "dma"))
        for i in range(NDS_SW):
            n = (self.dcnt_sw - i + NDS_SW - 1) // NDS_SW
            if n > 0:
                toks.append((self.dsem_sw[i], 16 * n, "dma"))
        for ek in self.eng:
            self._wait(ek, [t for t in toks if t[2] != ek], skip_same=True)

    def mm(self, out, lhsT, rhs, start, stop, reads, writes):
        return self.op("pe", lambda e: e.matmul(out, lhsT, rhs, start=start, stop=stop,
                                                skip_group_check=True),
                       reads=reads, writes=writes, skip_same=True)

    def tr(self, out, in_, ident, reads, writes):
        return self.op("pe", lambda e: e.transpose(out, in_, ident), reads=reads, writes=writes,
                       skip_same=True)


D = 1024
DEPTH_FULL = 4
NH = 16
HP = 64
NG = 2
DST = 128
CONVW = 4
CONVD = D + 2 * NG * DST
DSA_H = 8
DLAT = 128
IDX_H = 4
IDX_D = 64
MEM_LEN = 256
MEM_H = 4
MEM_D = 128
DMIX = 2048
NE = 32
TOPK = 4
DFF = 1024
LIM = 7.0
SW_ALPHA = 1.702
ALPHA = (2 * DEPTH_FULL) ** 0.25
LN_EPS = 1e-5
RMS_EPS = 1e-6
O_Z, O_XBC, O_DT, O_QL, O_CKV, O_QI, O_KI, O_WI, O_QM, N_IN = (
    0, 1024, 2560, 2576, 3600, 3728, 3984, 4048, 4052, 4564)
NEG = -1.0e30
EPS_TIE = 2.0 ** -30


def make_consts(S):
    c = {}
    c["c_ident"] = np.eye(128, dtype=np.float32)
    k = np.arange(128)
    c["c_U"] = (k[:, None] <= k[None, :]).astype(np.float32)
    c["c_SL"] = (k[:, None] > k[None, :]).astype(np.float32)
    c["c_SU"] = (k[:, None] < k[None, :]).astype(np.float32)
    c["c_ones"] = np.ones((128, 128), np.float32)
    c["c_cbm"] = np.where(k[None, :] <= k[:, None], 0.0, NEG).astype(np.float32)
    c["c_epspos"] = np.broadcast_to((-EPS_TIE * np.arange(S, dtype=np.float64)).astype(np.float32)[None, :],
                                    (128, S)).copy()
    import ml_dtypes
    c["c_i4"] = np.tile(np.eye(128, dtype=np.float32), (1, 4)).astype(ml_dtypes.bfloat16)
    sl = (2.0 ** (-8.0 * np.arange(1, DSA_H + 1) / DSA_H)).astype(np.float32)
    c["c_sl128"] = np.repeat(sl * 128.0, 128)[None, :].astype(np.float32)
    onei = np.zeros((2, 128), np.float32)
    onei[0] = 1.0
    onei[1] = np.arange(128)
    c["c_onei"] = onei
    c["c_slrow"] = np.repeat(sl, 128)[None, :].astype(np.float32)
    c["c_pow2"] = np.broadcast_to((2.0 ** -(np.arange(64) + 1.0)).astype(np.float32)[None, :], (128, 64)).copy()
    c["c_eidx"] = np.broadcast_to(np.arange(NE, dtype=np.float32)[None, :], (128, NE)).copy()
    return c


class MK:
    def __init__(self, S, depth, TS, CAP, NIT, NSEL, stop_after=None):
        self.S, self.L, self.TS, self.CAP, self.NIT, self.NSEL = S, depth, TS, CAP, NIT, NSEL
        self.NT = S // 128
        self.NU = S // TS
        self.TPU = TS // 128
        self.stop_after = stop_after
        self.nc = bass.Bass("TRN2", target_bir_lowering=False)

    def din(self, name, shape, dt=F32):
        return Buf(self.nc.dram_tensor(name, list(shape), dt, kind="ExternalInput").ap(), name)

    def build(self):
        nc, S, L, NT, CAP = self.nc, self.S, self.L, self.NT, self.CAP
        din = self.din
        self.x_in = din("x", [S, D]); self.mem_in = din("mem", [MEM_LEN, D])
        self.ln_in_g = din("ln_in_g", [1, D]); self.ln_in_b = din("ln_in_b", [1, D])
        self.w_in = din("w_in", [L, D, N_IN])
        self.conv_w = din("conv_w", [L, CONVW, CONVD]); self.conv_b = din("conv_b", [L, CONVD])
        self.dt_bias = din("dt_bias", [L, NH]); self.a_log = din("a_log", [L, NH]); self.d_skip = din("d_skip", [L, NH])
        self.ssd_norm_g = din("ssd_norm_g", [L, D]); self.kv_norm_g = din("kv_norm_g", [L, DLAT])
        self.w_uv = din("w_uv", [L, DSA_H, DLAT, 64])
        self.w_mem_k = din("w_mem_k", [L, D, 512]); self.w_mem_v = din("w_mem_v", [L, D, 512])
        self.w_out = din("w_out", [L, DMIX, D])
        self.ln1_g = din("ln1_g", [L, D]); self.ln1_b = din("ln1_b", [L, D])
        self.router_w = din("router_w", [L, D, NE]); self.router_b = din("router_b", [L, NE])
        self.w_gu = din("w_gu", [L, NE, D, 2 * DFF]); self.b_gu = din("b_gu", [L, NE, 2 * DFF])
        self.w_down = din("w_down", [L, NE, DFF, D]); self.b_down = din("b_down", [L, NE, D])
        self.ln2_g = din("ln2_g", [L, D]); self.ln2_b = din("ln2_b", [L, D])
        self.cin = {}
        for k, v in make_consts(S).items():
            self.cin[k] = din(k, list(v.shape), BF16 if v.dtype.itemsize == 2 else F32)
        self.out = Buf(nc.dram_tensor("out", [S, D], F32, kind="ExternalOutput").ap(), "out")

        with ExitStack() as st:
            f = self.f = FW(nc, st)
            self.H = f.dram("H", [S, D]); self.H2 = f.dram("H2", [S, D])
            self.Z = f.dram("Z", [S, D]); self.DTs = f.dram("DTs", [S, NH])
            self.X = f.dram("X", [S, D]); self.BTM = f.dram("BTM", [S, 256])
            self.BT = f.dram("BT", [128, 2, S]); self.CTs = f.dram("CTs", [128, 2, S])
            self.QL = f.dram("QL", [128, NT, 1024]); self.QI = f.dram("QI", [128, 2, S])
            self.KI = f.dram("KI", [128, S]); self.WI = f.dram("WI", [S, 4])
            self.CV = f.dram("CV", [S, 129]); self.CT = f.dram("CT", [128, S])
            self.QM = f.dram("QM", [128, 4, S])
            self.MIX = f.dram("MIX", [S, DMIX])
            self.GATE4 = f.dram("GATE4", [S, 4]); self.SLOT4 = f.dram("SLOT4", [S, 4], I32)
            self.XD = f.dram("XD", [NE * CAP, D]); self.YD = f.dram("YD", [NE * CAP, D])

            self.ident = f.sb("ident", [128, 128]); self.cU = f.sb("cU", [128, 128])
            self.cSL = f.sb("cSL", [128, 128]); self.cSU = f.sb("cSU", [128, 128])
            self.ones = f.sb("ones", [128, 128])
            for sbt, nm in ((self.ident, "c_ident"), (self.cU, "c_U"), (self.cSL, "c_SL"),
                            (self.cSU, "c_SU"), (self.ones, "c_ones")):
                f.dma("sp", sbt[:], self.cin[nm][:], writes=[sbt])
            self.epsln = f.sb("epsln", [128, 4])
            f.op("dve", lambda e: e.memset(self.epsln[:, 0:1], LN_EPS), writes=[self.epsln])
            f.op("dve", lambda e: e.memset(self.epsln[:, 1:2], RMS_EPS), reads=[self.epsln], writes=[self.epsln])
            f.op("dve", lambda e: e.memset(self.epsln[:, 2:3], 1.0), reads=[self.epsln], writes=[self.epsln])
            self.PS = f.ps("PS", [128, 4096])

            stages = [("ln_in", lambda: self.phase_ln_in())]
            for l in range(L):
                stages += [("a1_%d" % l, lambda l=l: self.phase_a1(l)),
                           ("ssd_%d" % l, lambda l=l: self.phase_ssd(l)),
                           ("a2_%d" % l, lambda l=l: self.phase_a2(l)),
                           ("dsa_%d" % l, lambda l=l: self.phase_dsa(l)),
                           ("mem_%d" % l, lambda l=l: self.phase_mem(l)),
                           ("outp_%d" % l, lambda l=l: self.phase_outp(l)),
                           ("moe_%d" % l, lambda l=l: self.phase_moe(l)),
                           ("comb_%d" % l, lambda l=l: self.phase_comb(l, last=(l == L - 1)))]
            for name, fn in stages:
                with ExitStack() as ph:
                    old = f.stack
                    f.stack = ph
                    fn()
                    f.barrier()
                    f.stack = old
                if self.stop_after == name:
                    break
        return nc

    def bc_reg(self):
        if getattr(self, "_bc_reg", None) is None:
            self._bc_reg = self.nc.gpsimd.to_reg(NE * self.CAP - 1)
        return self._bc_reg

    def bank(self, i, n=1):
        return self.PS[:, i * 512:(i + n) * 512]

    def bv(self, i, n=1):
        return [V(self.PS, j) for j in range(i, i + n)]

    def ln_tile(self, src, dst, g_bc, b_bc, scr, stats, mv):
        f, epsln = self.f, self.epsln
        f.op("dve", lambda e: e.bn_stats(stats[:, 0:6], src[:, 0:512]), reads=[src], writes=[stats])
        f.op("dve", lambda e: e.bn_stats(stats[:, 6:12], src[:, 512:1024]), reads=[src, stats], writes=[stats])
        f.op("dve", lambda e: e.bn_aggr(mv[:, 0:2], stats[:, 0:12]), reads=[stats], writes=[mv])
        f.op("act", lambda e: e.activation(mv[:, 2:3], mv[:, 1:2], AF.Sqrt, bias=epsln[:, 0:1], scale=1.0),
             reads=[mv, epsln], writes=[mv])
        f.op("dve", lambda e: e.reciprocal(mv[:, 3:4], mv[:, 2:3]), reads=[mv], writes=[mv])
        f.op("dve", lambda e: e.tensor_scalar(scr[:], src[:], mv[:, 0:1], mv[:, 3:4],
                                              op0=ALU.subtract, op1=ALU.mult), reads=[src, mv], writes=[scr])
        f.op("dve", lambda e: e.tensor_tensor(out=scr[:], in0=scr[:], in1=g_bc[:], op=ALU.mult),
             reads=[scr, g_bc], writes=[scr])
        f.op("dve", lambda e: e.tensor_tensor(out=dst[:], in0=scr[:], in1=b_bc[:], op=ALU.add),
             reads=[scr, b_bc], writes=[dst])

    def transposes(self, src_fn, nchunks, dst_fn, pbank, rd, wr, evac="act", scale=None):
        f, PS = self.f, self.PS
        for gi, c0 in enumerate(range(0, nchunks, 4)):
            n = min(4, nchunks - c0)
            bk = pbank + gi % 2
            for c in range(c0, c0 + n):
                f.tr(PS[:, bk * 512 + (c - c0) * 128: bk * 512 + (c - c0 + 1) * 128],
                     src_fn(c), self.ident[:], reads=list(rd) + [self.ident], writes=self.bv(bk))
            dst = dst_fn(c0, n)
            srcp = PS[:, bk * 512: bk * 512 + n * 128].rearrange("p (c t) -> p c t", t=128)
            if scale is not None:
                f.op("act", lambda e: e.activation(dst, srcp, AF.Copy, scale=scale), reads=self.bv(bk), writes=wr)
            elif evac == "act":
                f.op("act", lambda e: e.copy(dst, srcp), reads=self.bv(bk), writes=wr)
            else:
                f.op("dve", lambda e: e.tensor_copy(dst, srcp), reads=self.bv(bk), writes=wr)

    def bcast_load(self, name, src_row_ap, n):
        t = self.f.sb(name, [128, n])
        self.f.dma("sp", t[:], src_row_ap.partition_broadcast(128), writes=[t])
        return t

    def phase_ln_in(self):
        f, NT = self.f, self.NT
        g_bc = self.bcast_load("p0_g", self.ln_in_g[0, :], D)
        b_bc = self.bcast_load("p0_b", self.ln_in_b[0, :], D)
        xt = [f.sb("p0_x%d" % i, [128, D]) for i in range(2)]
        ot = [f.sb("p0_o%d" % i, [128, D]) for i in range(2)]
        scr = f.sb("p0_scr", [128, D]); stats = f.sb("p0_st", [128, 12]); mv = f.sb("p0_mv", [128, 4])
        for i in range(NT):
            a, o = xt[i % 2], ot[i % 2]
            f.dma("sp", a[:], self.x_in[i * 128:(i + 1) * 128, :], writes=[a])
            self.ln_tile(a, o, g_bc, b_bc, scr, stats, mv)
            f.dma("sp", self.H[i * 128:(i + 1) * 128, :], o[:], reads=[o], writes=[V(self.H, i)])

    def load_hT(self, hT, u, htiles):
        f = self.f
        for i in range(self.TPU):
            ti = u * self.TPU + i
            a = htiles[ti % 2]
            f.dma("sp", a[:], self.H[ti * 128:(ti + 1) * 128, :], reads=[V(self.H, ti)], writes=[a])
            self.transposes(lambda c: a[:, c * 128:(c + 1) * 128], 8,
                            lambda c0, n: hT[:, c0:c0 + n, i * 128:(i + 1) * 128], 0, [a], [hT])

    def phase_a1(self, l):
        f, TS, TPU, NU = self.f, self.TS, self.TPU, self.NU
        NW = O_QL
        W1 = f.sb("a1_W", [128, 8, NW])
        for kc in range(8):
            f.dma("sp", W1[:, kc, :], self.w_in[l, kc * 128:(kc + 1) * 128, 0:NW], writes=[V(W1, kc)])
        CW = f.sb("a1_cw", [128, 12, 4]); CBs = f.sb("a1_cb", [128, 12])
        for j in range(4):
            f.dma("sp", CW[:, :, j], self.conv_w[l, j, :].rearrange("(c p) -> p c", p=128), writes=[V(CW, j)],
                  allow_slow_non_contiguous=True)
        f.dma("sp", CBs[:], self.conv_b[l, :].rearrange("(c p) -> p c", p=128), writes=[CBs],
              allow_slow_non_contiguous=True)
        dtb = self.bcast_load("a1_dtb", self.dt_bias[l, :], NH)
        hT = f.sb("a1_hT", [128, 8, TS])
        htiles = [f.sb("a1_h%d" % i, [128, D]) for i in range(2)]
        xpre = f.sb("a1_xpre", [128, 12, TS + 3])
        xc = f.sb("a1_xc", [128, 12, TS])
        acc = f.sb("a1_acc", [128, TS])
        zt = [f.sb("a1_z%d" % i, [128, D]) for i in range(2)]
        dtt = f.sb("a1_dt", [128, NH]); dte = f.sb("a1_dte", [128, NH])
        xtm = [f.sb("a1_xtm%d" % i, [128, D + 256]) for i in range(2)]
        f.op("dve", lambda e: e.memset(xpre[:, :, 0:3], 0.0), writes=[xpre])
        for u in range(NU):
            self.load_hT(hT, u, htiles)
            for i in range(TPU):
                ti = u * TPU + i
                z = zt[ti % 2]
                for sl in range(2):
                    bk = 2 + sl
                    for kc in range(8):
                        f.mm(self.bank(bk), hT[:, kc, i * 128:(i + 1) * 128], W1[:, kc, sl * 512:(sl + 1) * 512],
                             kc == 0, kc == 7, [hT, V(W1, kc)], self.bv(bk))
                    f.op("act", lambda e: e.copy(z[:, sl * 512:(sl + 1) * 512], self.bank(bk)),
                         reads=self.bv(bk), writes=[z])
                f.dma("sp", self.Z[ti * 128:(ti + 1) * 128, :], z[:], reads=[z], writes=[V(self.Z, ti)])
                for kc in range(8):
                    f.mm(self.PS[:, 4 * 512:4 * 512 + NH], hT[:, kc, i * 128:(i + 1) * 128],
                         W1[:, kc, O_DT:O_DT + NH], kc == 0, kc == 7, [hT, V(W1, kc)], self.bv(4))
                f.op("dve", lambda e: e.tensor_tensor(out=dte[:], in0=self.PS[:, 4 * 512:4 * 512 + NH], in1=dtb[:],
                                                      op=ALU.add), reads=self.bv(4) + [dtb], writes=[dte])
                f.op("act", lambda e: e.activation(dte[:], dte[:], AF.Exp), reads=[dte], writes=[dte])
                f.op("act", lambda e: e.activation(dtt[:], dte[:], AF.Ln, bias=self.epsln[:, 2:3], scale=1.0),
                     reads=[dte, self.epsln], writes=[dtt])
                f.dma("sp", self.DTs[ti * 128:(ti + 1) * 128, :], dtt[:], reads=[dtt], writes=[V(self.DTs, ti)])
            for cc in range(12):
                bk = 5 + cc % 2
                for kc in range(8):
                    f.mm(self.PS[:, bk * 512: bk * 512 + TS], W1[:, kc, O_XBC + cc * 128: O_XBC + (cc + 1) * 128],
                         hT[:, kc, :], kc == 0, kc == 7, [hT, V(W1, kc)], self.bv(bk))
                f.op("act", lambda e: e.copy(xpre[:, cc, 3:3 + TS], self.PS[:, bk * 512: bk * 512 + TS]),
                     reads=self.bv(bk), writes=[V(xpre, cc)])
                f.op("dve", lambda e: e.tensor_scalar(acc[:], xpre[:, cc, 0:TS], CW[:, cc, 0:1], CBs[:, cc:cc + 1],
                                                      op0=ALU.mult, op1=ALU.add),
                     reads=[V(xpre, cc), CW, CBs], writes=[acc])
                for j in range(1, 4):
                    f.op("dve", lambda e: e.scalar_tensor_tensor(out=acc[:], in0=xpre[:, cc, j:j + TS],
                                                                 scalar=CW[:, cc, j:j + 1], in1=acc[:],
                                                                 op0=ALU.mult, op1=ALU.add),
                         reads=[V(xpre, cc), CW, acc], writes=[acc])
                f.op("act", lambda e: e.activation(xc[:, cc, :], acc[:], AF.Silu), reads=[acc], writes=[V(xc, cc)])
                f.op("dve", lambda e: e.tensor_copy(xpre[:, cc, 0:3], xpre[:, cc, TS:TS + 3]),
                     reads=[V(xpre, cc)], writes=[V(xpre, cc)])
            f.dma("sp", self.BT[:, :, u * TS:(u + 1) * TS], xc[:, 8:10, :], reads=[V(xc, 8), V(xc, 9)],
                  writes=[V(self.BT, u)])
            f.dma("sp", self.CTs[:, :, u * TS:(u + 1) * TS], xc[:, 10:12, :], reads=[V(xc, 10), V(xc, 11)],
                  writes=[V(self.CTs, u)])
            for i in range(TPU):
                ti = u * TPU + i
                xm = xtm[ti % 2]
                self.transposes(lambda c: xc[:, c, i * 128:(i + 1) * 128], 10,
                                lambda c0, n: xm[:, c0 * 128:(c0 + n) * 128].rearrange("p (c t) -> p c t", t=128),
                                0, [xc], [xm], evac="dve")
                f.dma("sp", self.X[ti * 128:(ti + 1) * 128, :], xm[:, 0:D], reads=[xm], writes=[V(self.X, ti)])
                f.dma("sp", self.BTM[ti * 128:(ti + 1) * 128, :], xm[:, D:D + 256], reads=[xm],
                      writes=[V(self.BTM, ti)])

    def phase_ssd(self, l):
        f, NT = self.f, self.NT
        PS = self.PS
        cU, cSL, ones = self.cU, self.cSL, self.ones
        Abc = self.bcast_load("s_A", self.a_log[l, :], NH)
        f.op("act", lambda e: e.activation(Abc[:], Abc[:], AF.Exp), reads=[Abc], writes=[Abc])
        f.op("dve", lambda e: e.tensor_scalar(Abc[:], Abc[:], -1.0, None, op0=ALU.mult), reads=[Abc], writes=[Abc])
        Dbc = self.bcast_load("s_D", self.d_skip[l, :], NH)
        NGb = self.bcast_load("s_ng", self.ssd_norm_g[l, :], D)
        ST = f.sb("s_ST", [128, 2, 512])
        f.op("dve", lambda e: e.memset(ST[:], 0.0), writes=[ST])
        xt = [f.sb("s_x%d" % i, [128, NH, HP]) for i in range(2)]
        zt = [f.sb("s_z%d" % i, [128, D]) for i in range(2)]
        dtt = [f.sb("s_dt%d" % i, [128, NH]) for i in range(2)]
        bct = [f.sb("s_bc%d" % i, [128, 4, 128]) for i in range(2)]
        btm = [f.sb("s_bm%d" % i, [128, 256]) for i in range(2)]
        a = f.sb("s_a", [128, NH]); acum = f.sb("s_acum", [128, NH]); eacum = f.sb("s_eacum", [128, NH])
        alast = f.sb("s_alast", [128, NH]); ealast = f.sb("s_ealast", [128, NH]); dend = f.sb("s_dend", [128, NH])
        xdt = f.sb("s_xdt", [128, NH, HP]); xdec = f.sb("s_xdec", [128, NH, HP])
        mCB = f.sb("s_mcb", [128, 128])
        Wa = [f.sb("s_wa%d" % i, [128, 4, 128]) for i in range(2)]
        LT = [f.sb("s_lt%d" % i, [128, 4, 128]) for i in range(2)]
        G = [f.sb("s_g%d" % i, [128, 4, 128]) for i in range(2)]
        y = f.sb("s_y", [128, NH, HP]); yo = f.sb("s_yo", [128, 8, HP])
        sz = f.sb("s_sz", [128, D]); ss = f.sb("s_ss", [128, 4]); junk = f.sb("s_junk", [128, 512])
        yout = [f.sb("s_yout%d" % i, [128, D]) for i in range(2)]
        for c in range(NT):
            x_, z_, d_, bc_, bm_ = xt[c % 2], zt[c % 2], dtt[c % 2], bct[c % 2], btm[c % 2]
            tok = slice(c * 128, (c + 1) * 128)
            f.dma("sp", x_[:].rearrange("p h d -> p (h d)"), self.X[tok, :], reads=[V(self.X, c)], writes=[x_])
            f.dma("sp", z_[:], self.Z[tok, :], reads=[V(self.Z, c)], writes=[z_])
            f.dma("sp", d_[:], self.DTs[tok, :], reads=[V(self.DTs, c)], writes=[d_])
            f.dma("sp", bc_[:, 0:2, :], self.BT[:, :, tok], reads=[self.BT], writes=[V(bc_, 0)])
            f.dma("sp", bc_[:, 2:4, :], self.CTs[:, :, tok], reads=[self.CTs], writes=[V(bc_, 1)])
            f.dma("sp", bm_[:], self.BTM[tok, :], reads=[V(self.BTM, c)], writes=[bm_])
            f.op("dve", lambda e: e.tensor_tensor(out=a[:], in0=d_[:], in1=Abc[:], op=ALU.mult),
                 reads=[d_, Abc], writes=[a])
            f.mm(PS[:, 0:NH], cU[:], a[:], True, True, [cU, a], self.bv(0))
            f.mm(PS[:, 512:512 + NH], ones[:], a[:], True, True, [ones, a], self.bv(1))
            f.op("dve", lambda e: e.tensor_copy(acum[:], PS[:, 0:NH]), reads=self.bv(0), writes=[acum])
            f.op("act", lambda e: e.activation(eacum[:], PS[:, 0:NH], AF.Exp), reads=self.bv(0), writes=[eacum])
            f.op("dve", lambda e: e.tensor_tensor(out=dend[:], in0=PS[:, 512:512 + NH], in1=acum[:], op=ALU.subtract),
                 reads=self.bv(1) + [acum], writes=[dend])
            f.op("act", lambda e: e.activation(dend[:], dend[:], AF.Exp), reads=[dend], writes=[dend])
            f.op("act", lambda e: e.activation(ealast[:], PS[:, 512:512 + NH], AF.Exp), reads=self.bv(1), writes=[ealast])
            f.op("dve", lambda e: e.tensor_tensor(out=xdt[:], in0=x_[:], in1=d_[:].unsqueeze(2).to_broadcast([128, NH, HP]),
                                                  op=ALU.mult), reads=[x_, d_], writes=[xdt])
            f.op("dve", lambda e: e.tensor_tensor(out=xdec[:], in0=xdt[:],
                                                  in1=dend[:].unsqueeze(2).to_broadcast([128, NH, HP]), op=ALU.mult),
                 reads=[xdt, dend], writes=[xdec])
            for g in range(2):
                f.mm(PS[:, 2 * 512:2 * 512 + 128], bc_[:, g, :], bc_[:, 2 + g, :], True, True, [bc_], self.bv(2))
                f.op("dve", lambda e: e.tensor_tensor(out=mCB[:], in0=PS[:, 2 * 512:2 * 512 + 128], in1=cU[:], op=ALU.mult),
                     reads=self.bv(2) + [cU], writes=[mCB])
                f.mm(self.bank(3), bc_[:, 2 + g, :], ST[:, g, :], True, True, [bc_, V(ST, g)], self.bv(3))
                for sg in range(2):
                    h0 = g * 8 + sg * 4
                    wa, lt, gg = Wa[sg], LT[sg], G[sg]
                    for hh in range(4):
                        f.op("pool", lambda e: e.tensor_scalar(wa[:, hh, :], cSL[:], a[:, h0 + hh:h0 + hh + 1], None,
                                                               op0=ALU.mult), reads=[cSL, a], writes=[V(wa, hh)])
                    bk = 4 + sg
                    for hh in range(4):
                        f.mm(PS[:, bk * 512 + hh * 128: bk * 512 + (hh + 1) * 128], wa[:, hh, :], cU[:], True, True,
                             [V(wa, hh), cU], self.bv(bk))
                    f.op("act", lambda e: e.activation(lt[:].rearrange("p h l -> p (h l)"), self.bank(bk), AF.Exp),
                         reads=self.bv(bk), writes=[lt])
                    f.op("dve", lambda e: e.tensor_tensor(out=gg[:], in0=lt[:],
                                                          in1=mCB[:].unsqueeze(1).to_broadcast([128, 4, 128]),
                                                          op=ALU.mult), reads=[lt, mCB], writes=[gg])
                    for hh in range(4):
                        h = h0 + hh
                        hl = sg * 4 + hh
                        f.mm(PS[:, 6 * 512 + hl * 64: 6 * 512 + (hl + 1) * 64], gg[:, hh, :], xdt[:, h, :], True, True,
                             [gg, xdt], self.bv(6))
                f.op("dve", lambda e: e.tensor_tensor(
                    out=yo[:], in0=self.bank(3).rearrange("p (h d) -> p h d", d=HP),
                    in1=eacum[:, g * 8:(g + 1) * 8].unsqueeze(2).to_broadcast([128, 8, HP]), op=ALU.mult),
                    reads=self.bv(3) + [eacum], writes=[yo])
                f.op("dve", lambda e: e.tensor_tensor(
                    out=y[:, g * 8:(g + 1) * 8, :], in0=self.bank(6).rearrange("p (h d) -> p h d", d=HP),
                    in1=yo[:], op=ALU.add), reads=self.bv(6) + [yo], writes=[V(y, g)])
                f.mm(self.bank(7), bm_[:, g * 128:(g + 1) * 128], xdec[:, g * 8:(g + 1) * 8, :].rearrange("p h d -> p (h d)"),
                     True, True, [bm_, xdec], self.bv(7))
                f.op("dve", lambda e: e.tensor_tensor(
                    out=ST[:, g, :].rearrange("p (h d) -> p h d", d=HP),
                    in0=ST[:, g, :].rearrange("p (h d) -> p h d", d=HP),
                    in1=ealast[:, g * 8:(g + 1) * 8].unsqueeze(2).to_broadcast([128, 8, HP]), op=ALU.mult),
                    reads=[V(ST, g), ealast], writes=[V(ST, g)])
                f.op("dve", lambda e: e.tensor_tensor(out=ST[:, g, :], in0=ST[:, g, :], in1=self.bank(7), op=ALU.add),
                     reads=[V(ST, g)] + self.bv(7), writes=[V(ST, g)])
            f.op("dve", lambda e: e.tensor_tensor(out=xdt[:], in0=x_[:],
                                                  in1=Dbc[:].unsqueeze(2).to_broadcast([128, NH, HP]), op=ALU.mult),
                 reads=[x_, Dbc], writes=[xdt])
            f.op("dve", lambda e: e.tensor_tensor(out=y[:], in0=y[:], in1=xdt[:], op=ALU.add),
                 reads=[y, xdt], writes=[y])
            f.op("act", lambda e: e.activation(sz[:], z_[:], AF.Silu), reads=[z_], writes=[sz])
            yf = y[:].rearrange("p h d -> p (h d)")
            f.op("dve", lambda e: e.tensor_tensor(out=sz[:], in0=sz[:], in1=yf, op=ALU.mult), reads=[sz, y], writes=[sz])
            for g in range(2):
                f.op("act", lambda e: e.activation(junk[:], sz[:, g * 512:(g + 1) * 512], AF.Square,
                                                   accum_out=ss[:, g:g + 1]), reads=[sz], writes=[junk, V(ss, g)])
            f.op("act", lambda e: e.activation(ss[:, 2:4], ss[:, 0:2], AF.Sqrt, bias=self.epsln[:, 1:2], scale=1.0 / 512),
                 reads=[ss, self.epsln], writes=[ss])
            f.op("dve", lambda e: e.reciprocal(ss[:, 2:4], ss[:, 2:4]), reads=[ss], writes=[ss])
            yo_ = yout[c % 2]
            for g in range(2):
                f.op("dve", lambda e: e.scalar_tensor_tensor(out=yo_[:, g * 512:(g + 1) * 512], in0=sz[:, g * 512:(g + 1) * 512],
                                                             scalar=ss[:, 2 + g:3 + g], in1=NGb[:, g * 512:(g + 1) * 512],
                                                             op0=ALU.mult, op1=ALU.mult),
                     reads=[sz, ss, NGb], writes=[yo_])
            f.dma("sp", self.MIX[tok, 0:D], yo_[:], reads=[yo_], writes=[V(self.MIX, ("ssd", c))])

    def phase_a2(self, l):
        f, TS, TPU, NU = self.f, self.TS, self.TPU, self.NU
        PS = self.PS
        NW = N_IN - O_QL
        oQL, oCKV, oQI, oKI, oWI, oQM = 0, O_CKV - O_QL, O_QI - O_QL, O_KI - O_QL, O_WI - O_QL, O_QM - O_QL
        W2 = f.sb("a2_W", [128, 8, NW])
        WK = f.sb("a2_WK", [128, 8, 128])
        for kc in range(8):
            f.dma("sp", W2[:, kc, :], self.w_in[l, kc * 128:(kc + 1) * 128, O_QL:N_IN], writes=[V(W2, kc)])
            for hf in range(2):
                f.dma("sp", WK[:, kc, hf * 64:(hf + 1) * 64], self.w_in[l, kc * 128:(kc + 1) * 128, O_KI:O_KI + 64],
                      writes=[V(WK, (kc, hf))])
        KVG = self.bcast_load("a2_kvg", self.kv_norm_g[l, :], DLAT)
        hT = f.sb("a2_hT", [128, 8, TS])
        htiles = [f.sb("a2_h%d" % i, [128, D]) for i in range(2)]
        qst = f.sb("a2_qst", [128, TPU, 8, 128])
        qist = f.sb("a2_qist", [128, 2, TS]); kist = f.sb("a2_kist", [128, TS])
        qmst = f.sb("a2_qmst", [128, 4, TS]); ctst = f.sb("a2_ctst", [128, TS])
        cv = [f.sb("a2_cv%d" % i, [128, 129]) for i in range(2)]
        wit = [f.sb("a2_wi%d" % i, [128, 4]) for i in range(2)]
        ss = f.sb("a2_ss", [128, 4]); junk = f.sb("a2_junk", [128, 128])
        for c_ in cv:
            f.op("dve", lambda e: e.memset(c_[:, 128:129], 1.0), writes=[V(c_, "one")])
        sc_lat = DLAT ** -0.5
        sc_mem = MEM_D ** -0.5
        nb = 0
        for u in range(NU):
            self.load_hT(hT, u, htiles)
            tsl = slice(u * TS, (u + 1) * TS)

            def fm_chunk(wcols, dst, scale, wr):
                nonlocal nb
                bk = 2 + nb % 2
                nb += 1
                for kc in range(8):
                    f.mm(PS[:, bk * 512: bk * 512 + TS], wcols(kc), hT[:, kc, :], kc == 0, kc == 7,
                         [hT, W2, WK], self.bv(bk))
                src = PS[:, bk * 512: bk * 512 + TS]
                if len(dst.shape) == 3:
                    src = src.rearrange("p (i t) -> p i t", t=128)
                if scale is None:
                    f.op("act", lambda e: e.copy(dst, src), reads=self.bv(bk), writes=wr)
                else:
                    f.op("act", lambda e: e.activation(dst, src, AF.Copy, scale=scale), reads=self.bv(bk), writes=wr)

            for h in range(8):
                fm_chunk(lambda kc: W2[:, kc, oQL + h * 128: oQL + (h + 1) * 128],
                         qst[:, :, h, :], sc_lat, [V(qst, h)])
            f.dma("sp", self.QL[:, u * TPU:(u + 1) * TPU, :], qst[:].rearrange("p i h t -> p i (h t)"),
                  reads=[qst], writes=[V(self.QL, u)])
            for j in range(2):
                fm_chunk(lambda kc: W2[:, kc, oQI + j * 128: oQI + (j + 1) * 128], qist[:, j, :], None, [V(qist, j)])
            f.dma("sp", self.QI[:, :, tsl], qist[:], reads=[qist], writes=[V(self.QI, u)])
            fm_chunk(lambda kc: WK[:, kc, :], kist[:], None, [kist])
            f.dma("sp", self.KI[:, tsl], kist[:], reads=[kist], writes=[V(self.KI, u)])
            for h in range(4):
                fm_chunk(lambda kc: W2[:, kc, oQM + h * 128: oQM + (h + 1) * 128], qmst[:, h, :], sc_mem, [V(qmst, h)])
            f.dma("sp", self.QM[:, :, tsl], qmst[:], reads=[qmst], writes=[V(self.QM, u)])
            for i in range(TPU):
                ti = u * TPU + i
                tok = slice(ti * 128, (ti + 1) * 128)
                c_, w_ = cv[ti % 2], wit[ti % 2]
                for kc in range(8):
                    f.mm(PS[:, 4 * 512: 4 * 512 + 128], hT[:, kc, i * 128:(i + 1) * 128], W2[:, kc, oCKV:oCKV + 128],
                         kc == 0, kc == 7, [hT, W2], self.bv(4))
                for kc in range(8):
                    f.mm(PS[:, 5 * 512: 5 * 512 + 4], hT[:, kc, i * 128:(i + 1) * 128], W2[:, kc, oWI:oWI + 4],
                         kc == 0, kc == 7, [hT, W2], self.bv(5))
                f.op("act", lambda e: e.copy(w_[:], PS[:, 5 * 512: 5 * 512 + 4]), reads=self.bv(5), writes=[w_])
                f.dma("sp", self.WI[tok, :], w_[:], reads=[w_], writes=[V(self.WI, ti)])
                f.op("act", lambda e: e.activation(junk[:], PS[:, 4 * 512: 4 * 512 + 128], AF.Square, accum_out=ss[:, 0:1]),
                     reads=self.bv(4), writes=[junk, ss])
                f.op("act", lambda e: e.activation(ss[:, 1:2], ss[:, 0:1], AF.Sqrt, bias=self.epsln[:, 1:2], scale=1.0 / DLAT),
                     reads=[ss, self.epsln], writes=[ss])
                f.op("dve", lambda e: e.reciprocal(ss[:, 2:3], ss[:, 1:2]), reads=[ss], writes=[ss])
                f.op("dve", lambda e: e.scalar_tensor_tensor(out=c_[:, 0:128], in0=PS[:, 4 * 512: 4 * 512 + 128],
                                                             scalar=ss[:, 2:3], in1=KVG[:], op0=ALU.mult, op1=ALU.mult),
                     reads=self.bv(4) + [ss, KVG], writes=[V(c_, "c")])
                f.dma("sp", self.CV[tok, :], c_[:], reads=[c_], writes=[V(self.CV, ti)])
                f.tr(PS[:, 6 * 512: 6 * 512 + 128], c_[:, 0:128], self.ident[:], [V(c_, "c"), self.ident], self.bv(6))
                f.op("act", lambda e: e.copy(ctst[:, i * 128:(i + 1) * 128], PS[:, 6 * 512: 6 * 512 + 128]),
                     reads=self.bv(6), writes=[V(ctst, i)])
            f.dma("sp", self.CT[:, tsl], ctst[:], reads=[ctst], writes=[V(self.CT, u)])

    def phase_dsa(self, l):
        f, S, NT, NIT, NSEL = self.f, self.S, self.NT, self.NIT, self.NSEL
        PS = self.PS
        CTr = f.sb("d_CT", [128, S]); EPSP = f.sb("d_eps", [128, S])
        VAt = [f.sb("d_VA%d" % i, [128, 129]) for i in range(4)]
        f.dma("sp", CTr[:], self.CT[:], reads=[self.CT], writes=[CTr])
        f.dma("sp", EPSP[:], self.cin["c_epspos"][:], writes=[EPSP])
        WUV = f.sb("d_wuv", [128, 8, 64])
        f.dma("sp", WUV[:], self.w_uv[l].rearrange("h c d -> c h d"), writes=[WUV])
        I4 = f.sb("d_i4", [128, 512], BF16); CBM = f.sb("d_cbm", [128, 128]); ONEI = f.sb("d_onei", [2, 128])
        POW2 = f.sb("d_pow2", [128, 64])
        f.dma("sp", I4[:], self.cin["c_i4"][:], writes=[I4])
        f.dma("sp", CBM[:], self.cin["c_cbm"][:], writes=[CBM])
        f.dma("sp", ONEI[:], self.cin["c_onei"][:], writes=[ONEI])
        f.dma("sp", POW2[:], self.cin["c_pow2"][:], writes=[POW2])
        RB = [f.sb("d_rb%d" % i, [2, 1024]) for i in range(2)]
        SL128 = f.sb("d_sl128", [1, 1024])
        f.dma("sp", SL128[:], self.cin["c_sl128"][:], writes=[SL128])
        for r in RB:
            f.dma("sp", r[1:2, :], self.cin["c_slrow"][:], writes=[V(r, 1)])
        SC = f.sb("d_SC", [128, S]); MB = f.sb("d_MB", [128, S], BF16)
        Pt = [f.sb("d_P%d" % i, [128, 1024]) for i in range(2)]
        QLb = [f.sb("d_ql%d" % i, [128, 1024]) for i in range(2)]
        QIb = [f.sb("d_qi%d" % i, [128, 2, 128]) for i in range(2)]
        WIb = [f.sb("d_wi%d" % i, [128, 4]) for i in range(2)]
        KIs = [f.sb("d_ki%d" % i, [128, 512]) for i in range(2)]
        rl = [f.sb("d_rl%d" % i, [128, 512]) for i in range(2)]
        sm = f.sb("d_sm", [128, 16])
        hs = f.sb("d_hs", [128, 64])
        ctx = f.sb("d_ctx", [128, 8, 128]); ctxT = f.sb("d_ctxT", [128, 8, 128])
        rinv = f.sb("d_rinv", [128, 8]); yd = [f.sb("d_yd%d" % i, [128, 512]) for i in range(2)]
        ACC = PS[:, 4 * 512: 8 * 512].rearrange("p (h c) -> p h c", c=256)
        nrl = 0
        nkt = 0
        for b in range(NT):
            t0 = b * 128
            te = t0 + 128
            ql, qi, wi = QLb[b % 2], QIb[b % 2], WIb[b % 2]
            f.dma("sp", ql[:], self.QL[:, b, :], reads=[self.QL], writes=[ql])
            f.dma("sp", qi[:], self.QI[:, :, t0:te], reads=[self.QI], writes=[qi])
            f.dma("sp", wi[:], self.WI[t0:te, :], reads=[self.WI], writes=[wi])
            nsl = (te + 511) // 512
            for si in range(nsl):
                c0 = si * 512
                ncol = min(512, te - c0)
                ks = KIs[si % 2]
                f.dma("sp", ks[:, 0:ncol], self.KI[:, c0:c0 + ncol], reads=[self.KI], writes=[ks])
                for hi in range(4):
                    bk = nrl % 4
                    r_ = rl[nrl % 2]
                    nrl += 1
                    pb = (hi % 2) * 64
                    f.mm(PS[:, bk * 512: bk * 512 + ncol], qi[pb:pb + 64, hi // 2, :], ks[pb:pb + 64, 0:ncol], True, True,
                         [qi, ks], self.bv(bk))
                    f.op("act", lambda e: e.activation(r_[:, 0:ncol], PS[:, bk * 512: bk * 512 + ncol], AF.Relu),
                         reads=self.bv(bk), writes=[r_])
                    prev = EPSP if hi == 0 else SC
                    f.op("dve", lambda e: e.scalar_tensor_tensor(out=SC[:, c0:c0 + ncol], in0=r_[:, 0:ncol],
                                                                 scalar=wi[:, hi:hi + 1], in1=prev[:, c0:c0 + ncol],
                                                                 op0=ALU.mult, op1=ALU.add),
                         reads=[r_, wi, EPSP, SC], writes=[SC])
            f.op("dve", lambda e: e.tensor_reduce(out=sm[:, 0:1], in_=SC[:, 0:te], axis=AX.X, op=ALU.min),
                 reads=[SC], writes=[V(sm, 0)])
            f.op("dve", lambda e: e.tensor_tensor(out=SC[:, t0:te], in0=SC[:, t0:te], in1=CBM[:], op=ALU.add),
                 reads=[SC, CBM], writes=[SC])
            f.op("dve", lambda e: e.tensor_reduce(out=sm[:, 1:2], in_=SC[:, 0:te], axis=AX.X, op=ALU.max),
                 reads=[SC], writes=[V(sm, 1)])
            f.op("dve", lambda e: e.tensor_tensor(out=sm[:, 7:8], in0=sm[:, 1:2], in1=sm[:, 0:1], op=ALU.subtract),
                 reads=[V(sm, 0), V(sm, 1)], writes=[V(sm, 7)])
            f.op("dve", lambda e: e.tensor_scalar(sm[:, 7:8], sm[:, 7:8], 1.001, 1e-6, op0=ALU.mult, op1=ALU.add),
                 reads=[V(sm, 7)], writes=[V(sm, 7)])
            f.op("dve", lambda e: e.tensor_scalar(hs[:, 0:NIT], POW2[:, 0:NIT], sm[:, 7:8], None, op0=ALU.mult),
                 reads=[POW2, V(sm, 7)], writes=[hs])
            f.op("dve", lambda e: e.tensor_copy(sm[:, 2:3], sm[:, 0:1]), reads=[V(sm, 0)], writes=[V(sm, 2)])
            for it in range(NIT):
                f.op("dve", lambda e: e.tensor_tensor(out=sm[:, 3:4], in0=sm[:, 2:3], in1=hs[:, it:it + 1], op=ALU.add),
                     reads=[V(sm, 2), hs], writes=[V(sm, 3)])
                f.op("dve", lambda e: e.tensor_scalar(MB[:, 0:te], SC[:, 0:te], sm[:, 3:4], 0.0, op0=ALU.is_ge, op1=ALU.add,
                                                      accum_out=sm[:, 4:5]), reads=[SC, V(sm, 3)], writes=[MB, V(sm, 4)])
                f.op("dve", lambda e: e.tensor_scalar(sm[:, 5:6], sm[:, 4:5], float(NSEL) - 0.5, hs[:, it:it + 1],
                                                      op0=ALU.is_ge, op1=ALU.mult), reads=[V(sm, 4), hs], writes=[V(sm, 5)])
                f.op("dve", lambda e: e.tensor_tensor(out=sm[:, 2:3], in0=sm[:, 2:3], in1=sm[:, 5:6], op=ALU.add),
                     reads=[V(sm, 2), V(sm, 5)], writes=[V(sm, 2)])
            f.op("dve", lambda e: e.tensor_scalar(MB[:, 0:te], SC[:, 0:te], sm[:, 2:3], NEG, op0=ALU.is_lt, op1=ALU.mult),
                 reads=[SC, V(sm, 2)], writes=[MB])
            f.op("dve", lambda e: e.tensor_tensor(out=SC[:, 0:te], in0=MB[:, 0:te], in1=EPSP[:, 0:te], op=ALU.subtract),
                 reads=[EPSP, MB], writes=[SC])
            f.op("dve", lambda e: e.tensor_reduce(out=sm[:, 6:7], in_=SC[:, 0:te], axis=AX.X, op=ALU.max),
                 reads=[SC], writes=[V(sm, 6)])
            f.tr(PS[0:1, 0:128], sm[:, 6:7], self.ident[:], [V(sm, 6), self.ident], self.bv(0))
            rb0 = RB[nkt % 2]
            f.op("dve", lambda e: e.scalar_tensor_tensor(out=rb0[0:1, :].rearrange("p (h t) -> p h t", t=128),
                                                         in0=PS[0:1, 0:128].unsqueeze(1).to_broadcast([1, 8, 128]),
                                                         scalar=-(2.0 ** 23),
                                                         in1=SL128[:].rearrange("p (h t) -> p h t", t=128),
                                                         op0=ALU.mult, op1=ALU.mult),
                 reads=[SL128] + self.bv(0), writes=[V(rb0, 0)])
            for j in range(b + 1):
                rb = RB[nkt % 2]
                p_ = Pt[nkt % 2]
                lb = (nkt % 2) * 2
                nkt += 1
                va = VAt[nkt % 4]
                f.dma("sp", va[:], self.CV[j * 128:(j + 1) * 128, :], reads=[self.CV], writes=[va])
                if j > 0:
                    rbp = RB[(nkt - 2) % 2]
                    f.op("pool", lambda e: e.tensor_tensor(out=rb[0:1, :], in0=rbp[0:1, :], in1=SL128[:], op=ALU.add),
                         reads=[V(rbp, 0), SL128], writes=[V(rb, 0)])
                for g in range(2):
                    o_ = self.bank(lb + g)
                    f.mm(o_, CTr[:, j * 128:(j + 1) * 128], ql[:, g * 512:(g + 1) * 512], True, False, [CTr, ql], self.bv(lb + g))
                    f.mm(o_, ONEI[0:2, :], rb[0:2, g * 512:(g + 1) * 512], False, False, [ONEI, rb], self.bv(lb + g))
                    f.mm(o_, MB[:, j * 128:(j + 1) * 128], I4[:], False, True, [MB, I4], self.bv(lb + g))
                    f.op("act", lambda e: e.activation(p_[:, g * 512:(g + 1) * 512], o_, AF.Exp),
                         reads=self.bv(lb + g), writes=[V(p_, g)])
                for h in range(8):
                    f.mm(ACC[:, h, 0:129], p_[:, h * 128:(h + 1) * 128], va[:], (j == 0 and h % 2 == 0), j == b,
                         [V(p_, h // 4), va], self.bv(4 + h // 2))
            f.op("dve", lambda e: e.reciprocal(rinv[:], ACC[:, :, 128]), reads=self.bv(4, 4), writes=[rinv])
            f.op("dve", lambda e: e.tensor_tensor(out=ctx[:], in0=ACC[:, :, 0:128],
                                                  in1=rinv[:].unsqueeze(2).to_broadcast([128, 8, 128]), op=ALU.mult),
                 reads=self.bv(4, 4) + [rinv], writes=[ctx])
            self.transposes(lambda c: ctx[:, c, :], 8, lambda c0, n: ctxT[:, c0:c0 + n, :], 0, [ctx], [ctxT])
            for h in range(8):
                f.mm(PS[:, 2 * 512 + h * 64: 2 * 512 + (h + 1) * 64], ctxT[:, h, :], WUV[:, h, :], True, True,
                     [ctxT, WUV], self.bv(2))
            y_ = yd[b % 2]
            f.op("act", lambda e: e.copy(y_[:], self.bank(2)), reads=self.bv(2), writes=[y_])
            f.dma("sp", self.MIX[t0:te, D:D + 512], y_[:], reads=[y_], writes=[V(self.MIX, ("dsa", b))])

    def phase_mem(self, l):
        f, NT = self.f, self.NT
        PS = self.PS
        WKm = f.sb("m_wk", [128, 8, 512]); WVm = f.sb("m_wv", [128, 8, 512])
        f.dma("sp", WKm[:], self.w_mem_k[l].rearrange("(c p) n -> p c n", p=128), writes=[WKm])
        f.dma("sp", WVm[:], self.w_mem_v[l].rearrange("(c p) n -> p c n", p=128), writes=[WVm])
        memKT = f.sb("m_kT", [128, 4, MEM_LEN]); memV = f.sb("m_v", [128, 2, 512])
        self.memT = f.sb("m_memT", [128, 8, MEM_LEN])
        mt_ = f.sb("m_mem", [128, D])
        for mi in range(MEM_LEN // 128):
            f.dma("sp", mt_[:], self.mem_in[mi * 128:(mi + 1) * 128, :], writes=[mt_])
            self.transposes(lambda c: mt_[:, c * 128:(c + 1) * 128], 8,
                            lambda c0, n: self.memT[:, c0:c0 + n, mi * 128:(mi + 1) * 128], 0, [mt_], [self.memT])
        for h in range(4):
            for kc in range(8):
                f.mm(PS[:, 0:MEM_LEN], WKm[:, kc, h * 128:(h + 1) * 128], self.memT[:, kc, :], kc == 0, kc == 7,
                     [WKm, self.memT], self.bv(0))
            f.op("act", lambda e: e.copy(memKT[:, h, :], PS[:, 0:MEM_LEN]), reads=self.bv(0), writes=[memKT])
        for mt in range(2):
            for kc in range(8):
                f.mm(self.bank(1), self.memT[:, kc, mt * 128:(mt + 1) * 128], WVm[:, kc, :], kc == 0, kc == 7,
                     [WVm, self.memT], self.bv(1))
            f.op("act", lambda e: e.copy(memV[:, mt, :], self.bank(1)), reads=self.bv(1), writes=[memV])
        QMt = [f.sb("m_q%d" % i, [128, 4, 128]) for i in range(2)]
        Pm = f.sb("m_P", [128, MEM_LEN]); PT = f.sb("m_PT", [128, 2, 128])
        sm = f.sb("m_sm", [128, 4]); ym = [f.sb("m_y%d" % i, [128, 512]) for i in range(2)]
        for i in range(NT):
            q_, y_ = QMt[i % 2], ym[i % 2]
            tok = slice(i * 128, (i + 1) * 128)
            f.dma("sp", q_[:], self.QM[:, :, tok], reads=[self.QM], writes=[q_])
            for h in range(4):
                bk = 2 + h % 2
                f.mm(PS[:, bk * 512: bk * 512 + MEM_LEN], q_[:, h, :], memKT[:, h, :], True, True, [q_, memKT], self.bv(bk))
                f.op("dve", lambda e: e.tensor_reduce(out=sm[:, 0:1], in_=PS[:, bk * 512: bk * 512 + MEM_LEN], axis=AX.X,
                                                      op=ALU.max), reads=self.bv(bk), writes=[V(sm, 0)])
                f.op("dve", lambda e: e.tensor_scalar(sm[:, 1:2], sm[:, 0:1], -1.0, None, op0=ALU.mult),
                     reads=[V(sm, 0)], writes=[V(sm, 1)])
                f.op("act", lambda e: e.activation(Pm[:], PS[:, bk * 512: bk * 512 + MEM_LEN], AF.Exp, bias=sm[:, 1:2],
                                                   scale=1.0, accum_out=sm[:, 2:3]),
                     reads=self.bv(bk) + [V(sm, 1)], writes=[Pm, V(sm, 2)])
                f.op("dve", lambda e: e.reciprocal(sm[:, 3:4], sm[:, 2:3]), reads=[V(sm, 2)], writes=[V(sm, 3)])
                self.transposes(lambda c: Pm[:, c * 128:(c + 1) * 128], 2, lambda c0, n: PT[:, c0:c0 + n, :], 4, [Pm], [PT])
                for mt in range(2):
                    f.mm(PS[:, 6 * 512: 6 * 512 + 128], PT[:, mt, :], memV[:, mt, h * 128:(h + 1) * 128], mt == 0, mt == 1,
                         [PT, memV], self.bv(6))
                f.op("dve", lambda e: e.tensor_scalar(y_[:, h * 128:(h + 1) * 128], PS[:, 6 * 512: 6 * 512 + 128],
                                                      sm[:, 3:4], None, op0=ALU.mult),
                     reads=self.bv(6) + [V(sm, 3)], writes=[y_])
            f.dma("sp", self.MIX[tok, D + 512:D + 1024], y_[:], reads=[y_], writes=[V(self.MIX, ("mem", i))])

    def phase_outp(self, l):
        f, NT, CAP = self.f, self.NT, self.CAP
        PS = self.PS
        WO = f.sb("o_WO", [128, 16, D])
        for kc in range(16):
            f.dma("sp", WO[:, kc, :], self.w_out[l, kc * 128:(kc + 1) * 128, :], writes=[V(WO, kc)])
        RW = f.sb("o_RW", [128, 8, NE])
        f.dma("sp", RW[:], self.router_w[l].rearrange("(c p) n -> p c n", p=128), writes=[RW])
        RBb = self.bcast_load("o_rb", self.router_b[l, :], NE)
        g1 = self.bcast_load("o_g1", self.ln1_g[l, :], D)
        b1 = self.bcast_load("o_b1", self.ln1_b[l, :], D)
        ECAP = f.sb("o_ecap", [128, NE])
        f.dma("sp", ECAP[:], self.cin["c_eidx"][:], writes=[ECAP])
        f.op("dve", lambda e: e.tensor_scalar(ECAP[:], ECAP[:], float(CAP), None, op0=ALU.mult), reads=[ECAP], writes=[ECAP])
        base = f.sb("o_base", [128, NE])
        f.op("dve", lambda e: e.memset(base[:], 0.0), writes=[base])
        if l == 0:
            zt = f.sb("o_zero", [128, D])
            f.op("dve", lambda e: e.memset(zt[:], 0.0), writes=[zt])
            NR = CAP // 128
            for e_ in range(NE):
                f.dma("sp", self.XD[e_ * CAP:(e_ + 1) * CAP, :].rearrange("(r p) d -> p r d", p=128),
                      zt[:].unsqueeze(1).to_broadcast([128, NR, D]), reads=[zt], writes=[V(self.XD, ("z", e_))])
            f.barrier()
        mixt = [f.sb("o_mix%d" % i, [128, DMIX]) for i in range(2)]
        ht = [f.sb("o_h%d" % i, [128, D]) for i in range(2)]
        mixT = f.sb("o_mixT", [128, 16, 128])
        r_ = f.sb("o_r", [128, D]); scr = f.sb("o_scr", [128, D])
        h2t = [f.sb("o_h2%d" % i, [128, D]) for i in range(2)]
        h2T = f.sb("o_h2T", [128, 8, 128])
        stats = f.sb("o_st", [128, 12]); mv = f.sb("o_mv", [128, 4])
        lg = f.sb("o_lg", [128, NE]); m8 = f.sb("o_m8", [128, 8]); sel = f.sb("o_sel", [128, NE])
        e4 = f.sb("o_e4", [128, 4]); sm = f.sb("o_sm", [128, 4])
        g4 = [f.sb("o_g4%d" % i, [128, 4]) for i in range(2)]
        slotf = f.sb("o_slotf", [128, NE]); tmp = f.sb("o_tmp", [128, NE]); eq = f.sb("o_eq", [128, NE])
        junk = f.sb("o_junk", [128, NE])
        s4f = f.sb("o_s4f", [128, 4])
        s4i = [f.sb("o_s4i%d" % i, [128, 4], I32) for i in range(2)]
        for i in range(NT):
            tok = slice(i * 128, (i + 1) * 128)
            mx, h_, h2_, g4_, s4_ = mixt[i % 2], ht[i % 2], h2t[i % 2], g4[i % 2], s4i[i % 2]
            f.dma("sp", mx[:], self.MIX[tok, :], reads=[self.MIX], writes=[mx])
            f.dma("sp", h_[:], self.H[tok, :], reads=[V(self.H, i)], writes=[h_])
            self.transposes(lambda c: mx[:, c * 128:(c + 1) * 128], 16, lambda c0, n: mixT[:, c0:c0 + n, :], 0, [mx], [mixT])
            for sl in range(2):
                for kc in range(16):
                    f.mm(self.bank(2 + sl), mixT[:, kc, :], WO[:, kc, sl * 512:(sl + 1) * 512], kc == 0, kc == 15,
                         [mixT, V(WO, kc)], self.bv(2 + sl))
                f.op("dve", lambda e: e.scalar_tensor_tensor(out=r_[:, sl * 512:(sl + 1) * 512], in0=h_[:, sl * 512:(sl + 1) * 512],
                                                             scalar=float(ALPHA), in1=self.bank(2 + sl), op0=ALU.mult, op1=ALU.add),
                     reads=[h_] + self.bv(2 + sl), writes=[r_])
            self.ln_tile(r_, h2_, g1, b1, scr, stats, mv)
            f.dma("sp", self.H2[tok, :], h2_[:], reads=[h2_], writes=[V(self.H2, i)])
            self.transposes(lambda c: h2_[:, c * 128:(c + 1) * 128], 8, lambda c0, n: h2T[:, c0:c0 + n, :], 0, [h2_], [h2T])
            for kc in range(8):
                f.mm(PS[:, 4 * 512: 4 * 512 + NE], h2T[:, kc, :], RW[:, kc, :], kc == 0, kc == 7, [h2T, RW], self.bv(4))
            f.op("dve", lambda e: e.tensor_tensor(out=lg[:], in0=PS[:, 4 * 512: 4 * 512 + NE], in1=RBb[:], op=ALU.add),
                 reads=self.bv(4) + [RBb], writes=[lg])
            f.op("dve", lambda e: e.max(out=m8[:], in_=lg[:]), reads=[lg], writes=[m8])
            f.op("dve", lambda e: e.tensor_scalar(sel[:], lg[:], m8[:, 3:4], None, op0=ALU.is_ge), reads=[lg, m8], writes=[sel])
            f.op("dve", lambda e: e.tensor_scalar(sm[:, 0:1], m8[:, 0:1], -1.0, None, op0=ALU.mult), reads=[m8], writes=[V(sm, 0)])
            f.op("act", lambda e: e.activation(e4[:], m8[:, 0:4], AF.Exp, bias=sm[:, 0:1], scale=1.0, accum_out=sm[:, 1:2]),
                 reads=[m8, V(sm, 0)], writes=[e4, V(sm, 1)])
            f.op("dve", lambda e: e.reciprocal(sm[:, 2:3], sm[:, 1:2]), reads=[V(sm, 1)], writes=[V(sm, 2)])
            f.op("dve", lambda e: e.tensor_scalar(g4_[:], e4[:], sm[:, 2:3], None, op0=ALU.mult), reads=[e4, V(sm, 2)], writes=[g4_])
            f.dma("sp", self.GATE4[tok, :], g4_[:], reads=[g4_], writes=[V(self.GATE4, i)])
            f.mm(PS[:, 5 * 512: 5 * 512 + NE], self.cSU[:], sel[:], True, True, [self.cSU, sel], self.bv(5))
            f.mm(PS[:, 6 * 512: 6 * 512 + NE], self.ones[:], sel[:], True, True, [self.ones, sel], self.bv(6))
            f.op("dve", lambda e: e.tensor_tensor(out=tmp[:], in0=PS[:, 5 * 512: 5 * 512 + NE], in1=base[:], op=ALU.add),
                 reads=self.bv(5) + [base], writes=[tmp])
            f.op("dve", lambda e: e.tensor_tensor(out=slotf[:], in0=tmp[:], in1=ECAP[:], op=ALU.add), reads=[tmp, ECAP], writes=[slotf])
            f.op("dve", lambda e: e.tensor_scalar(tmp[:], tmp[:], float(CAP) - 0.5, 1.0e9, op0=ALU.is_ge, op1=ALU.mult),
                 reads=[tmp], writes=[tmp])
            f.op("dve", lambda e: e.tensor_tensor(out=slotf[:], in0=slotf[:], in1=tmp[:], op=ALU.add), reads=[slotf, tmp], writes=[slotf])
            f.op("dve", lambda e: e.tensor_tensor(out=base[:], in0=base[:], in1=PS[:, 6 * 512: 6 * 512 + NE], op=ALU.add),
                 reads=[base] + self.bv(6), writes=[base])
            for k in range(4):
                f.op("dve", lambda e: e.scalar_tensor_tensor(out=junk[:], in0=lg[:], scalar=m8[:, k:k + 1], in1=slotf[:],
                                                             op0=ALU.is_equal, op1=ALU.mult, accum_out=s4f[:, k:k + 1]),
                     reads=[lg, m8, slotf], writes=[junk, V(s4f, k)])
            f.op("dve", lambda e: e.tensor_copy(s4_[:], s4f[:]), reads=[s4f], writes=[s4_])
            f.dma("sp", self.SLOT4[tok, :], s4_[:], reads=[s4_], writes=[V(self.SLOT4, i)])
            for k in range(4):
                f.dma("pool", self.XD[:, :], h2_[:, :], reads=[h2_, s4_], writes=[V(self.XD, ("s", i, k))],
                      indirect=dict(out_offset=bass.IndirectOffsetOnAxis(ap=s4_[:, k:k + 1], axis=0), in_offset=None,
                                    bounds_check=self.bc_reg(), oob_is_err=False))

    def phase_moe(self, l):
        f, CAP = self.f, self.CAP
        PS = self.PS
        NR = CAP // 128
        BGall = f.sb("e_bgall", [128, 16, NE])
        with ExitStack() as tmpst:
            old_st = f.stack
            f.stack = tmpst
            bgl = f.sb("e_bgl", [NE, 2 * DFF])
            f.dma("sp", bgl[:], self.b_gu[l], writes=[bgl])
            for c in range(16):
                bk = c % 2
                f.tr(PS[0:128, bk * 512: bk * 512 + NE], bgl[:, c * 128:(c + 1) * 128], self.ident[0:NE, 0:NE],
                     [bgl, self.ident], self.bv(bk))
                f.op("act", lambda e: e.copy(BGall[:, c, :], PS[:, bk * 512: bk * 512 + NE]), reads=self.bv(bk), writes=[V(BGall, c)])
            f.barrier()
            f.stack = old_st
        XT = f.sb("e_XT", [128, 8, CAP]); AT = f.sb("e_AT", [128, 8, CAP])
        WD = [f.sb("e_WD%d" % i, [128, 8, D]) for i in range(2)]
        WG = [f.sb("e_WG%d" % i, [128, 8, 256]) for i in range(2)]
        xr = [f.sb("e_xr%d" % i, [128, D]) for i in range(2)]
        yr = [f.sb("e_yr%d" % i, [128, D]) for i in range(2)]
        BD = [f.sb("e_BD%d" % i, [128, D]) for i in range(2)]
        tg = f.sb("e_tg", [128, 512]); tsg = f.sb("e_tsg", [128, 512]); tl = f.sb("e_tl", [128, 512])
        slabs = [(s0, min(512, CAP - s0)) for s0 in range(0, CAP, 512)]
        nwg = 0
        nps = 0
        nrow = 0
        for e_ in range(NE):
            wd, bd = WD[e_ % 2], BD[e_ % 2]
            f.dma("sp", wd[:], self.w_down[l, e_].rearrange("(c p) n -> p c n", p=128), writes=[wd])
            f.dma("sp", bd[:], self.b_down[l, e_, :].partition_broadcast(128), writes=[bd])
            for r in range(NR):
                x_ = xr[nrow % 2]
                nrow += 1
                f.dma("sp", x_[:], self.XD[e_ * CAP + r * 128: e_ * CAP + (r + 1) * 128, :], reads=[self.XD], writes=[x_])
                self.transposes(lambda c: x_[:, c * 128:(c + 1) * 128], 8, lambda c0, n: XT[:, c0:c0 + n, r * 128:(r + 1) * 128],
                                0, [x_], [V(XT, r)])
            for j in range(8):
                wg = WG[nwg % 2]
                nwg += 1
                f.dma("sp", wg[:, :, 0:128], self.w_gu[l, e_, :, j * 128:(j + 1) * 128].rearrange("(c p) n -> p c n", p=128),
                      writes=[V(wg, 0)])
                f.dma("sp", wg[:, :, 128:256],
                      self.w_gu[l, e_, :, DFF + j * 128: DFF + (j + 1) * 128].rearrange("(c p) n -> p c n", p=128),
                      writes=[V(wg, 1)])
                for (s0, n) in slabs:
                    bg_, bl_ = 2 + (nps % 2) * 2, 3 + (nps % 2) * 2
                    nps += 1
                    for kc in range(8):
                        f.mm(PS[:, bg_ * 512: bg_ * 512 + n], wg[:, kc, 0:128], XT[:, kc, s0:s0 + n], kc == 0, kc == 7,
                             [V(wg, 0), XT], self.bv(bg_))
                    for kc in range(8):
                        f.mm(PS[:, bl_ * 512: bl_ * 512 + n], wg[:, kc, 128:256], XT[:, kc, s0:s0 + n], kc == 0, kc == 7,
                             [V(wg, 1), XT], self.bv(bl_))
                    f.op("dve", lambda e: e.tensor_scalar(tg[:, 0:n], PS[:, bg_ * 512: bg_ * 512 + n], BGall[:, j, e_:e_ + 1], LIM,
                                                          op0=ALU.add, op1=ALU.min), reads=self.bv(bg_) + [BGall], writes=[tg])
                    f.op("act", lambda e: e.activation(tsg[:, 0:n], tg[:, 0:n], AF.Sigmoid, scale=SW_ALPHA), reads=[tg], writes=[tsg])
                    f.op("dve", lambda e: e.tensor_scalar(tl[:, 0:n], PS[:, bl_ * 512: bl_ * 512 + n], BGall[:, 8 + j, e_:e_ + 1], LIM,
                                                          op0=ALU.add, op1=ALU.min), reads=self.bv(bl_) + [BGall], writes=[tl])
                    f.op("pool", lambda e: e.tensor_scalar(tl[:, 0:n], tl[:, 0:n], -LIM, 1.0, op0=ALU.max, op1=ALU.add),
                         reads=[tl], writes=[tl])
                    f.op("pool", lambda e: e.tensor_tensor(out=tg[:, 0:n], in0=tg[:, 0:n], in1=tsg[:, 0:n], op=ALU.mult),
                         reads=[tg, tsg], writes=[tg])
                    f.op("dve", lambda e: e.tensor_tensor(out=AT[:, j, s0:s0 + n], in0=tg[:, 0:n], in1=tl[:, 0:n], op=ALU.mult),
                         reads=[tg, tl], writes=[V(AT, j)])
            for r in range(NR):
                y_ = yr[r % 2]
                for sl in range(2):
                    for fc in range(8):
                        f.mm(self.bank(6 + sl), AT[:, fc, r * 128:(r + 1) * 128], wd[:, fc, sl * 512:(sl + 1) * 512],
                             fc == 0, fc == 7, [AT, wd], self.bv(6 + sl))
                    f.op("dve", lambda e: e.tensor_tensor(out=y_[:, sl * 512:(sl + 1) * 512], in0=self.bank(6 + sl),
                                                          in1=bd[:, sl * 512:(sl + 1) * 512], op=ALU.add),
                         reads=self.bv(6 + sl) + [bd], writes=[y_])
                f.dma("sp", self.YD[e_ * CAP + r * 128: e_ * CAP + (r + 1) * 128, :], y_[:], reads=[y_],
                      writes=[V(self.YD, (e_, r))])

    def phase_comb(self, l, last):
        f, NT, CAP = self.f, self.NT, self.CAP
        g2 = self.bcast_load("c_g2", self.ln2_g[l, :], D)
        b2 = self.bcast_load("c_b2", self.ln2_b[l, :], D)
        h2t = [f.sb("c_h2%d" % i, [128, D]) for i in range(2)]
        g4 = [f.sb("c_g4%d" % i, [128, 4]) for i in range(2)]
        s4 = [f.sb("c_s4%d" % i, [128, 4], I32) for i in range(2)]
        yk = [f.sb("c_yk%d" % i, [128, D]) for i in range(4)]
        acc = f.sb("c_acc", [128, D]); scr = f.sb("c_scr", [128, D])
        ot = [f.sb("c_o%d" % i, [128, D]) for i in range(2)]
        stats = f.sb("c_st", [128, 12]); mv = f.sb("c_mv", [128, 4])
        dst = self.out if last else self.H
        for i in range(NT):
            tok = slice(i * 128, (i + 1) * 128)
            h2_, g4_, s4_, o_ = h2t[i % 2], g4[i % 2], s4[i % 2], ot[i % 2]
            f.dma("sp", h2_[:], self.H2[tok, :], reads=[V(self.H2, i)], writes=[h2_])
            f.dma("sp", g4_[:], self.GATE4[tok, :], reads=[V(self.GATE4, i)], writes=[g4_])
            f.dma("sp", s4_[:], self.SLOT4[tok, :], reads=[V(self.SLOT4, i)], writes=[s4_])
            for k in range(4):
                y_ = yk[k]
                f.dma("pool", y_[:, :], self.YD[:, :], reads=[self.YD, s4_], writes=[y_],
                      indirect=dict(out_offset=None, in_offset=bass.IndirectOffsetOnAxis(ap=s4_[:, k:k + 1], axis=0),
                                    bounds_check=self.bc_reg(), oob_is_err=False))
                if k == 0:
                    f.op("dve", lambda e: e.tensor_scalar(acc[:], y_[:], g4_[:, 0:1], None, op0=ALU.mult),
                         reads=[y_, g4_], writes=[acc])
                else:
                    f.op("dve", lambda e: e.scalar_tensor_tensor(out=acc[:], in0=y_[:], scalar=g4_[:, k:k + 1], in1=acc[:],
                                                                 op0=ALU.mult, op1=ALU.add), reads=[y_, g4_, acc], writes=[acc])
            f.op("dve", lambda e: e.scalar_tensor_tensor(out=acc[:], in0=h2_[:], scalar=float(ALPHA), in1=acc[:],
                                                         op0=ALU.mult, op1=ALU.add), reads=[h2_, acc], writes=[acc])
            self.ln_tile(acc, o_, g2, b2, scr, stats, mv)
            f.dma("sp", dst[tok, :], o_[:], reads=[o_], writes=[V(dst, i)])


_WNAMES = ["w_in", "conv_w", "conv_b", "dt_bias", "a_log", "d_skip", "ssd_norm_g", "kv_norm_g", "w_uv",
           "w_mem_k", "w_mem_v", "w_out", "ln1_g", "ln1_b", "router_w", "router_b", "w_gu", "b_gu",
           "w_down", "b_down", "ln2_g", "ln2_b"]


def core_inputs(inp, b, S, L, consts=None):
    feed = {"x": np.ascontiguousarray(np.asarray(inp["x"])[b, :S]), "mem": np.ascontiguousarray(np.asarray(inp["mem"])[b]),
            "ln_in_g": np.asarray(inp["ln_in_g"]).reshape(1, D), "ln_in_b": np.asarray(inp["ln_in_b"]).reshape(1, D)}
    for k in _WNAMES:
        feed[k] = np.ascontiguousarray(np.asarray(inp[k])[:L])
    feed.update(consts if consts is not None else make_consts(S))
    return feed


SEQ = 8192
BATCH = 4
N_CORES = 8
CFG = dict(S=SEQ, depth=DEPTH_FULL, TS=512, CAP=1280, NIT=34, NSEL=256)


def kernel(**inputs):
    inputs = {k: np.asarray(v) for k, v in inputs.items()}
    mk = MK(CFG["S"], CFG["depth"], CFG["TS"], CFG["CAP"], CFG["NIT"], CFG["NSEL"])
    nc = mk.build()
    consts = make_consts(SEQ)
    shared = {k: np.ascontiguousarray(inputs[k]) for k in _WNAMES}
    shared["ln_in_g"] = inputs["ln_in_g"].reshape(1, D)
    shared["ln_in_b"] = inputs["ln_in_b"].reshape(1, D)
    shared.update(consts)
    in_maps = []
    for c in range(N_CORES):
        b = c % BATCH
        m = dict(shared)
        m["x"] = np.ascontiguousarray(inputs["x"][b])
        m["mem"] = np.ascontiguousarray(inputs["mem"][b])
        in_maps.append(m)
    res = run_bass_kernel_spmd(nc, in_maps, core_ids=list(range(N_CORES)))
    out = np.stack([np.asarray(res.results[b]["out"]) for b in range(BATCH)], axis=0)
    return out.astype(np.float32)
```

```python
import math
import numpy as np
from contextlib import ExitStack
import concourse.bass as bass
import concourse.mybir as mybir
from concourse.bass_utils import run_bass_kernel_spmd

F32 = mybir.dt.float32
BF16 = mybir.dt.bfloat16
I32 = mybir.dt.int32
U32 = mybir.dt.uint32
AF = mybir.ActivationFunctionType
ALU = mybir.AluOpType
AX = mybir.AxisListType

NDS = 24
NDS_SW = 8


class Buf:
    def __init__(self, t, name):
        self.t = t
        self.name = name
        self.w = {}
        self.r = {}

    def __getitem__(self, idx):
        return self.t[idx]


class V:
    def __init__(self, buf, tag=None):
        self.buf = buf
        self.tag = tag


def _norm(x):
    if isinstance(x, V):
        return x.buf, x.tag
    return x, None


class FW:
    def __init__(self, nc, stack, same_engine_sync=True):
        self.nc = nc
        self.stack = stack
        self.eng = {"pe": nc.tensor, "dve": nc.vector, "act": nc.scalar,
                    "pool": nc.gpsimd, "sp": nc.sync}
        self.sem = {k: stack.enter_context(nc.semaphore("s_" + k)) for k in self.eng}
        self.cnt = {k: 0 for k in self.eng}
        self.waited = {k: {} for k in self.eng}
        self.dsem = [stack.enter_context(nc.semaphore("d%d" % i)) for i in range(NDS)]
        self.dcnt = 0
        self.dsem_sw = [stack.enter_context(nc.semaphore("w%d" % i)) for i in range(NDS_SW)]
        self.dcnt_sw = 0
        self.ses = same_engine_sync
        self.ninstr = 0

    def sb(self, name, shape, dt=F32):
        self.nalloc = getattr(self, "nalloc", 0) + 1
        name = "%s_%d" % (name, self.nalloc)
        return Buf(self.stack.enter_context(self.nc.sbuf_tensor(name, list(shape), dt)), name)

    def ps(self, name, shape, dt=F32):
        return Buf(self.stack.enter_context(self.nc.psum_tensor(name, list(shape), dt)), name)

    def dram(self, name, shape, dt=F32, kind="Internal"):
        return Buf(self.nc.dram_tensor(name, list(shape), dt, kind=kind).ap(), name)

    def _deps(self, reads, writes):
        deps = []
        for x in reads:
            b, tag = _norm(x)
            for tg, tok in b.w.items():
                if tag is None or tg is None or tg == tag:
                    deps.append(tok)
        for x in writes:
            b, tag = _norm(x)
            for tg, tok in b.w.items():
                if tag is None or tg is None or tg == tag:
                    deps.append(tok)
            for tg, toks in b.r.items():
                if tag is None or tg is None or tg == tag:
                    deps.extend(toks)
        return deps

    def _wait(self, ek, deps, skip_same=False):
        e = self.eng[ek]
        need = {}
        for (sem, val, src) in deps:
            if src == ek and (skip_same or not self.ses):
                continue
            key = id(sem)
            if self.waited[ek].get(key, 0) >= val:
                continue
            if key not in need or need[key][1] < val:
                need[key] = (sem, val)
        for key, (sem, val) in need.items():
            e.wait_ge(sem, val)
            self.waited[ek][key] = val
            self.ninstr += 1

    def _record(self, tok, reads, writes):
        for x in reads:
            b, tag = _norm(x)
            lst = b.r.setdefault(tag, [])
            lst[:] = [t for t in lst if t[0] is not tok[0]] + [tok]
        for x in writes:
            b, tag = _norm(x)
            if tag is None:
                b.w = {None: tok}
                b.r = {}
            else:
                b.w[tag] = tok
                b.r[tag] = []

    def op(self, ek, fn, reads=(), writes=(), skip_same=False):
        self._wait(ek, self._deps(reads, writes), skip_same=skip_same)
        ins = fn(self.eng[ek])
        self.cnt[ek] += 1
        ins.then_inc(self.sem[ek], 1)
        tok = (self.sem[ek], self.cnt[ek], ek)
        self._record(tok, reads, writes)
        self.ninstr += 1
        return tok

    def dma(self, qk, out, in_, reads=(), writes=(), indirect=None, **kw):
        self._wait(qk, self._deps(reads, writes))
        if qk == "pool":
            i = self.dcnt_sw % NDS_SW
            rnd = self.dcnt_sw // NDS_SW
            self.dcnt_sw += 1
            sem = self.dsem_sw[i]
        else:
            i = self.dcnt % NDS
            rnd = self.dcnt // NDS
            self.dcnt += 1
            sem = self.dsem[i]
        if rnd > 0:
            key = id(sem)
            if self.waited[qk].get(key, 0) < 16 * rnd:
                self.eng[qk].wait_ge(sem, 16 * rnd)
                self.waited[qk][key] = 16 * rnd
        if indirect is None:
            self.eng[qk].dma_start(out=out, in_=in_, **kw).then_inc(sem, 16)
        else:
            self.eng[qk].indirect_dma_start(out=out, in_=in_, **indirect).then_inc(sem, 16)
        tok = (sem, 16 * (rnd + 1), "dma")
        self._record(tok, reads, writes)
        self.ninstr += 1
        return tok

    def barrier(self):
        toks = [(self.sem[k], self.cnt[k], k) for k in self.eng if self.cnt[k] > 0]
        for i in range(NDS):
            n = (self.dcnt - i + NDS - 1) // NDS
            if n > 0:
                toks.append((self.dsem[i], 16 * n, "dma"))
        for i in range(NDS_SW):
            n = (self.dcnt_sw - i + NDS_SW - 1) // NDS_SW
            if n > 0:
                toks.append((self.dsem_sw[i], 16 * n, "dma"))
        for ek in self.eng:
            self._wait(ek, [t for t in toks if t[2] != ek], skip_same=True)

    def mm(self, out, lhsT, rhs, start, stop, reads, writes):
        return self.op("pe", lambda e: e.matmul(out, lhsT, rhs, start=start, stop=stop,
                                                skip_group_check=True),
                       reads=reads, writes=writes, skip_same=True)

    def tr(self, out, in_, ident, reads, writes):
        return self.op("pe", lambda e: e.transpose(out, in_, ident), reads=reads, writes=writes,
                       skip_same=True)


D = 1024
DEPTH_FULL = 4
NH = 16
HP = 64
NG = 2
DST = 128
CONVW = 4
CONVD = D + 2 * NG * DST
DSA_H = 8
DLAT = 128
IDX_H = 4
IDX_D = 64
MEM_LEN = 256
MEM_H = 4
MEM_D = 128
DMIX = 2048
NE = 32
TOPK = 4
DFF = 1024
LIM = 7.0
SW_ALPHA = 1.702
ALPHA = (2 * DEPTH_FULL) ** 0.25
LN_EPS = 1e-5
RMS_EPS = 1e-6
O_Z, O_XBC, O_DT, O_QL, O_CKV, O_QI, O_KI, O_WI, O_QM, N_IN = (
    0, 1024, 2560, 2576, 3600, 3728, 3984, 4048, 4052, 4564)
NEG = -1.0e30
EPS_TIE = 2.0 ** -30


def make_consts(S):
    c = {}
    c["c_ident"] = np.eye(128, dtype=np.float32)
    k = np.arange(128)
    c["c_U"] = (k[:, None] <= k[None, :]).astype(np.float32)
    c["c_SL"] = (k[:, None] > k[None, :]).astype(np.float32)
    c["c_SU"] = (k[:, None] < k[None, :]).astype(np.float32)
    c["c_ones"] = np.ones((128, 128), np.float32)
    c["c_cbm"] = np.where(k[None, :] <= k[:, None], 0.0, NEG).astype(np.float32)
    c["c_epspos"] = np.broadcast_to((-EPS_TIE * np.arange(S, dtype=np.float64)).astype(np.float32)[None, :],
                                    (128, S)).copy()
    import ml_dtypes
    c["c_i4"] = np.tile(np.eye(128, dtype=np.float32), (1, 4)).astype(ml_dtypes.bfloat16)
    sl = (2.0 ** (-8.0 * np.arange(1, DSA_H + 1) / DSA_H)).astype(np.float32)
    c["c_sl128"] = np.repeat(sl * 128.0, 128)[None, :].astype(np.float32)
    onei = np.zeros((65, 128), np.float32)
    onei[0] = 1.0
    onei[32] = 1.0
    onei[64] = np.arange(128)
    c["c_onei"] = onei.astype(ml_dtypes.bfloat16)
    rbinit = np.zeros((65, 1024), np.float32)
    rbinit[64] = np.repeat(sl, 128)
    c["c_rbinit"] = rbinit.astype(ml_dtypes.bfloat16)
    c["c_negsl33"] = np.broadcast_to(np.repeat(-sl, 128)[None, :], (33, 1024)).astype(np.float32).copy()
    c["c_slrow"] = np.repeat(sl, 128)[None, :].astype(np.float32)
    c["c_pow2"] = np.broadcast_to((2.0 ** -(np.arange(64) + 1.0)).astype(np.float32)[None, :], (128, 64)).copy()
    c["c_eidx"] = np.broadcast_to(np.arange(NE, dtype=np.float32)[None, :], (128, NE)).copy()
    return c


class MK:
    def __init__(self, S, depth, TS, CAP, NIT, NSEL, stop_after=None):
        self.S, self.L, self.TS, self.CAP, self.NIT, self.NSEL = S, depth, TS, CAP, NIT, NSEL
        self.NT = S // 128
        self.NU = S // TS
        self.TPU = TS // 128
        self.stop_after = stop_after
        self.nc = bass.Bass("TRN2", target_bir_lowering=False)

    def din(self, name, shape, dt=F32):
        return Buf(self.nc.dram_tensor(name, list(shape), dt, kind="ExternalInput").ap(), name)

    def build(self):
        nc, S, L, NT, CAP = self.nc, self.S, self.L, self.NT, self.CAP
        din = self.din
        self.x_in = din("x", [S, D]); self.mem_in = din("mem", [MEM_LEN, D])
        self.ln_in_g = din("ln_in_g", [1, D]); self.ln_in_b = din("ln_in_b", [1, D])
        self.w_in = din("w_in", [L, D, N_IN])
        self.conv_w = din("conv_w", [L, CONVW, CONVD]); self.conv_b = din("conv_b", [L, CONVD])
        self.dt_bias = din("dt_bias", [L, NH]); self.a_log = din("a_log", [L, NH]); self.d_skip = din("d_skip", [L, NH])
        self.ssd_norm_g = din("ssd_norm_g", [L, D]); self.kv_norm_g = din("kv_norm_g", [L, DLAT])
        self.w_uv = din("w_uv", [L, DSA_H, DLAT, 64])
        self.w_mem_k = din("w_mem_k", [L, D, 512]); self.w_mem_v = din("w_mem_v", [L, D, 512])
        self.w_out = din("w_out", [L, DMIX, D])
        self.ln1_g = din("ln1_g", [L, D]); self.ln1_b = din("ln1_b", [L, D])
        self.router_w = din("router_w", [L, D, NE]); self.router_b = din("router_b", [L, NE])
        self.w_gu = din("w_gu", [L, NE, D, 2 * DFF]); self.b_gu = din("b_gu", [L, NE, 2 * DFF])
        self.w_down = din("w_down", [L, NE, DFF, D]); self.b_down = din("b_down", [L, NE, D])
        self.ln2_g = din("ln2_g", [L, D]); self.ln2_b = din("ln2_b", [L, D])
        self.cin = {}
        for k, v in make_consts(S).items():
            self.cin[k] = din(k, list(v.shape), BF16 if v.dtype.itemsize == 2 else F32)
        self.out = Buf(nc.dram_tensor("out", [S, D], F32, kind="ExternalOutput").ap(), "out")

        with ExitStack() as st:
            f = self.f = FW(nc, st)
            self.H = f.dram("H", [S, D]); self.H2 = f.dram("H2", [S, D])
            self.Z = f.dram("Z", [S, D]); self.DTs = f.dram("DTs", [S, NH])
            self.X = f.dram("X", [S, D]); self.BTM = f.dram("BTM", [S, 256])
            self.BT = f.dram("BT", [128, 2, S]); self.CTs = f.dram("CTs", [128, 2, S])
            self.QL = f.dram("QL", [128, NT, 1024], BF16); self.QI = f.dram("QI", [128, 2, S])
            self.KI = f.dram("KI", [128, S]); self.WI = f.dram("WI", [S, 4])
            self.CV = f.dram("CV", [S, 129], BF16); self.CT = f.dram("CT", [128, S], BF16)
            self.QM = f.dram("QM", [128, 4, S])
            self.MIX = f.dram("MIX", [S, DMIX])
            self.GATE4 = f.dram("GATE4", [S, 4]); self.SLOT4 = f.dram("SLOT4", [S, 4], I32)
            self.XD = f.dram("XD", [NE * CAP, D]); self.YD = f.dram("YD", [NE * CAP, D])

            self.ident = f.sb("ident", [128, 128]); self.cU = f.sb("cU", [128, 128])
            self.cSL = f.sb("cSL", [128, 128]); self.cSU = f.sb("cSU", [128, 128])
            self.ones = f.sb("ones", [128, 128])
            for sbt, nm in ((self.ident, "c_ident"), (self.cU, "c_U"), (self.cSL, "c_SL"),
                            (self.cSU, "c_SU"), (self.ones, "c_ones")):
                f.dma("sp", sbt[:], self.cin[nm][:], writes=[sbt])
            self.epsln = f.sb("epsln", [128, 4])
            f.op("dve", lambda e: e.memset(self.epsln[:, 0:1], LN_EPS), writes=[self.epsln])
            f.op("dve", lambda e: e.memset(self.epsln[:, 1:2], RMS_EPS), reads=[self.epsln], writes=[self.epsln])
            f.op("dve", lambda e: e.memset(self.epsln[:, 2:3], 1.0), reads=[self.epsln], writes=[self.epsln])
            self.PS = f.ps("PS", [128, 4096])

            stages = [("ln_in", lambda: self.phase_ln_in())]
            for l in range(L):
                stages += [("a1_%d" % l, lambda l=l: self.phase_a1(l)),
                           ("ssd_%d" % l, lambda l=l: self.phase_ssd(l)),
                           ("a2_%d" % l, lambda l=l: self.phase_a2(l)),
                           ("dsa_%d" % l, lambda l=l: self.phase_dsa(l)),
                           ("mem_%d" % l, lambda l=l: self.phase_mem(l)),
                           ("outp_%d" % l, lambda l=l: self.phase_outp(l)),
                           ("moe_%d" % l, lambda l=l: self.phase_moe(l)),
                           ("comb_%d" % l, lambda l=l: self.phase_comb(l, last=(l == L - 1)))]
            for name, fn in stages:
                with ExitStack() as ph:
                    old = f.stack
                    f.stack = ph
                    fn()
                    f.barrier()
                    f.stack = old
                if self.stop_after == name:
                    break
        return nc

    def bc_reg(self):
        if getattr(self, "_bc_reg", None) is None:
            self._bc_reg = self.nc.gpsimd.to_reg(NE * self.CAP - 1)
        return self._bc_reg

    def bank(self, i, n=1):
        return self.PS[:, i * 512:(i + n) * 512]

    def bv(self, i, n=1):
        return [V(self.PS, j) for j in range(i, i + n)]

    def ln_tile(self, src, dst, g_bc, b_bc, scr, stats, mv):
        f, epsln = self.f, self.epsln
        f.op("dve", lambda e: e.bn_stats(stats[:, 0:6], src[:, 0:512]), reads=[src], writes=[stats])
        f.op("dve", lambda e: e.bn_stats(stats[:, 6:12], src[:, 512:1024]), reads=[src, stats], writes=[stats])
        f.op("dve", lambda e: e.bn_aggr(mv[:, 0:2], stats[:, 0:12]), reads=[stats], writes=[mv])
        f.op("act", lambda e: e.activation(mv[:, 2:3], mv[:, 1:2], AF.Sqrt, bias=epsln[:, 0:1], scale=1.0),
             reads=[mv, epsln], writes=[mv])
        f.op("dve", lambda e: e.reciprocal(mv[:, 3:4], mv[:, 2:3]), reads=[mv], writes=[mv])
        f.op("dve", lambda e: e.tensor_scalar(scr[:], src[:], mv[:, 0:1], mv[:, 3:4],
                                              op0=ALU.subtract, op1=ALU.mult), reads=[src, mv], writes=[scr])
        f.op("dve", lambda e: e.tensor_tensor(out=scr[:], in0=scr[:], in1=g_bc[:], op=ALU.mult),
             reads=[scr, g_bc], writes=[scr])
        f.op("dve", lambda e: e.tensor_tensor(out=dst[:], in0=scr[:], in1=b_bc[:], op=ALU.add),
             reads=[scr, b_bc], writes=[dst])

    def transposes(self, src_fn, nchunks, dst_fn, pbank, rd, wr, evac="act", scale=None):
        f, PS = self.f, self.PS
        for gi, c0 in enumerate(range(0, nchunks, 4)):
            n = min(4, nchunks - c0)
            bk = pbank + gi % 2
            for c in range(c0, c0 + n):
                f.tr(PS[:, bk * 512 + (c - c0) * 128: bk * 512 + (c - c0 + 1) * 128],
                     src_fn(c), self.ident[:], reads=list(rd) + [self.ident], writes=self.bv(bk))
            dst = dst_fn(c0, n)
            srcp = PS[:, bk * 512: bk * 512 + n * 128].rearrange("p (c t) -> p c t", t=128)
            if scale is not None:
                f.op("act", lambda e: e.activation(dst, srcp, AF.Copy, scale=scale), reads=self.bv(bk), writes=wr)
            elif evac == "act":
                f.op("act", lambda e: e.copy(dst, srcp), reads=self.bv(bk), writes=wr)
            else:
                f.op("dve", lambda e: e.tensor_copy(dst, srcp), reads=self.bv(bk), writes=wr)

    def bcast_load(self, name, src_row_ap, n):
        t = self.f.sb(name, [128, n])
        self.f.dma("sp", t[:], src_row_ap.partition_broadcast(128), writes=[t])
        return t

    def phase_ln_in(self):
        f, NT = self.f, self.NT
        g_bc = self.bcast_load("p0_g", self.ln_in_g[0, :], D)
        b_bc = self.bcast_load("p0_b", self.ln_in_b[0, :], D)
        xt = [f.sb("p0_x%d" % i, [128, D]) for i in range(2)]
        ot = [f.sb("p0_o%d" % i, [128, D]) for i in range(2)]
        scr = f.sb("p0_scr", [128, D]); stats = f.sb("p0_st", [128, 12]); mv = f.sb("p0_mv", [128, 4])
        for i in range(NT):
            a, o = xt[i % 2], ot[i % 2]
            f.dma("sp", a[:], self.x_in[i * 128:(i + 1) * 128, :], writes=[a])
            self.ln_tile(a, o, g_bc, b_bc, scr, stats, mv)
            f.dma("sp", self.H[i * 128:(i + 1) * 128, :], o[:], reads=[o], writes=[V(self.H, i)])

    def load_hT(self, hT, u, htiles, hTb=None):
        f = self.f
        for i in range(self.TPU):
            ti = u * self.TPU + i
            a = htiles[ti % 2]
            f.dma("sp", a[:], self.H[ti * 128:(ti + 1) * 128, :], reads=[V(self.H, ti)], writes=[a])
            self.transposes(lambda c: a[:, c * 128:(c + 1) * 128], 8,
                            lambda c0, n: hT[:, c0:c0 + n, i * 128:(i + 1) * 128], 0, [a], [hT])
            if hTb is not None:
                f.op("pool", lambda e: e.tensor_copy(hTb[:, :, i * 128:(i + 1) * 128], hT[:, :, i * 128:(i + 1) * 128]),
                     reads=[hT], writes=[hTb])

    def phase_a1(self, l):
        f, TS, TPU, NU = self.f, self.TS, self.TPU, self.NU
        NW = O_QL
        W1 = f.sb("a1_W", [128, 8, NW], BF16)
        W1s = [f.sb("a1_Ws%d" % i, [128, NW]) for i in range(2)]
        for kc in range(8):
            ws = W1s[kc % 2]
            f.dma("sp", ws[:], self.w_in[l, kc * 128:(kc + 1) * 128, 0:NW], writes=[ws])
            f.op("pool", lambda e: e.tensor_copy(W1[:, kc, :], ws[:]), reads=[ws], writes=[V(W1, kc)])
        CW = f.sb("a1_cw", [128, 12, 4]); CBs = f.sb("a1_cb", [128, 12])
        for j in range(4):
            f.dma("sp", CW[:, :, j], self.conv_w[l, j, :].rearrange("(c p) -> p c", p=128), writes=[V(CW, j)],
                  allow_slow_non_contiguous=True)
        f.dma("sp", CBs[:], self.conv_b[l, :].rearrange("(c p) -> p c", p=128), writes=[CBs],
              allow_slow_non_contiguous=True)
        dtb = self.bcast_load("a1_dtb", self.dt_bias[l, :], NH)
        hT = f.sb("a1_hT", [128, 8, TS], BF16)
        htiles = [f.sb("a1_h%d" % i, [128, D]) for i in range(2)]
        xpre = f.sb("a1_xpre", [128, 12, TS + 3])
        xc = f.sb("a1_xc", [128, 12, TS])
        acc = f.sb("a1_acc", [128, TS])
        zt = [f.sb("a1_z%d" % i, [128, D]) for i in range(2)]
        dtt = f.sb("a1_dt", [128, NH]); dte = f.sb("a1_dte", [128, NH])
        xtm = [f.sb("a1_xtm%d" % i, [128, D + 256]) for i in range(2)]
        f.op("dve", lambda e: e.memset(xpre[:, :, 0:3], 0.0), writes=[xpre])
        for u in range(NU):
            self.load_hT(hT, u, htiles)
            for i in range(TPU):
                ti = u * TPU + i
                z = zt[ti % 2]
                for sl in range(2):
                    bk = 2 + sl
                    for kc in range(8):
                        f.mm(self.bank(bk), hT[:, kc, i * 128:(i + 1) * 128], W1[:, kc, sl * 512:(sl + 1) * 512],
                             kc == 0, kc == 7, [hT, V(W1, kc)], self.bv(bk))
                    f.op("act", lambda e: e.copy(z[:, sl * 512:(sl + 1) * 512], self.bank(bk)),
                         reads=self.bv(bk), writes=[z])
                f.dma("sp", self.Z[ti * 128:(ti + 1) * 128, :], z[:], reads=[z], writes=[V(self.Z, ti)])
                for kc in range(8):
                    f.mm(self.PS[:, 4 * 512:4 * 512 + NH], hT[:, kc, i * 128:(i + 1) * 128],
                         W1[:, kc, O_DT:O_DT + NH], kc == 0, kc == 7, [hT, V(W1, kc)], self.bv(4))
                f.op("dve", lambda e: e.tensor_tensor(out=dte[:], in0=self.PS[:, 4 * 512:4 * 512 + NH], in1=dtb[:],
                                                      op=ALU.add), reads=self.bv(4) + [dtb], writes=[dte])
                f.op("act", lambda e: e.activation(dte[:], dte[:], AF.Exp), reads=[dte], writes=[dte])
                f.op("act", lambda e: e.activation(dtt[:], dte[:], AF.Ln, bias=self.epsln[:, 2:3], scale=1.0),
                     reads=[dte, self.epsln], writes=[dtt])
                f.dma("sp", self.DTs[ti * 128:(ti + 1) * 128, :], dtt[:], reads=[dtt], writes=[V(self.DTs, ti)])
            for cc in range(12):
                bk = 5 + cc % 2
                for kc in range(8):
                    f.mm(self.PS[:, bk * 512: bk * 512 + TS], W1[:, kc, O_XBC + cc * 128: O_XBC + (cc + 1) * 128],
                         hT[:, kc, :], kc == 0, kc == 7, [hT, V(W1, kc)], self.bv(bk))
                f.op("act", lambda e: e.copy(xpre[:, cc, 3:3 + TS], self.PS[:, bk * 512: bk * 512 + TS]),
                     reads=self.bv(bk), writes=[V(xpre, cc)])
                f.op("dve", lambda e: e.tensor_scalar(acc[:], xpre[:, cc, 0:TS], CW[:, cc, 0:1], CBs[:, cc:cc + 1],
                                                      op0=ALU.mult, op1=ALU.add),
                     reads=[V(xpre, cc), CW, CBs], writes=[acc])
                for j in range(1, 4):
                    f.op("dve", lambda e: e.scalar_tensor_tensor(out=acc[:], in0=xpre[:, cc, j:j + TS],
                                                                 scalar=CW[:, cc, j:j + 1], in1=acc[:],
                                                                 op0=ALU.mult, op1=ALU.add),
                         reads=[V(xpre, cc), CW, acc], writes=[acc])
                f.op("act", lambda e: e.activation(xc[:, cc, :], acc[:], AF.Silu), reads=[acc], writes=[V(xc, cc)])
                f.op("dve", lambda e: e.tensor_copy(xpre[:, cc, 0:3], xpre[:, cc, TS:TS + 3]),
                     reads=[V(xpre, cc)], writes=[V(xpre, cc)])
            f.dma("sp", self.BT[:, :, u * TS:(u + 1) * TS], xc[:, 8:10, :], reads=[V(xc, 8), V(xc, 9)],
                  writes=[V(self.BT, u)])
            f.dma("sp", self.CTs[:, :, u * TS:(u + 1) * TS], xc[:, 10:12, :], reads=[V(xc, 10), V(xc, 11)],
                  writes=[V(self.CTs, u)])
            for i in range(TPU):
                ti = u * TPU + i
                xm = xtm[ti % 2]
                self.transposes(lambda c: xc[:, c, i * 128:(i + 1) * 128], 10,
                                lambda c0, n: xm[:, c0 * 128:(c0 + n) * 128].rearrange("p (c t) -> p c t", t=128),
                                0, [xc], [xm], evac="dve")
                f.dma("sp", self.X[ti * 128:(ti + 1) * 128, :], xm[:, 0:D], reads=[xm], writes=[V(self.X, ti)])
                f.dma("sp", self.BTM[ti * 128:(ti + 1) * 128, :], xm[:, D:D + 256], reads=[xm],
                      writes=[V(self.BTM, ti)])

    def phase_ssd(self, l):
        f, NT = self.f, self.NT
        PS = self.PS
        cU, cSL, ones = self.cU, self.cSL, self.ones
        Abc = self.bcast_load("s_A", self.a_log[l, :], NH)
        f.op("act", lambda e: e.activation(Abc[:], Abc[:], AF.Exp), reads=[Abc], writes=[Abc])
        f.op("dve", lambda e: e.tensor_scalar(Abc[:], Abc[:], -1.0, None, op0=ALU.mult), reads=[Abc], writes=[Abc])
        Dbc = self.bcast_load("s_D", self.d_skip[l, :], NH)
        NGb = self.bcast_load("s_ng", self.ssd_norm_g[l, :], D)
        ST = f.sb("s_ST", [128, 2, 512])
        f.op("dve", lambda e: e.memset(ST[:], 0.0), writes=[ST])
        xt = [f.sb("s_x%d" % i, [128, NH, HP]) for i in range(2)]
        zt = [f.sb("s_z%d" % i, [128, D]) for i in range(2)]
        dtt = [f.sb("s_dt%d" % i, [128, NH]) for i in range(2)]
        bct = [f.sb("s_bc%d" % i, [128, 4, 128]) for i in range(2)]
        btm = [f.sb("s_bm%d" % i, [128, 256]) for i in range(2)]
        a = f.sb("s_a", [128, NH]); acum = f.sb("s_acum", [128, NH]); eacum = f.sb("s_eacum", [128, NH])
        alast = f.sb("s_alast", [128, NH]); ealast = f.sb("s_ealast", [128, NH]); dend = f.sb("s_dend", [128, NH])
        xdt = f.sb("s_xdt", [128, NH, HP]); xdec = f.sb("s_xdec", [128, NH, HP])
        mCB = f.sb("s_mcb", [128, 128])
        Wa = [f.sb("s_wa%d" % i, [128, 4, 128]) for i in range(2)]
        LT = [f.sb("s_lt%d" % i, [128, 4, 128]) for i in range(2)]
        G = [f.sb("s_g%d" % i, [128, 4, 128]) for i in range(2)]
        y = f.sb("s_y", [128, NH, HP]); yo = f.sb("s_yo", [128, 8, HP])
        sz = f.sb("s_sz", [128, D]); ss = f.sb("s_ss", [128, 4]); junk = f.sb("s_junk", [128, 512])
        yout = [f.sb("s_yout%d" % i, [128, D]) for i in range(2)]
        for c in range(NT):
            x_, z_, d_, bc_, bm_ = xt[c % 2], zt[c % 2], dtt[c % 2], bct[c % 2], btm[c % 2]
            tok = slice(c * 128, (c + 1) * 128)
            f.dma("sp", x_[:].rearrange("p h d -> p (h d)"), self.X[tok, :], reads=[V(self.X, c)], writes=[x_])
            f.dma("sp", z_[:], self.Z[tok, :], reads=[V(self.Z, c)], writes=[z_])
            f.dma("sp", d_[:], self.DTs[tok, :], reads=[V(self.DTs, c)], writes=[d_])
            f.dma("sp", bc_[:, 0:2, :], self.BT[:, :, tok], reads=[self.BT], writes=[V(bc_, 0)])
            f.dma("sp", bc_[:, 2:4, :], self.CTs[:, :, tok], reads=[self.CTs], writes=[V(bc_, 1)])
            f.dma("sp", bm_[:], self.BTM[tok, :], reads=[V(self.BTM, c)], writes=[bm_])
            f.op("dve", lambda e: e.tensor_tensor(out=a[:], in0=d_[:], in1=Abc[:], op=ALU.mult),
                 reads=[d_, Abc], writes=[a])
            f.mm(PS[:, 0:NH], cU[:], a[:], True, True, [cU, a], self.bv(0))
            f.mm(PS[:, 512:512 + NH], ones[:], a[:], True, True, [ones, a], self.bv(1))
            f.op("dve", lambda e: e.tensor_copy(acum[:], PS[:, 0:NH]), reads=self.bv(0), writes=[acum])
            f.op("act", lambda e: e.activation(eacum[:], PS[:, 0:NH], AF.Exp), reads=self.bv(0), writes=[eacum])
            f.op("dve", lambda e: e.tensor_tensor(out=dend[:], in0=PS[:, 512:512 + NH], in1=acum[:], op=ALU.subtract),
                 reads=self.bv(1) + [acum], writes=[dend])
            f.op("act", lambda e: e.activation(dend[:], dend[:], AF.Exp), reads=[dend], writes=[dend])
            f.op("act", lambda e: e.activation(ealast[:], PS[:, 512:512 + NH], AF.Exp), reads=self.bv(1), writes=[ealast])
            f.op("dve", lambda e: e.tensor_tensor(out=xdt[:], in0=x_[:], in1=d_[:].unsqueeze(2).to_broadcast([128, NH, HP]),
                                                  op=ALU.mult), reads=[x_, d_], writes=[xdt])
            f.op("dve", lambda e: e.tensor_tensor(out=xdec[:], in0=xdt[:],
                                                  in1=dend[:].unsqueeze(2).to_broadcast([128, NH, HP]), op=ALU.mult),
                 reads=[xdt, dend], writes=[xdec])
            for g in range(2):
                f.mm(PS[:, 2 * 512:2 * 512 + 128], bc_[:, g, :], bc_[:, 2 + g, :], True, True, [bc_], self.bv(2))
                f.op("dve", lambda e: e.tensor_tensor(out=mCB[:], in0=PS[:, 2 * 512:2 * 512 + 128], in1=cU[:], op=ALU.mult),
                     reads=self.bv(2) + [cU], writes=[mCB])
                f.mm(self.bank(3), bc_[:, 2 + g, :], ST[:, g, :], True, True, [bc_, V(ST, g)], self.bv(3))
                for sg in range(2):
                    h0 = g * 8 + sg * 4
                    wa, lt, gg = Wa[sg], LT[sg], G[sg]
                    for hh in range(4):
                        f.op("pool", lambda e: e.tensor_scalar(wa[:, hh, :], cSL[:], a[:, h0 + hh:h0 + hh + 1], None,
                                                               op0=ALU.mult), reads=[cSL, a], writes=[V(wa, hh)])
                    bk = 4 + sg
                    for hh in range(4):
                        f.mm(PS[:, bk * 512 + hh * 128: bk * 512 + (hh + 1) * 128], wa[:, hh, :], cU[:], True, True,
                             [V(wa, hh), cU], self.bv(bk))
                    f.op("act", lambda e: e.activation(lt[:].rearrange("p h l -> p (h l)"), self.bank(bk), AF.Exp),
                         reads=self.bv(bk), writes=[lt])
                    f.op("dve", lambda e: e.tensor_tensor(out=gg[:], in0=lt[:],
                                                          in1=mCB[:].unsqueeze(1).to_broadcast([128, 4, 128]),
                                                          op=ALU.mult), reads=[lt, mCB], writes=[gg])
                    for hh in range(4):
                        h = h0 + hh
                        hl = sg * 4 + hh
                        f.mm(PS[:, 6 * 512 + hl * 64: 6 * 512 + (hl + 1) * 64], gg[:, hh, :], xdt[:, h, :], True, True,
                             [gg, xdt], self.bv(6))
                f.op("dve", lambda e: e.tensor_tensor(
                    out=yo[:], in0=self.bank(3).rearrange("p (h d) -> p h d", d=HP),
                    in1=eacum[:, g * 8:(g + 1) * 8].unsqueeze(2).to_broadcast([128, 8, HP]), op=ALU.mult),
                    reads=self.bv(3) + [eacum], writes=[yo])
                f.op("dve", lambda e: e.tensor_tensor(
                    out=y[:, g * 8:(g + 1) * 8, :], in0=self.bank(6).rearrange("p (h d) -> p h d", d=HP),
                    in1=yo[:], op=ALU.add), reads=self.bv(6) + [yo], writes=[V(y, g)])
                f.mm(self.bank(7), bm_[:, g * 128:(g + 1) * 128], xdec[:, g * 8:(g + 1) * 8, :].rearrange("p h d -> p (h d)"),
                     True, True, [bm_, xdec], self.bv(7))
                f.op("dve", lambda e: e.tensor_tensor(
                    out=ST[:, g, :].rearrange("p (h d) -> p h d", d=HP),
                    in0=ST[:, g, :].rearrange("p (h d) -> p h d", d=HP),
                    in1=ealast[:, g * 8:(g + 1) * 8].unsqueeze(2).to_broadcast([128, 8, HP]), op=ALU.mult),
                    reads=[V(ST, g), ealast], writes=[V(ST, g)])
                f.op("dve", lambda e: e.tensor_tensor(out=ST[:, g, :], in0=ST[:, g, :], in1=self.bank(7), op=ALU.add),
                     reads=[V(ST, g)] + self.bv(7), writes=[V(ST, g)])
            f.op("dve", lambda e: e.tensor_tensor(out=xdt[:], in0=x_[:],
                                                  in1=Dbc[:].unsqueeze(2).to_broadcast([128, NH, HP]), op=ALU.mult),
                 reads=[x_, Dbc], writes=[xdt])
            f.op("dve", lambda e: e.tensor_tensor(out=y[:], in0=y[:], in1=xdt[:], op=ALU.add),
                 reads=[y, xdt], writes=[y])
            f.op("act", lambda e: e.activation(sz[:], z_[:], AF.Silu), reads=[z_], writes=[sz])
            yf = y[:].rearrange("p h d -> p (h d)")
            f.op("dve", lambda e: e.tensor_tensor(out=sz[:], in0=sz[:], in1=yf, op=ALU.mult), reads=[sz, y], writes=[sz])
            for g in range(2):
                f.op("act", lambda e: e.activation(junk[:], sz[:, g * 512:(g + 1) * 512], AF.Square,
                                                   accum_out=ss[:, g:g + 1]), reads=[sz], writes=[junk, V(ss, g)])
            f.op("act", lambda e: e.activation(ss[:, 2:4], ss[:, 0:2], AF.Sqrt, bias=self.epsln[:, 1:2], scale=1.0 / 512),
                 reads=[ss, self.epsln], writes=[ss])
            f.op("dve", lambda e: e.reciprocal(ss[:, 2:4], ss[:, 2:4]), reads=[ss], writes=[ss])
            yo_ = yout[c % 2]
            for g in range(2):
                f.op("dve", lambda e: e.scalar_tensor_tensor(out=yo_[:, g * 512:(g + 1) * 512], in0=sz[:, g * 512:(g + 1) * 512],
                                                             scalar=ss[:, 2 + g:3 + g], in1=NGb[:, g * 512:(g + 1) * 512],
                                                             op0=ALU.mult, op1=ALU.mult),
                     reads=[sz, ss, NGb], writes=[yo_])
            f.dma("sp", self.MIX[tok, 0:D], yo_[:], reads=[yo_], writes=[V(self.MIX, ("ssd", c))])

    def phase_a2(self, l):
        f, TS, TPU, NU = self.f, self.TS, self.TPU, self.NU
        PS = self.PS
        NW = N_IN - O_QL
        oQL, oCKV, oQI, oKI, oWI, oQM = 0, O_CKV - O_QL, O_QI - O_QL, O_KI - O_QL, O_WI - O_QL, O_QM - O_QL
        W2 = f.sb("a2_W", [128, 8, NW])
        WK = f.sb("a2_WK", [128, 8, 128])
        for kc in range(8):
            f.dma("sp", W2[:, kc, :], self.w_in[l, kc * 128:(kc + 1) * 128, O_QL:N_IN], writes=[V(W2, kc)])
            for hf in range(2):
                f.dma("sp", WK[:, kc, hf * 64:(hf + 1) * 64], self.w_in[l, kc * 128:(kc + 1) * 128, O_KI:O_KI + 64],
                      writes=[V(WK, (kc, hf))])
        W2b = f.sb("a2_Wb", [128, 8, NW], BF16)
        for kc in range(8):
            f.op("pool", lambda e: e.tensor_copy(W2b[:, kc, :], W2[:, kc, :]), reads=[V(W2, kc)], writes=[V(W2b, kc)])
        KVG = self.bcast_load("a2_kvg", self.kv_norm_g[l, :], DLAT)
        hT = f.sb("a2_hT", [128, 8, TS])
        hTb = f.sb("a2_hTb", [128, 8, TS], BF16)
        htiles = [f.sb("a2_h%d" % i, [128, D]) for i in range(2)]
        qst = f.sb("a2_qst", [128, TPU, 8, 128], BF16)
        qist = f.sb("a2_qist", [128, 2, TS]); kist = f.sb("a2_kist", [128, TS])
        qmst = f.sb("a2_qmst", [128, 4, TS]); ctst = f.sb("a2_ctst", [128, TS], BF16)
        cvb = [f.sb("a2_cvb%d" % i, [128, 129], BF16) for i in range(2)]
        cv = [f.sb("a2_cv%d" % i, [128, 129]) for i in range(2)]
        wit = [f.sb("a2_wi%d" % i, [128, 4]) for i in range(2)]
        ss = f.sb("a2_ss", [128, 4]); junk = f.sb("a2_junk", [128, 128])
        for c_ in cv:
            f.op("dve", lambda e: e.memset(c_[:, 128:129], 1.0), writes=[V(c_, "one")])
        sc_lat = DLAT ** -0.5
        sc_mem = MEM_D ** -0.5
        nb = 0
        for u in range(NU):
            self.load_hT(hT, u, htiles, hTb)
            tsl = slice(u * TS, (u + 1) * TS)

            def fm_chunk(wcols, dst, scale, wr, lowp=False):
                nonlocal nb
                bk = 2 + nb % 2
                nb += 1
                hsrc = hTb if lowp else hT
                for kc in range(8):
                    f.mm(PS[:, bk * 512: bk * 512 + TS], wcols(kc), hsrc[:, kc, :], kc == 0, kc == 7,
                         [hsrc, W2, W2b, WK], self.bv(bk))
                src = PS[:, bk * 512: bk * 512 + TS]
                if len(dst.shape) == 3:
                    src = src.rearrange("p (i t) -> p i t", t=128)
                if scale is None:
                    f.op("act", lambda e: e.copy(dst, src), reads=self.bv(bk), writes=wr)
                else:
                    f.op("act", lambda e: e.activation(dst, src, AF.Copy, scale=scale), reads=self.bv(bk), writes=wr)

            for h in range(8):
                fm_chunk(lambda kc: W2b[:, kc, oQL + h * 128: oQL + (h + 1) * 128],
                         qst[:, :, h, :], sc_lat, [V(qst, h)], lowp=True)
            f.dma("sp", self.QL[:, u * TPU:(u + 1) * TPU, :], qst[:].rearrange("p i h t -> p i (h t)"),
                  reads=[qst], writes=[V(self.QL, u)])
            for j in range(2):
                fm_chunk(lambda kc: W2[:, kc, oQI + j * 128: oQI + (j + 1) * 128], qist[:, j, :], None, [V(qist, j)])
            f.dma("sp", self.QI[:, :, tsl], qist[:], reads=[qist], writes=[V(self.QI, u)])
            fm_chunk(lambda kc: WK[:, kc, :], kist[:], None, [kist])
            f.dma("sp", self.KI[:, tsl], kist[:], reads=[kist], writes=[V(self.KI, u)])
            for h in range(4):
                fm_chunk(lambda kc: W2b[:, kc, oQM + h * 128: oQM + (h + 1) * 128], qmst[:, h, :], sc_mem, [V(qmst, h)], lowp=True)
            f.dma("sp", self.QM[:, :, tsl], qmst[:], reads=[qmst], writes=[V(self.QM, u)])
            for i in range(TPU):
                ti = u * TPU + i
                tok = slice(ti * 128, (ti + 1) * 128)
                c_, w_ = cv[ti % 2], wit[ti % 2]
                for kc in range(8):
                    f.mm(PS[:, 4 * 512: 4 * 512 + 128], hTb[:, kc, i * 128:(i + 1) * 128], W2b[:, kc, oCKV:oCKV + 128],
                         kc == 0, kc == 7, [hTb, W2b], self.bv(4))
                for kc in range(8):
                    f.mm(PS[:, 5 * 512: 5 * 512 + 4], hT[:, kc, i * 128:(i + 1) * 128], W2[:, kc, oWI:oWI + 4],
                         kc == 0, kc == 7, [hT, W2], self.bv(5))
                f.op("act", lambda e: e.copy(w_[:], PS[:, 5 * 512: 5 * 512 + 4]), reads=self.bv(5), writes=[w_])
                f.dma("sp", self.WI[tok, :], w_[:], reads=[w_], writes=[V(self.WI, ti)])
                f.op("act", lambda e: e.activation(junk[:], PS[:, 4 * 512: 4 * 512 + 128], AF.Square, accum_out=ss[:, 0:1]),
                     reads=self.bv(4), writes=[junk, ss])
                f.op("act", lambda e: e.activation(ss[:, 1:2], ss[:, 0:1], AF.Sqrt, bias=self.epsln[:, 1:2], scale=1.0 / DLAT),
                     reads=[ss, self.epsln], writes=[ss])
                f.op("dve", lambda e: e.reciprocal(ss[:, 2:3], ss[:, 1:2]), reads=[ss], writes=[ss])
                f.op("dve", lambda e: e.scalar_tensor_tensor(out=c_[:, 0:128], in0=PS[:, 4 * 512: 4 * 512 + 128],
                                                             scalar=ss[:, 2:3], in1=KVG[:], op0=ALU.mult, op1=ALU.mult),
                     reads=self.bv(4) + [ss, KVG], writes=[V(c_, "c")])
                cb_ = cvb[ti % 2]
                f.op("act", lambda e: e.copy(cb_[:], c_[:]), reads=[c_], writes=[cb_])
                f.dma("sp", self.CV[tok, :], cb_[:], reads=[cb_], writes=[V(self.CV, ti)])
                f.tr(PS[:, 6 * 512: 6 * 512 + 128], c_[:, 0:128], self.ident[:], [V(c_, "c"), self.ident], self.bv(6))
                f.op("act", lambda e: e.copy(ctst[:, i * 128:(i + 1) * 128], PS[:, 6 * 512: 6 * 512 + 128]),
                     reads=self.bv(6), writes=[V(ctst, i)])
            f.dma("sp", self.CT[:, tsl], ctst[:], reads=[ctst], writes=[V(self.CT, u)])

    def phase_dsa(self, l):
        f, S, NT, NIT, NSEL = self.f, self.S, self.NT, self.NIT, self.NSEL
        PS = self.PS
        CTr = f.sb("d_CT", [128, S], BF16); EPSP = f.sb("d_eps", [128, S])
        VAt = [f.sb("d_VA%d" % i, [128, 129], BF16) for i in range(4)]
        f.dma("sp", CTr[:], self.CT[:], reads=[self.CT], writes=[CTr])
        f.dma("sp", EPSP[:], self.cin["c_epspos"][:], writes=[EPSP])
        WUV = f.sb("d_wuv", [128, 8, 64])
        f.dma("sp", WUV[:], self.w_uv[l].rearrange("h c d -> c h d"), writes=[WUV])
        I4 = f.sb("d_i4", [128, 512], BF16); CBM = f.sb("d_cbm", [128, 128]); ONEI = f.sb("d_onei", [65, 128], BF16)
        NSL = f.sb("d_nsl", [33, 1024])
        f.dma("sp", NSL[:], self.cin["c_negsl33"][:], writes=[NSL])
        POW2 = f.sb("d_pow2", [128, 64])
        f.dma("sp", I4[:], self.cin["c_i4"][:], writes=[I4])
        f.dma("sp", CBM[:], self.cin["c_cbm"][:], writes=[CBM])
        f.dma("sp", ONEI[:], self.cin["c_onei"][:], writes=[ONEI])
        f.dma("sp", POW2[:], self.cin["c_pow2"][:], writes=[POW2])
        RB = [f.sb("d_rb%d" % i, [65, 1024], BF16) for i in range(2)]
        SL128 = f.sb("d_sl128", [1, 1024])
        f.dma("sp", SL128[:], self.cin["c_sl128"][:], writes=[SL128])
        for r in RB:
            f.dma("sp", r[:], self.cin["c_rbinit"][:], writes=[r])
        smrep = f.sb("d_smrep", [128, 33])
        srow = f.sb("d_srow", [33, 4, 128]); srow_i = f.sb("d_srowi", [33, 2, 128], I32)
        SC = f.sb("d_SC", [128, S]); MB = f.sb("d_MB", [128, S], BF16)
        Pt = [f.sb("d_P%d" % i, [128, 1024], BF16) for i in range(2)]
        QLb = [f.sb("d_ql%d" % i, [128, 1024], BF16) for i in range(2)]
        QIb = [f.sb("d_qi%d" % i, [128, 2, 128]) for i in range(2)]
        WIb = [f.sb("d_wi%d" % i, [128, 4]) for i in range(2)]
        KIs = [f.sb("d_ki%d" % i, [128, 512]) for i in range(2)]
        rl = [f.sb("d_rl%d" % i, [128, 512]) for i in range(2)]
        sm = f.sb("d_sm", [128, 16])
        hs = f.sb("d_hs", [128, 64])
        ctx = f.sb("d_ctx", [128, 8, 128]); ctxT = f.sb("d_ctxT", [128, 8, 128])
        rinv = f.sb("d_rinv", [128, 8]); yd = [f.sb("d_yd%d" % i, [128, 512]) for i in range(2)]
        ACC = PS[:, 4 * 512: 8 * 512].rearrange("p (h c) -> p h c", c=256)
        nrl = 0
        nkt = 0
        for b in range(NT):
            t0 = b * 128
            te = t0 + 128
            ql, qi, wi = QLb[b % 2], QIb[b % 2], WIb[b % 2]
            f.dma("sp", ql[:], self.QL[:, b, :], reads=[self.QL], writes=[ql])
            f.dma("sp", qi[:], self.QI[:, :, t0:te], reads=[self.QI], writes=[qi])
            f.dma("sp", wi[:], self.WI[t0:te, :], reads=[self.WI], writes=[wi])
            nsl = (te + 511) // 512
            for si in range(nsl):
                c0 = si * 512
                ncol = min(512, te - c0)
                ks = KIs[si % 2]
                f.dma("sp", ks[:, 0:ncol], self.KI[:, c0:c0 + ncol], reads=[self.KI], writes=[ks])
                for hi in range(4):
                    bk = nrl % 4
                    r_ = rl[nrl % 2]
                    nrl += 1
                    pb = (hi % 2) * 64
                    f.mm(PS[:, bk * 512: bk * 512 + ncol], qi[pb:pb + 64, hi // 2, :], ks[pb:pb + 64, 0:ncol], True, True,
                         [qi, ks], self.bv(bk))
                    f.op("act", lambda e: e.activation(r_[:, 0:ncol], PS[:, bk * 512: bk * 512 + ncol], AF.Relu),
                         reads=self.bv(bk), writes=[r_])
                    prev = EPSP if hi == 0 else SC
                    f.op("dve", lambda e: e.scalar_tensor_tensor(out=SC[:, c0:c0 + ncol], in0=r_[:, 0:ncol],
                                                                 scalar=wi[:, hi:hi + 1], in1=prev[:, c0:c0 + ncol],
                                                                 op0=ALU.mult, op1=ALU.add),
                         reads=[r_, wi, EPSP, SC], writes=[SC])
            f.op("dve", lambda e: e.tensor_reduce(out=sm[:, 0:1], in_=SC[:, 0:te], axis=AX.X, op=ALU.min),
                 reads=[SC], writes=[V(sm, 0)])
            f.op("dve", lambda e: e.tensor_tensor(out=SC[:, t0:te], in0=SC[:, t0:te], in1=CBM[:], op=ALU.add),
                 reads=[SC, CBM], writes=[SC])
            f.op("dve", lambda e: e.tensor_reduce(out=sm[:, 1:2], in_=SC[:, 0:te], axis=AX.X, op=ALU.max),
                 reads=[SC], writes=[V(sm, 1)])
            f.op("dve", lambda e: e.tensor_tensor(out=sm[:, 7:8], in0=sm[:, 1:2], in1=sm[:, 0:1], op=ALU.subtract),
                 reads=[V(sm, 0), V(sm, 1)], writes=[V(sm, 7)])
            f.op("dve", lambda e: e.tensor_scalar(sm[:, 7:8], sm[:, 7:8], 1.001, 1e-6, op0=ALU.mult, op1=ALU.add),
                 reads=[V(sm, 7)], writes=[V(sm, 7)])
            f.op("dve", lambda e: e.tensor_scalar(hs[:, 0:NIT], POW2[:, 0:NIT], sm[:, 7:8], None, op0=ALU.mult),
                 reads=[POW2, V(sm, 7)], writes=[hs])
            f.op("dve", lambda e: e.tensor_copy(sm[:, 2:3], sm[:, 0:1]), reads=[V(sm, 0)], writes=[V(sm, 2)])
            for it in range(NIT):
                f.op("dve", lambda e: e.tensor_tensor(out=sm[:, 3:4], in0=sm[:, 2:3], in1=hs[:, it:it + 1], op=ALU.add),
                     reads=[V(sm, 2), hs], writes=[V(sm, 3)])
                f.op("dve", lambda e: e.tensor_scalar(MB[:, 0:te], SC[:, 0:te], sm[:, 3:4], 0.0, op0=ALU.is_ge, op1=ALU.add,
                                                      accum_out=sm[:, 4:5]), reads=[SC, V(sm, 3)], writes=[MB, V(sm, 4)])
                f.op("dve", lambda e: e.tensor_scalar(sm[:, 5:6], sm[:, 4:5], float(NSEL) - 0.5, hs[:, it:it + 1],
                                                      op0=ALU.is_ge, op1=ALU.mult), reads=[V(sm, 4), hs], writes=[V(sm, 5)])
                f.op("dve", lambda e: e.tensor_tensor(out=sm[:, 2:3], in0=sm[:, 2:3], in1=sm[:, 5:6], op=ALU.add),
                     reads=[V(sm, 2), V(sm, 5)], writes=[V(sm, 2)])
            f.op("dve", lambda e: e.tensor_scalar(MB[:, 0:te], SC[:, 0:te], sm[:, 2:3], NEG, op0=ALU.is_lt, op1=ALU.mult),
                 reads=[SC, V(sm, 2)], writes=[MB])
            f.op("dve", lambda e: e.tensor_tensor(out=SC[:, 0:te], in0=MB[:, 0:te], in1=EPSP[:, 0:te], op=ALU.subtract),
                 reads=[EPSP, MB], writes=[SC])
            f.op("dve", lambda e: e.tensor_reduce(out=sm[:, 6:7], in_=SC[:, 0:te], axis=AX.X, op=ALU.max),
                 reads=[SC], writes=[V(sm, 6)])
            f.op("dve", lambda e: e.tensor_copy(smrep[:], sm[:, 6:7].to_broadcast([128, 33])), reads=[V(sm, 6)], writes=[smrep])
            f.tr(PS[0:33, 0:128], smrep[:], self.ident[:], [smrep, self.ident], self.bv(0))
            f.op("dve", lambda e: e.tensor_scalar(srow[:, 0, :], PS[0:33, 0:128], 2.0 ** 30, None, op0=ALU.mult),
                 reads=self.bv(0), writes=[V(srow, 0)])
            f.op("dve", lambda e: e.tensor_copy(srow_i[:, 0, :], srow[:, 0, :]), reads=[V(srow, 0)], writes=[V(srow_i, 0)])
            f.op("dve", lambda e: e.tensor_scalar(srow_i[:, 1, :], srow_i[:, 0, :], 127, None, op0=ALU.bitwise_and),
                 reads=[V(srow_i, 0)], writes=[V(srow_i, 1)])
            f.op("dve", lambda e: e.tensor_copy(srow[:, 1, :], srow_i[:, 1, :]), reads=[V(srow_i, 1)], writes=[V(srow, 1)])
            f.op("dve", lambda e: e.tensor_tensor(out=srow[:, 2, :], in0=srow[:, 0, :], in1=srow[:, 1, :], op=ALU.subtract),
                 reads=[V(srow, 0), V(srow, 1)], writes=[V(srow, 2)])
            rb0 = RB[nkt % 2]
            f.op("dve", lambda e: e.tensor_tensor(out=rb0[0:1, :].rearrange("p (h t) -> p h t", t=128),
                                                  in0=NSL[0:1, :].rearrange("p (h t) -> p h t", t=128),
                                                  in1=srow[0:1, 2, :].unsqueeze(1).to_broadcast([1, 8, 128]), op=ALU.mult),
                 reads=[NSL, V(srow, 2)], writes=[V(rb0, 0)])
            for r_ in RB:
                f.op("dve", lambda e: e.tensor_tensor(out=r_[32:33, :].rearrange("p (h t) -> p h t", t=128),
                                                      in0=NSL[32:33, :].rearrange("p (h t) -> p h t", t=128),
                                                      in1=srow[32:33, 1, :].unsqueeze(1).to_broadcast([1, 8, 128]), op=ALU.mult),
                     reads=[NSL, V(srow, 1)], writes=[V(r_, 32)])
            for j in range(b + 1):
                rb = RB[nkt % 2]
                p_ = Pt[nkt % 2]
                lb = (nkt % 2) * 2
                nkt += 1
                va = VAt[nkt % 4]
                f.dma("sp", va[:], self.CV[j * 128:(j + 1) * 128, :], reads=[self.CV], writes=[va])
                if j > 0:
                    rbp = RB[(nkt - 2) % 2]
                    f.op("pool", lambda e: e.tensor_tensor(out=rb[0:1, :], in0=rbp[0:1, :], in1=SL128[:], op=ALU.add),
                         reads=[V(rbp, 0), SL128], writes=[V(rb, 0)])
                for g in range(2):
                    o_ = self.bank(lb + g)
                    f.mm(o_, CTr[:, j * 128:(j + 1) * 128], ql[:, g * 512:(g + 1) * 512], True, False, [CTr, ql], self.bv(lb + g))
                    f.mm(o_, ONEI[:], rb[:, g * 512:(g + 1) * 512], False, False, [ONEI, rb], self.bv(lb + g))
                    f.mm(o_, MB[:, j * 128:(j + 1) * 128], I4[:], False, True, [MB, I4], self.bv(lb + g))
                    f.op("act", lambda e: e.activation(p_[:, g * 512:(g + 1) * 512], o_, AF.Exp),
                         reads=self.bv(lb + g), writes=[V(p_, g)])
                for h in range(8):
                    f.mm(ACC[:, h, 0:129], p_[:, h * 128:(h + 1) * 128], va[:], (j == 0 and h % 2 == 0), j == b,
                         [V(p_, h // 4), va], self.bv(4 + h // 2))
            f.op("dve", lambda e: e.reciprocal(rinv[:], ACC[:, :, 128]), reads=self.bv(4, 4), writes=[rinv])
            f.op("dve", lambda e: e.tensor_tensor(out=ctx[:], in0=ACC[:, :, 0:128],
                                                  in1=rinv[:].unsqueeze(2).to_broadcast([128, 8, 128]), op=ALU.mult),
                 reads=self.bv(4, 4) + [rinv], writes=[ctx])
            self.transposes(lambda c: ctx[:, c, :], 8, lambda c0, n: ctxT[:, c0:c0 + n, :], 0, [ctx], [ctxT])
            for h in range(8):
                f.mm(PS[:, 2 * 512 + h * 64: 2 * 512 + (h + 1) * 64], ctxT[:, h, :], WUV[:, h, :], True, True,
                     [ctxT, WUV], self.bv(2))
            y_ = yd[b % 2]
            f.op("act", lambda e: e.copy(y_[:], self.bank(2)), reads=self.bv(2), writes=[y_])
            f.dma("sp", self.MIX[t0:te, D:D + 512], y_[:], reads=[y_], writes=[V(self.MIX, ("dsa", b))])

    def phase_mem(self, l):
        f, NT = self.f, self.NT
        PS = self.PS
        WKm = f.sb("m_wk", [128, 8, 512]); WVm = f.sb("m_wv", [128, 8, 512])
        f.dma("sp", WKm[:], self.w_mem_k[l].rearrange("(c p) n -> p c n", p=128), writes=[WKm])
        f.dma("sp", WVm[:], self.w_mem_v[l].rearrange("(c p) n -> p c n", p=128), writes=[WVm])
        memKT = f.sb("m_kT", [128, 4, MEM_LEN]); memV = f.sb("m_v", [128, 2, 512])
        self.memT = f.sb("m_memT", [128, 8, MEM_LEN])
        mt_ = f.sb("m_mem", [128, D])
        for mi in range(MEM_LEN // 128):
            f.dma("sp", mt_[:], self.mem_in[mi * 128:(mi + 1) * 128, :], writes=[mt_])
            self.transposes(lambda c: mt_[:, c * 128:(c + 1) * 128], 8,
                            lambda c0, n: self.memT[:, c0:c0 + n, mi * 128:(mi + 1) * 128], 0, [mt_], [self.memT])
        for h in range(4):
            for kc in range(8):
                f.mm(PS[:, 0:MEM_LEN], WKm[:, kc, h * 128:(h + 1) * 128], self.memT[:, kc, :], kc == 0, kc == 7,
                     [WKm, self.memT], self.bv(0))
            f.op("act", lambda e: e.copy(memKT[:, h, :], PS[:, 0:MEM_LEN]), reads=self.bv(0), writes=[memKT])
        for mt in range(2):
            for kc in range(8):
                f.mm(self.bank(1), self.memT[:, kc, mt * 128:(mt + 1) * 128], WVm[:, kc, :], kc == 0, kc == 7,
                     [WVm, self.memT], self.bv(1))
            f.op("act", lambda e: e.copy(memV[:, mt, :], self.bank(1)), reads=self.bv(1), writes=[memV])
        QMt = [f.sb("m_q%d" % i, [128, 4, 128]) for i in range(2)]
        Pm = f.sb("m_P", [128, MEM_LEN]); PT = f.sb("m_PT", [128, 2, 128])
        sm = f.sb("m_sm", [128, 4]); ym = [f.sb("m_y%d" % i, [128, 512]) for i in range(2)]
        for i in range(NT):
            q_, y_ = QMt[i % 2], ym[i % 2]
            tok = slice(i * 128, (i + 1) * 128)
            f.dma("sp", q_[:], self.QM[:, :, tok], reads=[self.QM], writes=[q_])
            for h in range(4):
                bk = 2 + h % 2
                f.mm(PS[:, bk * 512: bk * 512 + MEM_LEN], q_[:, h, :], memKT[:, h, :], True, True, [q_, memKT], self.bv(bk))
                f.op("dve", lambda e: e.tensor_reduce(out=sm[:, 0:1], in_=PS[:, bk * 512: bk * 512 + MEM_LEN], axis=AX.X,
                                                      op=ALU.max), reads=self.bv(bk), writes=[V(sm, 0)])
                f.op("dve", lambda e: e.tensor_scalar(sm[:, 1:2], sm[:, 0:1], -1.0, None, op0=ALU.mult),
                     reads=[V(sm, 0)], writes=[V(sm, 1)])
                f.op("act", lambda e: e.activation(Pm[:], PS[:, bk * 512: bk * 512 + MEM_LEN], AF.Exp, bias=sm[:, 1:2],
                                                   scale=1.0, accum_out=sm[:, 2:3]),
                     reads=self.bv(bk) + [V(sm, 1)], writes=[Pm, V(sm, 2)])
                f.op("dve", lambda e: e.reciprocal(sm[:, 3:4], sm[:, 2:3]), reads=[V(sm, 2)], writes=[V(sm, 3)])
                self.transposes(lambda c: Pm[:, c * 128:(c + 1) * 128], 2, lambda c0, n: PT[:, c0:c0 + n, :], 4, [Pm], [PT])
                for mt in range(2):
                    f.mm(PS[:, 6 * 512: 6 * 512 + 128], PT[:, mt, :], memV[:, mt, h * 128:(h + 1) * 128], mt == 0, mt == 1,
                         [PT, memV], self.bv(6))
                f.op("dve", lambda e: e.tensor_scalar(y_[:, h * 128:(h + 1) * 128], PS[:, 6 * 512: 6 * 512 + 128],
                                                      sm[:, 3:4], None, op0=ALU.mult),
                     reads=self.bv(6) + [V(sm, 3)], writes=[y_])
            f.dma("sp", self.MIX[tok, D + 512:D + 1024], y_[:], reads=[y_], writes=[V(self.MIX, ("mem", i))])

    def phase_outp(self, l):
        f, NT, CAP = self.f, self.NT, self.CAP
        PS = self.PS
        WO = f.sb("o_WO", [128, 16, D], BF16)
        WOs = [f.sb("o_WOs%d" % i, [128, D]) for i in range(2)]
        for kc in range(16):
            ws = WOs[kc % 2]
            f.dma("sp", ws[:], self.w_out[l, kc * 128:(kc + 1) * 128, :], writes=[ws])
            f.op("pool", lambda e: e.tensor_copy(WO[:, kc, :], ws[:]), reads=[ws], writes=[V(WO, kc)])
        RW = f.sb("o_RW", [128, 8, NE])
        f.dma("sp", RW[:], self.router_w[l].rearrange("(c p) n -> p c n", p=128), writes=[RW])
        RBb = self.bcast_load("o_rb", self.router_b[l, :], NE)
        g1 = self.bcast_load("o_g1", self.ln1_g[l, :], D)
        b1 = self.bcast_load("o_b1", self.ln1_b[l, :], D)
        ECAP = f.sb("o_ecap", [128, NE])
        f.dma("sp", ECAP[:], self.cin["c_eidx"][:], writes=[ECAP])
        f.op("dve", lambda e: e.tensor_scalar(ECAP[:], ECAP[:], float(CAP), None, op0=ALU.mult), reads=[ECAP], writes=[ECAP])
        base = f.sb("o_base", [128, NE])
        f.op("dve", lambda e: e.memset(base[:], 0.0), writes=[base])
        if l == 0:
            zt = f.sb("o_zero", [128, D])
            f.op("dve", lambda e: e.memset(zt[:], 0.0), writes=[zt])
            NR = CAP // 128
            for e_ in range(NE):
                f.dma("sp", self.XD[e_ * CAP:(e_ + 1) * CAP, :].rearrange("(r p) d -> p r d", p=128),
                      zt[:].unsqueeze(1).to_broadcast([128, NR, D]), reads=[zt], writes=[V(self.XD, ("z", e_))])
            f.barrier()
        mixt = [f.sb("o_mix%d" % i, [128, DMIX]) for i in range(2)]
        ht = [f.sb("o_h%d" % i, [128, D]) for i in range(2)]
        mixT = f.sb("o_mixT", [128, 16, 128], BF16)
        r_ = f.sb("o_r", [128, D]); scr = f.sb("o_scr", [128, D])
        h2t = [f.sb("o_h2%d" % i, [128, D]) for i in range(2)]
        h2T = f.sb("o_h2T", [128, 8, 128])
        stats = f.sb("o_st", [128, 12]); mv = f.sb("o_mv", [128, 4])
        lg = f.sb("o_lg", [128, NE]); m8 = f.sb("o_m8", [128, 8]); sel = f.sb("o_sel", [128, NE])
        e4 = f.sb("o_e4", [128, 4]); sm = f.sb("o_sm", [128, 4])
        g4 = [f.sb("o_g4%d" % i, [128, 4]) for i in range(2)]
        slotf = f.sb("o_slotf", [128, NE]); tmp = f.sb("o_tmp", [128, NE]); eq = f.sb("o_eq", [128, NE])
        junk = f.sb("o_junk", [128, NE])
        s4f = f.sb("o_s4f", [128, 4])
        s4i = [f.sb("o_s4i%d" % i, [128, 4], I32) for i in range(2)]
        for i in range(NT):
            tok = slice(i * 128, (i + 1) * 128)
            mx, h_, h2_, g4_, s4_ = mixt[i % 2], ht[i % 2], h2t[i % 2], g4[i % 2], s4i[i % 2]
            f.dma("sp", mx[:], self.MIX[tok, :], reads=[self.MIX], writes=[mx])
            f.dma("sp", h_[:], self.H[tok, :], reads=[V(self.H, i)], writes=[h_])
            self.transposes(lambda c: mx[:, c * 128:(c + 1) * 128], 16, lambda c0, n: mixT[:, c0:c0 + n, :], 0, [mx], [mixT])
            for sl in range(2):
                for kc in range(16):
                    f.mm(self.bank(2 + sl), mixT[:, kc, :], WO[:, kc, sl * 512:(sl + 1) * 512], kc == 0, kc == 15,
                         [mixT, V(WO, kc)], self.bv(2 + sl))
                f.op("dve", lambda e: e.scalar_tensor_tensor(out=r_[:, sl * 512:(sl + 1) * 512], in0=h_[:, sl * 512:(sl + 1) * 512],
                                                             scalar=float(ALPHA), in1=self.bank(2 + sl), op0=ALU.mult, op1=ALU.add),
                     reads=[h_] + self.bv(2 + sl), writes=[r_])
            self.ln_tile(r_, h2_, g1, b1, scr, stats, mv)
            f.dma("sp", self.H2[tok, :], h2_[:], reads=[h2_], writes=[V(self.H2, i)])
            self.transposes(lambda c: h2_[:, c * 128:(c + 1) * 128], 8, lambda c0, n: h2T[:, c0:c0 + n, :], 0, [h2_], [h2T])
            for kc in range(8):
                f.mm(PS[:, 4 * 512: 4 * 512 + NE], h2T[:, kc, :], RW[:, kc, :], kc == 0, kc == 7, [h2T, RW], self.bv(4))
            f.op("dve", lambda e: e.tensor_tensor(out=lg[:], in0=PS[:, 4 * 512: 4 * 512 + NE], in1=RBb[:], op=ALU.add),
                 reads=self.bv(4) + [RBb], writes=[lg])
            f.op("dve", lambda e: e.max(out=m8[:], in_=lg[:]), reads=[lg], writes=[m8])
            f.op("dve", lambda e: e.tensor_scalar(sel[:], lg[:], m8[:, 3:4], None, op0=ALU.is_ge), reads=[lg, m8], writes=[sel])
            f.op("dve", lambda e: e.tensor_scalar(sm[:, 0:1], m8[:, 0:1], -1.0, None, op0=ALU.mult), reads=[m8], writes=[V(sm, 0)])
            f.op("act", lambda e: e.activation(e4[:], m8[:, 0:4], AF.Exp, bias=sm[:, 0:1], scale=1.0, accum_out=sm[:, 1:2]),
                 reads=[m8, V(sm, 0)], writes=[e4, V(sm, 1)])
            f.op("dve", lambda e: e.reciprocal(sm[:, 2:3], sm[:, 1:2]), reads=[V(sm, 1)], writes=[V(sm, 2)])
            f.op("dve", lambda e: e.tensor_scalar(g4_[:], e4[:], sm[:, 2:3], None, op0=ALU.mult), reads=[e4, V(sm, 2)], writes=[g4_])
            f.dma("sp", self.GATE4[tok, :], g4_[:], reads=[g4_], writes=[V(self.GATE4, i)])
            f.mm(PS[:, 5 * 512: 5 * 512 + NE], self.cSU[:], sel[:], True, True, [self.cSU, sel], self.bv(5))
            f.mm(PS[:, 6 * 512: 6 * 512 + NE], self.ones[:], sel[:], True, True, [self.ones, sel], self.bv(6))
            f.op("dve", lambda e: e.tensor_tensor(out=tmp[:], in0=PS[:, 5 * 512: 5 * 512 + NE], in1=base[:], op=ALU.add),
                 reads=self.bv(5) + [base], writes=[tmp])
            f.op("dve", lambda e: e.tensor_tensor(out=slotf[:], in0=tmp[:], in1=ECAP[:], op=ALU.add), reads=[tmp, ECAP], writes=[slotf])
            f.op("dve", lambda e: e.tensor_scalar(tmp[:], tmp[:], float(CAP) - 0.5, 1.0e9, op0=ALU.is_ge, op1=ALU.mult),
                 reads=[tmp], writes=[tmp])
            f.op("dve", lambda e: e.tensor_tensor(out=slotf[:], in0=slotf[:], in1=tmp[:], op=ALU.add), reads=[slotf, tmp], writes=[slotf])
            f.op("dve", lambda e: e.tensor_tensor(out=base[:], in0=base[:], in1=PS[:, 6 * 512: 6 * 512 + NE], op=ALU.add),
                 reads=[base] + self.bv(6), writes=[base])
            for k in range(4):
                f.op("dve", lambda e: e.scalar_tensor_tensor(out=junk[:], in0=lg[:], scalar=m8[:, k:k + 1], in1=slotf[:],
                                                             op0=ALU.is_equal, op1=ALU.mult, accum_out=s4f[:, k:k + 1]),
                     reads=[lg, m8, slotf], writes=[junk, V(s4f, k)])
            f.op("dve", lambda e: e.tensor_copy(s4_[:], s4f[:]), reads=[s4f], writes=[s4_])
            f.dma("sp", self.SLOT4[tok, :], s4_[:], reads=[s4_], writes=[V(self.SLOT4, i)])
            for k in range(4):
                f.dma("pool", self.XD[:, :], h2_[:, :], reads=[h2_, s4_], writes=[V(self.XD, ("s", i, k))],
                      indirect=dict(out_offset=bass.IndirectOffsetOnAxis(ap=s4_[:, k:k + 1], axis=0), in_offset=None,
                                    bounds_check=self.bc_reg(), oob_is_err=False))

    def phase_moe(self, l):
        f, CAP = self.f, self.CAP
        PS = self.PS
        NR = CAP // 128
        BGall = f.sb("e_bgall", [128, 16, NE])
        with ExitStack() as tmpst:
            old_st = f.stack
            f.stack = tmpst
            bgl = f.sb("e_bgl", [NE, 2 * DFF])
            f.dma("sp", bgl[:], self.b_gu[l], writes=[bgl])
            for c in range(16):
                bk = c % 2
                f.tr(PS[0:128, bk * 512: bk * 512 + NE], bgl[:, c * 128:(c + 1) * 128], self.ident[0:NE, 0:NE],
                     [bgl, self.ident], self.bv(bk))
                f.op("act", lambda e: e.copy(BGall[:, c, :], PS[:, bk * 512: bk * 512 + NE]), reads=self.bv(bk), writes=[V(BGall, c)])
            f.barrier()
            f.stack = old_st
        XT = f.sb("e_XT", [128, 8, CAP], BF16); AT = f.sb("e_AT", [128, 8, CAP], BF16)
        WD = [f.sb("e_WD%d" % i, [128, 8, D], BF16) for i in range(2)]
        WDs = f.sb("e_WDs", [128, 8, D])
        WG = [f.sb("e_WG%d" % i, [128, 8, 256], BF16) for i in range(2)]
        WGs = [f.sb("e_WGs%d" % i, [128, 8, 256]) for i in range(2)]
        xr = [f.sb("e_xr%d" % i, [128, D]) for i in range(2)]
        yr = [f.sb("e_yr%d" % i, [128, D]) for i in range(2)]
        BD = [f.sb("e_BD%d" % i, [128, D]) for i in range(2)]
        tg = f.sb("e_tg", [128, 512]); tsg = f.sb("e_tsg", [128, 512]); tl = f.sb("e_tl", [128, 512])
        slabs = [(s0, min(512, CAP - s0)) for s0 in range(0, CAP, 512)]
        nwg = 0
        nps = 0
        nrow = 0
        for e_ in range(NE):
            wd, bd = WD[e_ % 2], BD[e_ % 2]
            f.dma("sp", bd[:], self.b_down[l, e_, :].partition_broadcast(128), writes=[bd])
            for r in range(NR):
                x_ = xr[nrow % 2]
                nrow += 1
                f.dma("sp", x_[:], self.XD[e_ * CAP + r * 128: e_ * CAP + (r + 1) * 128, :], reads=[self.XD], writes=[x_])
                self.transposes(lambda c: x_[:, c * 128:(c + 1) * 128], 8, lambda c0, n: XT[:, c0:c0 + n, r * 128:(r + 1) * 128],
                                0, [x_], [V(XT, r)])
            for j in range(8):
                if j == 3:
                    f.dma("sp", WDs[:], self.w_down[l, e_].rearrange("(c p) n -> p c n", p=128), writes=[WDs])
                if j == 5:
                    for hf in range(2):
                        f.op("pool", lambda e: e.tensor_copy(wd[:, hf * 4:(hf + 1) * 4, :], WDs[:, hf * 4:(hf + 1) * 4, :]),
                             reads=[WDs], writes=[V(wd, hf)])
                wg = WG[nwg % 2]
                nwg += 1
                wgs = WGs[(nwg - 1) % 2]
                f.dma("sp", wgs[:, :, 0:128], self.w_gu[l, e_, :, j * 128:(j + 1) * 128].rearrange("(c p) n -> p c n", p=128),
                      writes=[V(wgs, 0)])
                f.dma("sp", wgs[:, :, 128:256],
                      self.w_gu[l, e_, :, DFF + j * 128: DFF + (j + 1) * 128].rearrange("(c p) n -> p c n", p=128),
                      writes=[V(wgs, 1)])
                f.op("pool", lambda e: e.tensor_copy(wg[:], wgs[:]), reads=[wgs], writes=[wg])
                for (s0, n) in slabs:
                    bg_, bl_ = 2 + (nps % 2) * 2, 3 + (nps % 2) * 2
                    nps += 1
                    for kc in range(8):
                        f.mm(PS[:, bg_ * 512: bg_ * 512 + n], wg[:, kc, 0:128], XT[:, kc, s0:s0 + n], kc == 0, kc == 7,
                             [wg, XT], self.bv(bg_))
                    for kc in range(8):
                        f.mm(PS[:, bl_ * 512: bl_ * 512 + n], wg[:, kc, 128:256], XT[:, kc, s0:s0 + n], kc == 0, kc == 7,
                             [wg, XT], self.bv(bl_))
                    f.op("dve", lambda e: e.tensor_scalar(tg[:, 0:n], PS[:, bg_ * 512: bg_ * 512 + n], BGall[:, j, e_:e_ + 1], LIM,
                                                          op0=ALU.add, op1=ALU.min), reads=self.bv(bg_) + [BGall], writes=[tg])
                    f.op("act", lambda e: e.activation(tsg[:, 0:n], tg[:, 0:n], AF.Sigmoid, scale=SW_ALPHA), reads=[tg], writes=[tsg])
                    f.op("dve", lambda e: e.tensor_scalar(tl[:, 0:n], PS[:, bl_ * 512: bl_ * 512 + n], BGall[:, 8 + j, e_:e_ + 1], LIM,
                                                          op0=ALU.add, op1=ALU.min), reads=self.bv(bl_) + [BGall], writes=[tl])
                    f.op("pool", lambda e: e.tensor_scalar(tl[:, 0:n], tl[:, 0:n], -LIM, 1.0, op0=ALU.max, op1=ALU.add),
                         reads=[tl], writes=[tl])
                    f.op("pool", lambda e: e.tensor_tensor(out=tg[:, 0:n], in0=tg[:, 0:n], in1=tsg[:, 0:n], op=ALU.mult),
                         reads=[tg, tsg], writes=[tg])
                    f.op("dve", lambda e: e.tensor_tensor(out=AT[:, j, s0:s0 + n], in0=tg[:, 0:n], in1=tl[:, 0:n], op=ALU.mult),
                         reads=[tg, tl], writes=[V(AT, j)])
            for r in range(NR):
                y_ = yr[r % 2]
                for sl in range(2):
                    for fc in range(8):
                        f.mm(self.bank(6 + sl), AT[:, fc, r * 128:(r + 1) * 128], wd[:, fc, sl * 512:(sl + 1) * 512],
                             fc == 0, fc == 7, [AT, wd], self.bv(6 + sl))
                    f.op("dve", lambda e: e.tensor_tensor(out=y_[:, sl * 512:(sl + 1) * 512], in0=self.bank(6 + sl),
                                                          in1=bd[:, sl * 512:(sl + 1) * 512], op=ALU.add),
                         reads=self.bv(6 + sl) + [bd], writes=[y_])
                f.dma("sp", self.YD[e_ * CAP + r * 128: e_ * CAP + (r + 1) * 128, :], y_[:], reads=[y_],
                      writes=[V(self.YD, (e_, r))])

    def phase_comb(self, l, last):
        f, NT, CAP = self.f, self.NT, self.CAP
        g2 = self.bcast_load("c_g2", self.ln2_g[l, :], D)
        b2 = self.bcast_load("c_b2", self.ln2_b[l, :], D)
        h2t = [f.sb("c_h2%d" % i, [128, D]) for i in range(2)]
        g4 = [f.sb("c_g4%d" % i, [128, 4]) for i in range(2)]
        s4 = [f.sb("c_s4%d" % i, [128, 4], I32) for i in range(2)]
        yk = [f.sb("c_yk%d" % i, [128, D]) for i in range(4)]
        acc = f.sb("c_acc", [128, D]); scr = f.sb("c_scr", [128, D])
        ot = [f.sb("c_o%d" % i, [128, D]) for i in range(2)]
        stats = f.sb("c_st", [128, 12]); mv = f.sb("c_mv", [128, 4])
        dst = self.out if last else self.H
        for i in range(NT):
            tok = slice(i * 128, (i + 1) * 128)
            h2_, g4_, s4_, o_ = h2t[i % 2], g4[i % 2], s4[i % 2], ot[i % 2]
            f.dma("sp", h2_[:], self.H2[tok, :], reads=[V(self.H2, i)], writes=[h2_])
            f.dma("sp", g4_[:], self.GATE4[tok, :], reads=[V(self.GATE4, i)], writes=[g4_])
            f.dma("sp", s4_[:], self.SLOT4[tok, :], reads=[V(self.SLOT4, i)], writes=[s4_])
            for k in range(4):
                y_ = yk[k]
                f.dma("pool", y_[:, :], self.YD[:, :], reads=[self.YD, s4_], writes=[y_],
                      indirect=dict(out_offset=None, in_offset=bass.IndirectOffsetOnAxis(ap=s4_[:, k:k + 1], axis=0),
                                    bounds_check=self.bc_reg(), oob_is_err=False))
                if k == 0:
                    f.op("dve", lambda e: e.tensor_scalar(acc[:], y_[:], g4_[:, 0:1], None, op0=ALU.mult),
                         reads=[y_, g4_], writes=[acc])
                else:
                    f.op("dve", lambda e: e.scalar_tensor_tensor(out=acc[:], in0=y_[:], scalar=g4_[:, k:k + 1], in1=acc[:],
                                                                 op0=ALU.mult, op1=ALU.add), reads=[y_, g4_, acc], writes=[acc])
            f.op("dve", lambda e: e.scalar_tensor_tensor(out=acc[:], in0=h2_[:], scalar=float(ALPHA), in1=acc[:],
                                                         op0=ALU.mult, op1=ALU.add), reads=[h2_, acc], writes=[acc])
            self.ln_tile(acc, o_, g2, b2, scr, stats, mv)
            f.dma("sp", dst[tok, :], o_[:], reads=[o_], writes=[V(dst, i)])


_WNAMES = ["w_in", "conv_w", "conv_b", "dt_bias", "a_log", "d_skip", "ssd_norm_g", "kv_norm_g", "w_uv",
           "w_mem_k", "w_mem_v", "w_out", "ln1_g", "ln1_b", "router_w", "router_b", "w_gu", "b_gu",
           "w_down", "b_down", "ln2_g", "ln2_b"]


def core_inputs(inp, b, S, L, consts=None):
    feed = {"x": np.ascontiguousarray(np.asarray(inp["x"])[b, :S]), "mem": np.ascontiguousarray(np.asarray(inp["mem"])[b]),
            "ln_in_g": np.asarray(inp["ln_in_g"]).reshape(1, D), "ln_in_b": np.asarray(inp["ln_in_b"]).reshape(1, D)}
    for k in _WNAMES:
        feed[k] = np.ascontiguousarray(np.asarray(inp[k])[:L])
    feed.update(consts if consts is not None else make_consts(S))
    return feed


SEQ = 8192
BATCH = 4
N_CORES = 8
CFG = dict(S=SEQ, depth=DEPTH_FULL, TS=512, CAP=1280, NIT=34, NSEL=256)


def kernel(**inputs):
    inputs = {k: np.asarray(v) for k, v in inputs.items()}
    mk = MK(CFG["S"], CFG["depth"], CFG["TS"], CFG["CAP"], CFG["NIT"], CFG["NSEL"])
    nc = mk.build()
    consts = make_consts(SEQ)
    shared = {k: np.ascontiguousarray(inputs[k]) for k in _WNAMES}
    shared["ln_in_g"] = inputs["ln_in_g"].reshape(1, D)
    shared["ln_in_b"] = inputs["ln_in_b"].reshape(1, D)
    shared.update(consts)
    in_maps = []
    for c in range(N_CORES):
        b = c % BATCH
        m = dict(shared)
        m["x"] = np.ascontiguousarray(inputs["x"][b])
        m["mem"] = np.ascontiguousarray(inputs["mem"][b])
        in_maps.append(m)
    res = run_bass_kernel_spmd(nc, in_maps, core_ids=list(range(N_CORES)))
    out = np.stack([np.asarray(res.results[b]["out"]) for b in range(BATCH)], axis=0)
    return out.astype(np.float32)
```

```python
import math
import numpy as np
from contextlib import ExitStack
import concourse.bass as bass
import concourse.mybir as mybir
from concourse.bass_utils import run_bass_kernel_spmd

F32 = mybir.dt.float32
BF16 = mybir.dt.bfloat16
I32 = mybir.dt.int32
U32 = mybir.dt.uint32
AF = mybir.ActivationFunctionType
ALU = mybir.AluOpType
AX = mybir.AxisListType

NDS = 24
NDS_SW = 8


class Buf:
    def __init__(self, t, name):
        self.t = t
        self.name = name
        self.w = {}
        self.r = {}

    def __getitem__(self, idx):
        return self.t[idx]


class V:
    def __init__(self, buf, tag=None):
        self.buf = buf
        self.tag = tag


def _norm(x):
    if isinstance(x, V):
        return x.buf, x.tag
    return x, None


class FW:
    def __init__(self, nc, stack, same_engine_sync=True):
        self.nc = nc
        self.stack = stack
        self.eng = {"pe": nc.tensor, "dve": nc.vector, "act": nc.scalar,
                    "pool": nc.gpsimd, "sp": nc.sync}
        self.sem = {k: stack.enter_context(nc.semaphore("s_" + k)) for k in self.eng}
        self.cnt = {k: 0 for k in self.eng}
        self.waited = {k: {} for k in self.eng}
        self.dsem = [stack.enter_context(nc.semaphore("d%d" % i)) for i in range(NDS)]
        self.dcnt = 0
        self.dsem_sw = [stack.enter_context(nc.semaphore("w%d" % i)) for i in range(NDS_SW)]
        self.dcnt_sw = 0
        self.ses = same_engine_sync
        self.ninstr = 0

    def sb(self, name, shape, dt=F32):
        self.nalloc = getattr(self, "nalloc", 0) + 1
        name = "%s_%d" % (name, self.nalloc)
        return Buf(self.stack.enter_context(self.nc.sbuf_tensor(name, list(shape), dt)), name)

    def ps(self, name, shape, dt=F32):
        return Buf(self.stack.enter_context(self.nc.psum_tensor(name, list(shape), dt)), name)

    def dram(self, name, shape, dt=F32, kind="Internal"):
        return Buf(self.nc.dram_tensor(name, list(shape), dt, kind=kind).ap(), name)

    def _deps(self, reads, writes):
        deps = []
        for x in reads:
            b, tag = _norm(x)
            for tg, tok in b.w.items():
                if tag is None or tg is None or tg == tag:
                    deps.append(tok)
        for x in writes:
            b, tag = _norm(x)
            for tg, tok in b.w.items():
                if tag is None or tg is None or tg == tag:
                    deps.append(tok)
            for tg, toks in b.r.items():
                if tag is None or tg is None or tg == tag:
                    deps.extend(toks)
        return deps

    def _wait(self, ek, deps, skip_same=False):
        e = self.eng[ek]
        need = {}
        for (sem, val, src) in deps:
            if src == ek and (skip_same or not self.ses):
                continue
            key = id(sem)
            if self.waited[ek].get(key, 0) >= val:
                continue
            if key not in need or need[key][1] < val:
                need[key] = (sem, val)
        for key, (sem, val) in need.items():
            e.wait_ge(sem, val)
            self.waited[ek][key] = val
            self.ninstr += 1

    def _record(self, tok, reads, writes):
        for x in reads:
            b, tag = _norm(x)
            lst = b.r.setdefault(tag, [])
            lst[:] = [t for t in lst if t[0] is not tok[0]] + [tok]
        for x in writes:
            b, tag = _norm(x)
            if tag is None:
                b.w = {None: tok}
                b.r = {}
            else:
                b.w[tag] = tok
                b.r[tag] = []

    def op(self, ek, fn, reads=(), writes=(), skip_same=False):
        self._wait(ek, self._deps(reads, writes), skip_same=skip_same)
        ins = fn(self.eng[ek])
        self.cnt[ek] += 1
        ins.then_inc(self.sem[ek], 1)
        tok = (self.sem[ek], self.cnt[ek], ek)
        self._record(tok, reads, writes)
        self.ninstr += 1
        return tok

    def dma(self, qk, out, in_, reads=(), writes=(), indirect=None, **kw):
        self._wait(qk, self._deps(reads, writes))
        if qk == "pool":
            i = self.dcnt_sw % NDS_SW
            rnd = self.dcnt_sw // NDS_SW
            self.dcnt_sw += 1
            sem = self.dsem_sw[i]
        else:
            i = self.dcnt % NDS
            rnd = self.dcnt // NDS
            self.dcnt += 1
            sem = self.dsem[i]
        if rnd > 0:
            key = id(sem)
            if self.waited[qk].get(key, 0) < 16 * rnd:
                self.eng[qk].wait_ge(sem, 16 * rnd)
                self.waited[qk][key] = 16 * rnd
        if indirect is None:
            self.eng[qk].dma_start(out=out, in_=in_, **kw).then_inc(sem, 16)
        else:
            self.eng[qk].indirect_dma_start(out=out, in_=in_, **indirect).then_inc(sem, 16)
        tok = (sem, 16 * (rnd + 1), "dma")
        self._record(tok, reads, writes)
        self.ninstr += 1
        return tok

    def barrier(self):
        toks = [(self.sem[k], self.cnt[k], k) for k in self.eng if self.cnt[k] > 0]
        for i in range(NDS):
            n = (self.dcnt - i + NDS - 1) // NDS
            if n > 0:
                toks.append((self.dsem[i], 16 * n, "dma"))
        for i in range(NDS_SW):
            n = (self.dcnt_sw - i + NDS_SW - 1) // NDS_SW
            if n > 0:
                toks.append((self.dsem_sw[i], 16 * n, "dma"))
        for ek in self.eng:
            self._wait(ek, [t for t in toks if t[2] != ek], skip_same=True)

    def mm(self, out, lhsT, rhs, start, stop, reads, writes):
        return self.op("pe", lambda e: e.matmul(out, lhsT, rhs, start=start, stop=stop,
                                                skip_group_check=True),
                       reads=reads, writes=writes, skip_same=True)

    def tr(self, out, in_, ident, reads, writes):
        return self.op("pe", lambda e: e.transpose(out, in_, ident), reads=reads, writes=writes,
                       skip_same=True)


D = 1024
DEPTH_FULL = 4
NH = 16
HP = 64
NG = 2
DST = 128
CONVW = 4
CONVD = D + 2 * NG * DST
DSA_H = 8
DLAT = 128
IDX_H = 4
IDX_D = 64
MEM_LEN = 256
MEM_H = 4
MEM_D = 128
DMIX = 2048
NE = 32
TOPK = 4
DFF = 1024
LIM = 7.0
SW_ALPHA = 1.702
ALPHA = (2 * DEPTH_FULL) ** 0.25
LN_EPS = 1e-5
RMS_EPS = 1e-6
O_Z, O_XBC, O_DT, O_QL, O_CKV, O_QI, O_KI, O_WI, O_QM, N_IN = (
    0, 1024, 2560, 2576, 3600, 3728, 3984, 4048, 4052, 4564)
NEG = -1.0e30
EPS_TIE = 2.0 ** -30


def make_consts(S):
    c = {}
    c["c_ident"] = np.eye(128, dtype=np.float32)
    k = np.arange(128)
    c["c_U"] = (k[:, None] <= k[None, :]).astype(np.float32)
    c["c_SL"] = (k[:, None] > k[None, :]).astype(np.float32)
    c["c_SU"] = (k[:, None] < k[None, :]).astype(np.float32)
    c["c_ones"] = np.ones((128, 128), np.float32)
    c["c_cbm"] = np.where(k[None, :] <= k[:, None], 0.0, NEG).astype(np.float32)
    c["c_epspos"] = np.broadcast_to((-EPS_TIE * np.arange(S, dtype=np.float64)).astype(np.float32)[None, :],
                                    (128, S)).copy()
    import ml_dtypes
    c["c_i4"] = np.tile(np.eye(128, dtype=np.float32), (1, 4)).astype(ml_dtypes.bfloat16)
    sl = (2.0 ** (-8.0 * np.arange(1, DSA_H + 1) / DSA_H)).astype(np.float32)
    c["c_sl128"] = np.repeat(sl * 128.0, 128)[None, :].astype(np.float32)
    onei = np.zeros((65, 128), np.float32)
    onei[0] = 1.0
    onei[32] = 1.0
    onei[64] = np.arange(128)
    c["c_onei"] = onei.astype(ml_dtypes.bfloat16)
    rbinit = np.zeros((65, 1024), np.float32)
    rbinit[64] = np.repeat(sl, 128)
    c["c_rbinit"] = rbinit.astype(ml_dtypes.bfloat16)
    c["c_negsl33"] = np.broadcast_to(np.repeat(-sl, 128)[None, :], (33, 1024)).astype(np.float32).copy()
    c["c_slrow"] = np.repeat(sl, 128)[None, :].astype(np.float32)
    c["c_pow2"] = np.broadcast_to((2.0 ** -(np.arange(64) + 1.0)).astype(np.float32)[None, :], (128, 64)).copy()
    c["c_eidx"] = np.broadcast_to(np.arange(NE, dtype=np.float32)[None, :], (128, NE)).copy()
    return c


class MK:
    def __init__(self, S, depth, TS, CAP, NIT, NSEL, stop_after=None):
        self.S, self.L, self.TS, self.CAP, self.NIT, self.NSEL = S, depth, TS, CAP, NIT, NSEL
        self.NT = S // 128
        self.NU = S // TS
        self.TPU = TS // 128
        self.stop_after = stop_after
        self.nc = bass.Bass("TRN2", target_bir_lowering=False)

    def din(self, name, shape, dt=F32):
        return Buf(self.nc.dram_tensor(name, list(shape), dt, kind="ExternalInput").ap(), name)

    def build(self):
        nc, S, L, NT, CAP = self.nc, self.S, self.L, self.NT, self.CAP
        din = self.din
        self.x_in = din("x", [S, D]); self.mem_in = din("mem", [MEM_LEN, D])
        self.ln_in_g = din("ln_in_g", [1, D]); self.ln_in_b = din("ln_in_b", [1, D])
        self.w_in = din("w_in", [L, D, N_IN])
        self.conv_w = din("conv_w", [L, CONVW, CONVD]); self.conv_b = din("conv_b", [L, CONVD])
        self.dt_bias = din("dt_bias", [L, NH]); self.a_log = din("a_log", [L, NH]); self.d_skip = din("d_skip", [L, NH])
        self.ssd_norm_g = din("ssd_norm_g", [L, D]); self.kv_norm_g = din("kv_norm_g", [L, DLAT])
        self.w_uv = din("w_uv", [L, DSA_H, DLAT, 64])
        self.w_mem_k = din("w_mem_k", [L, D, 512]); self.w_mem_v = din("w_mem_v", [L, D, 512])
        self.w_out = din("w_out", [L, DMIX, D])
        self.ln1_g = din("ln1_g", [L, D]); self.ln1_b = din("ln1_b", [L, D])
        self.router_w = din("router_w", [L, D, NE]); self.router_b = din("router_b", [L, NE])
        self.w_gu = din("w_gu", [L, NE, D, 2 * DFF]); self.b_gu = din("b_gu", [L, NE, 2 * DFF])
        self.w_down = din("w_down", [L, NE, DFF, D]); self.b_down = din("b_down", [L, NE, D])
        self.ln2_g = din("ln2_g", [L, D]); self.ln2_b = din("ln2_b", [L, D])
        self.cin = {}
        for k, v in make_consts(S).items():
            self.cin[k] = din(k, list(v.shape), BF16 if v.dtype.itemsize == 2 else F32)
        self.out = Buf(nc.dram_tensor("out", [S, D], F32, kind="ExternalOutput").ap(), "out")

        with ExitStack() as st:
            f = self.f = FW(nc, st)
            self.H = f.dram("H", [S, D]); self.H2 = f.dram("H2", [S, D])
            self.Z = f.dram("Z", [S, D]); self.DTs = f.dram("DTs", [S, NH])
            self.X = f.dram("X", [S, D]); self.BTM = f.dram("BTM", [S, 256])
            self.BT = f.dram("BT", [128, 2, S]); self.CTs = f.dram("CTs", [128, 2, S])
            self.QL = f.dram("QL", [128, NT, 1024], BF16); self.QI = f.dram("QI", [128, 2, S])
            self.KI = f.dram("KI", [128, S]); self.WI = f.dram("WI", [S, 4])
            self.CV = f.dram("CV", [S, 129], BF16); self.CT = f.dram("CT", [128, S], BF16)
            self.QM = f.dram("QM", [128, 4, S])
            self.MIX = f.dram("MIX", [S, DMIX])
            self.GATE4 = f.dram("GATE4", [S, 4]); self.SLOT4 = f.dram("SLOT4", [S, 4], I32)
            self.XD = f.dram("XD", [NE * CAP, D]); self.YD = f.dram("YD", [NE * CAP, D])

            self.ident = f.sb("ident", [128, 128]); self.cU = f.sb("cU", [128, 128])
            self.cSL = f.sb("cSL", [128, 128]); self.cSU = f.sb("cSU", [128, 128])
            self.ones = f.sb("ones", [128, 128])
            for sbt, nm in ((self.ident, "c_ident"), (self.cU, "c_U"), (self.cSL, "c_SL"),
                            (self.cSU, "c_SU"), (self.ones, "c_ones")):
                f.dma("sp", sbt[:], self.cin[nm][:], writes=[sbt])
            self.epsln = f.sb("epsln", [128, 4])
            f.op("dve", lambda e: e.memset(self.epsln[:, 0:1], LN_EPS), writes=[self.epsln])
            f.op("dve", lambda e: e.memset(self.epsln[:, 1:2], RMS_EPS), reads=[self.epsln], writes=[self.epsln])
            f.op("dve", lambda e: e.memset(self.epsln[:, 2:3], 1.0), reads=[self.epsln], writes=[self.epsln])
            self.PS = f.ps("PS", [128, 4096])

            stages = [("ln_in", lambda: self.phase_ln_in())]
            for l in range(L):
                stages += [("a1_%d" % l, lambda l=l: self.phase_a1(l)),
                           ("ssd_%d" % l, lambda l=l: self.phase_ssd(l)),
                           ("a2_%d" % l, lambda l=l: self.phase_a2(l)),
                           ("dsa_%d" % l, lambda l=l: self.phase_dsa(l)),
                           ("mem_%d" % l, lambda l=l: self.phase_mem(l)),
                           ("outp_%d" % l, lambda l=l: self.phase_outp(l)),
                           ("moe_%d" % l, lambda l=l: self.phase_moe(l)),
                           ("comb_%d" % l, lambda l=l: self.phase_comb(l, last=(l == L - 1)))]
            for name, fn in stages:
                with ExitStack() as ph:
                    old = f.stack
                    f.stack = ph
                    fn()
                    f.barrier()
                    f.stack = old
                if self.stop_after == name:
                    break
        return nc

    def bc_reg(self):
        if getattr(self, "_bc_reg", None) is None:
            self._bc_reg = self.nc.gpsimd.to_reg(NE * self.CAP - 1)
        return self._bc_reg

    def bank(self, i, n=1):
        return self.PS[:, i * 512:(i + n) * 512]

    def bv(self, i, n=1):
        return [V(self.PS, j) for j in range(i, i + n)]

    def ln_tile(self, src, dst, g_bc, b_bc, scr, stats, mv):
        f, epsln = self.f, self.epsln
        f.op("dve", lambda e: e.bn_stats(stats[:, 0:6], src[:, 0:512]), reads=[src], writes=[stats])
        f.op("dve", lambda e: e.bn_stats(stats[:, 6:12], src[:, 512:1024]), reads=[src, stats], writes=[stats])
        f.op("dve", lambda e: e.bn_aggr(mv[:, 0:2], stats[:, 0:12]), reads=[stats], writes=[mv])
        f.op("act", lambda e: e.activation(mv[:, 2:3], mv[:, 1:2], AF.Sqrt, bias=epsln[:, 0:1], scale=1.0),
             reads=[mv, epsln], writes=[mv])
        f.op("dve", lambda e: e.reciprocal(mv[:, 3:4], mv[:, 2:3]), reads=[mv], writes=[mv])
        f.op("dve", lambda e: e.tensor_scalar(scr[:], src[:], mv[:, 0:1], mv[:, 3:4],
                                              op0=ALU.subtract, op1=ALU.mult), reads=[src, mv], writes=[scr])
        f.op("dve", lambda e: e.tensor_tensor(out=scr[:], in0=scr[:], in1=g_bc[:], op=ALU.mult),
             reads=[scr, g_bc], writes=[scr])
        f.op("dve", lambda e: e.tensor_tensor(out=dst[:], in0=scr[:], in1=b_bc[:], op=ALU.add),
             reads=[scr, b_bc], writes=[dst])

    def transposes(self, src_fn, nchunks, dst_fn, pbank, rd, wr, evac="act", scale=None):
        f, PS = self.f, self.PS
        for gi, c0 in enumerate(range(0, nchunks, 4)):
            n = min(4, nchunks - c0)
            bk = pbank + gi % 2
            for c in range(c0, c0 + n):
                f.tr(PS[:, bk * 512 + (c - c0) * 128: bk * 512 + (c - c0 + 1) * 128],
                     src_fn(c), self.ident[:], reads=list(rd) + [self.ident], writes=self.bv(bk))
            dst = dst_fn(c0, n)
            srcp = PS[:, bk * 512: bk * 512 + n * 128].rearrange("p (c t) -> p c t", t=128)
            if scale is not None:
                f.op("act", lambda e: e.activation(dst, srcp, AF.Copy, scale=scale), reads=self.bv(bk), writes=wr)
            elif evac == "act":
                f.op("act", lambda e: e.copy(dst, srcp), reads=self.bv(bk), writes=wr)
            else:
                f.op("dve", lambda e: e.tensor_copy(dst, srcp), reads=self.bv(bk), writes=wr)

    def bcast_load(self, name, src_row_ap, n):
        t = self.f.sb(name, [128, n])
        self.f.dma("sp", t[:], src_row_ap.partition_broadcast(128), writes=[t])
        return t

    def phase_ln_in(self):
        f, NT = self.f, self.NT
        g_bc = self.bcast_load("p0_g", self.ln_in_g[0, :], D)
        b_bc = self.bcast_load("p0_b", self.ln_in_b[0, :], D)
        xt = [f.sb("p0_x%d" % i, [128, D]) for i in range(2)]
        ot = [f.sb("p0_o%d" % i, [128, D]) for i in range(2)]
        scr = f.sb("p0_scr", [128, D]); stats = f.sb("p0_st", [128, 12]); mv = f.sb("p0_mv", [128, 4])
        for i in range(NT):
            a, o = xt[i % 2], ot[i % 2]
            f.dma("sp", a[:], self.x_in[i * 128:(i + 1) * 128, :], writes=[a])
            self.ln_tile(a, o, g_bc, b_bc, scr, stats, mv)
            f.dma("sp", self.H[i * 128:(i + 1) * 128, :], o[:], reads=[o], writes=[V(self.H, i)])

    def load_hT(self, hT, u, htiles, hTb=None):
        f = self.f
        for i in range(self.TPU):
            ti = u * self.TPU + i
            a = htiles[ti % 2]
            f.dma("sp", a[:], self.H[ti * 128:(ti + 1) * 128, :], reads=[V(self.H, ti)], writes=[a])
            self.transposes(lambda c: a[:, c * 128:(c + 1) * 128], 8,
                            lambda c0, n: hT[:, c0:c0 + n, i * 128:(i + 1) * 128], 0, [a], [hT])
            if hTb is not None:
                f.op("pool", lambda e: e.tensor_copy(hTb[:, :, i * 128:(i + 1) * 128], hT[:, :, i * 128:(i + 1) * 128]),
                     reads=[hT], writes=[hTb])

    def phase_a1(self, l):
        f, TS, TPU, NU = self.f, self.TS, self.TPU, self.NU
        NW = O_QL
        W1 = f.sb("a1_W", [128, 8, NW], BF16)
        W1s = [f.sb("a1_Ws%d" % i, [128, NW]) for i in range(2)]
        for kc in range(8):
            ws = W1s[kc % 2]
            f.dma("sp", ws[:], self.w_in[l, kc * 128:(kc + 1) * 128, 0:NW], writes=[ws])
            f.op("pool", lambda e: e.tensor_copy(W1[:, kc, :], ws[:]), reads=[ws], writes=[V(W1, kc)])
        CW = f.sb("a1_cw", [128, 12, 4]); CBs = f.sb("a1_cb", [128, 12])
        for j in range(4):
            f.dma("sp", CW[:, :, j], self.conv_w[l, j, :].rearrange("(c p) -> p c", p=128), writes=[V(CW, j)],
                  allow_slow_non_contiguous=True)
        f.dma("sp", CBs[:], self.conv_b[l, :].rearrange("(c p) -> p c", p=128), writes=[CBs],
              allow_slow_non_contiguous=True)
        dtb = self.bcast_load("a1_dtb", self.dt_bias[l, :], NH)
        hT = f.sb("a1_hT", [128, 8, TS], BF16)
        htiles = [f.sb("a1_h%d" % i, [128, D]) for i in range(2)]
        xpre = f.sb("a1_xpre", [128, 12, TS + 3])
        xc = f.sb("a1_xc", [128, 12, TS])
        acc = f.sb("a1_acc", [128, TS])
        zt = [f.sb("a1_z%d" % i, [128, D]) for i in range(2)]
        dtt = f.sb("a1_dt", [128, NH]); dte = f.sb("a1_dte", [128, NH])
        xtm = [f.sb("a1_xtm%d" % i, [128, D + 256]) for i in range(2)]
        f.op("dve", lambda e: e.memset(xpre[:, :, 0:3], 0.0), writes=[xpre])
        for u in range(NU):
            self.load_hT(hT, u, htiles)
            for i in range(TPU):
                ti = u * TPU + i
                z = zt[ti % 2]
                for sl in range(2):
                    bk = 2 + sl
                    for kc in range(8):
                        f.mm(self.bank(bk), hT[:, kc, i * 128:(i + 1) * 128], W1[:, kc, sl * 512:(sl + 1) * 512],
                             kc == 0, kc == 7, [hT, V(W1, kc)], self.bv(bk))
                    f.op("act", lambda e: e.copy(z[:, sl * 512:(sl + 1) * 512], self.bank(bk)),
                         reads=self.bv(bk), writes=[z])
                f.dma("sp", self.Z[ti * 128:(ti + 1) * 128, :], z[:], reads=[z], writes=[V(self.Z, ti)])
                for kc in range(8):
                    f.mm(self.PS[:, 4 * 512:4 * 512 + NH], hT[:, kc, i * 128:(i + 1) * 128],
                         W1[:, kc, O_DT:O_DT + NH], kc == 0, kc == 7, [hT, V(W1, kc)], self.bv(4))
                f.op("dve", lambda e: e.tensor_tensor(out=dte[:], in0=self.PS[:, 4 * 512:4 * 512 + NH], in1=dtb[:],
                                                      op=ALU.add), reads=self.bv(4) + [dtb], writes=[dte])
                f.op("act", lambda e: e.activation(dte[:], dte[:], AF.Exp), reads=[dte], writes=[dte])
                f.op("act", lambda e: e.activation(dtt[:], dte[:], AF.Ln, bias=self.epsln[:, 2:3], scale=1.0),
                     reads=[dte, self.epsln], writes=[dtt])
                f.dma("sp", self.DTs[ti * 128:(ti + 1) * 128, :], dtt[:], reads=[dtt], writes=[V(self.DTs, ti)])
            for cc in range(12):
                bk = 5 + cc % 2
                for kc in range(8):
                    f.mm(self.PS[:, bk * 512: bk * 512 + TS], W1[:, kc, O_XBC + cc * 128: O_XBC + (cc + 1) * 128],
                         hT[:, kc, :], kc == 0, kc == 7, [hT, V(W1, kc)], self.bv(bk))
                f.op("act", lambda e: e.copy(xpre[:, cc, 3:3 + TS], self.PS[:, bk * 512: bk * 512 + TS]),
                     reads=self.bv(bk), writes=[V(xpre, cc)])
                f.op("dve", lambda e: e.tensor_scalar(acc[:], xpre[:, cc, 0:TS], CW[:, cc, 0:1], CBs[:, cc:cc + 1],
                                                      op0=ALU.mult, op1=ALU.add),
                     reads=[V(xpre, cc), CW, CBs], writes=[acc])
                for j in range(1, 4):
                    f.op("dve", lambda e: e.scalar_tensor_tensor(out=acc[:], in0=xpre[:, cc, j:j + TS],
                                                                 scalar=CW[:, cc, j:j + 1], in1=acc[:],
                                                                 op0=ALU.mult, op1=ALU.add),
                         reads=[V(xpre, cc), CW, acc], writes=[acc])
                f.op("act", lambda e: e.activation(xc[:, cc, :], acc[:], AF.Silu), reads=[acc], writes=[V(xc, cc)])
                f.op("dve", lambda e: e.tensor_copy(xpre[:, cc, 0:3], xpre[:, cc, TS:TS + 3]),
                     reads=[V(xpre, cc)], writes=[V(xpre, cc)])
            f.dma("sp", self.BT[:, :, u * TS:(u + 1) * TS], xc[:, 8:10, :], reads=[V(xc, 8), V(xc, 9)],
                  writes=[V(self.BT, u)])
            f.dma("sp", self.CTs[:, :, u * TS:(u + 1) * TS], xc[:, 10:12, :], reads=[V(xc, 10), V(xc, 11)],
                  writes=[V(self.CTs, u)])
            for i in range(TPU):
                ti = u * TPU + i
                xm = xtm[ti % 2]
                self.transposes(lambda c: xc[:, c, i * 128:(i + 1) * 128], 10,
                                lambda c0, n: xm[:, c0 * 128:(c0 + n) * 128].rearrange("p (c t) -> p c t", t=128),
                                0, [xc], [xm], evac="dve")
                f.dma("sp", self.X[ti * 128:(ti + 1) * 128, :], xm[:, 0:D], reads=[xm], writes=[V(self.X, ti)])
                f.dma("sp", self.BTM[ti * 128:(ti + 1) * 128, :], xm[:, D:D + 256], reads=[xm],
                      writes=[V(self.BTM, ti)])

    def phase_ssd(self, l):
        f, NT = self.f, self.NT
        PS = self.PS
        cU, cSL, ones = self.cU, self.cSL, self.ones
        Abc = self.bcast_load("s_A", self.a_log[l, :], NH)
        f.op("act", lambda e: e.activation(Abc[:], Abc[:], AF.Exp), reads=[Abc], writes=[Abc])
        f.op("dve", lambda e: e.tensor_scalar(Abc[:], Abc[:], -1.0, None, op0=ALU.mult), reads=[Abc], writes=[Abc])
        Dbc = self.bcast_load("s_D", self.d_skip[l, :], NH)
        NGb = self.bcast_load("s_ng", self.ssd_norm_g[l, :], D)
        ST = f.sb("s_ST", [128, 2, 512])
        f.op("dve", lambda e: e.memset(ST[:], 0.0), writes=[ST])
        xt = [f.sb("s_x%d" % i, [128, NH, HP]) for i in range(2)]
        zt = [f.sb("s_z%d" % i, [128, D]) for i in range(2)]
        dtt = [f.sb("s_dt%d" % i, [128, NH]) for i in range(2)]
        bct = [f.sb("s_bc%d" % i, [128, 4, 128]) for i in range(2)]
        btm = [f.sb("s_bm%d" % i, [128, 256]) for i in range(2)]
        a = f.sb("s_a", [128, NH]); acum = f.sb("s_acum", [128, NH]); eacum = f.sb("s_eacum", [128, NH])
        alast = f.sb("s_alast", [128, NH]); ealast = f.sb("s_ealast", [128, NH]); dend = f.sb("s_dend", [128, NH])
        xdt = f.sb("s_xdt", [128, NH, HP]); xdec = f.sb("s_xdec", [128, NH, HP])
        mCB = f.sb("s_mcb", [128, 128])
        Wa = [f.sb("s_wa%d" % i, [128, 4, 128]) for i in range(2)]
        LT = [f.sb("s_lt%d" % i, [128, 4, 128]) for i in range(2)]
        G = [f.sb("s_g%d" % i, [128, 4, 128]) for i in range(2)]
        y = f.sb("s_y", [128, NH, HP]); yo = f.sb("s_yo", [128, 8, HP])
        sz = f.sb("s_sz", [128, D]); ss = f.sb("s_ss", [128, 4]); junk = f.sb("s_junk", [128, 512])
        yout = [f.sb("s_yout%d" % i, [128, D]) for i in range(2)]
        for c in range(NT):
            x_, z_, d_, bc_, bm_ = xt[c % 2], zt[c % 2], dtt[c % 2], bct[c % 2], btm[c % 2]
            tok = slice(c * 128, (c + 1) * 128)
            f.dma("sp", x_[:].rearrange("p h d -> p (h d)"), self.X[tok, :], reads=[V(self.X, c)], writes=[x_])
            f.dma("sp", z_[:], self.Z[tok, :], reads=[V(self.Z, c)], writes=[z_])
            f.dma("sp", d_[:], self.DTs[tok, :], reads=[V(self.DTs, c)], writes=[d_])
            f.dma("sp", bc_[:, 0:2, :], self.BT[:, :, tok], reads=[self.BT], writes=[V(bc_, 0)])
            f.dma("sp", bc_[:, 2:4, :], self.CTs[:, :, tok], reads=[self.CTs], writes=[V(bc_, 1)])
            f.dma("sp", bm_[:], self.BTM[tok, :], reads=[V(self.BTM, c)], writes=[bm_])
            f.op("dve", lambda e: e.tensor_tensor(out=a[:], in0=d_[:], in1=Abc[:], op=ALU.mult),
                 reads=[d_, Abc], writes=[a])
            f.mm(PS[:, 0:NH], cU[:], a[:], True, True, [cU, a], self.bv(0))
            f.mm(PS[:, 512:512 + NH], ones[:], a[:], True, True, [ones, a], self.bv(1))
            f.op("dve", lambda e: e.tensor_copy(acum[:], PS[:, 0:NH]), reads=self.bv(0), writes=[acum])
            f.op("act", lambda e: e.activation(eacum[:], PS[:, 0:NH], AF.Exp), reads=self.bv(0), writes=[eacum])
            f.op("dve", lambda e: e.tensor_tensor(out=dend[:], in0=PS[:, 512:512 + NH], in1=acum[:], op=ALU.subtract),
                 reads=self.bv(1) + [acum], writes=[dend])
            f.op("act", lambda e: e.activation(dend[:], dend[:], AF.Exp), reads=[dend], writes=[dend])
            f.op("act", lambda e: e.activation(ealast[:], PS[:, 512:512 + NH], AF.Exp), reads=self.bv(1), writes=[ealast])
            f.op("dve", lambda e: e.tensor_tensor(out=xdt[:], in0=x_[:], in1=d_[:].unsqueeze(2).to_broadcast([128, NH, HP]),
                                                  op=ALU.mult), reads=[x_, d_], writes=[xdt])
            f.op("dve", lambda e: e.tensor_tensor(out=xdec[:], in0=xdt[:],
                                                  in1=dend[:].unsqueeze(2).to_broadcast([128, NH, HP]), op=ALU.mult),
                 reads=[xdt, dend], writes=[xdec])
            for g in range(2):
                f.mm(PS[:, 2 * 512:2 * 512 + 128], bc_[:, g, :], bc_[:, 2 + g, :], True, True, [bc_], self.bv(2))
                f.op("dve", lambda e: e.tensor_tensor(out=mCB[:], in0=PS[:, 2 * 512:2 * 512 + 128], in1=cU[:], op=ALU.mult),
                     reads=self.bv(2) + [cU], writes=[mCB])
                f.mm(self.bank(3), bc_[:, 2 + g, :], ST[:, g, :], True, True, [bc_, V(ST, g)], self.bv(3))
                for sg in range(2):
                    h0 = g * 8 + sg * 4
                    wa, lt, gg = Wa[sg], LT[sg], G[sg]
                    f.op("dve", lambda e: e.tensor_tensor(out=wa[:], in0=cSL[:].unsqueeze(1).to_broadcast([128, 4, 128]),
                                                          in1=a[:, h0:h0 + 4].unsqueeze(2).to_broadcast([128, 4, 128]),
                                                          op=ALU.mult), reads=[cSL, a], writes=[wa])
                    bk = 4 + sg
                    for hh in range(4):
                        f.mm(PS[:, bk * 512 + hh * 128: bk * 512 + (hh + 1) * 128], wa[:, hh, :], cU[:], True, True,
                             [wa, cU], self.bv(bk))
                    f.op("act", lambda e: e.activation(lt[:].rearrange("p h l -> p (h l)"), self.bank(bk), AF.Exp),
                         reads=self.bv(bk), writes=[lt])
                    f.op("dve", lambda e: e.tensor_tensor(out=gg[:], in0=lt[:],
                                                          in1=mCB[:].unsqueeze(1).to_broadcast([128, 4, 128]),
                                                          op=ALU.mult), reads=[lt, mCB], writes=[gg])
                    for hh in range(4):
                        h = h0 + hh
                        hl = sg * 4 + hh
                        f.mm(PS[:, 6 * 512 + hl * 64: 6 * 512 + (hl + 1) * 64], gg[:, hh, :], xdt[:, h, :], True, True,
                             [gg, xdt], self.bv(6))
                f.op("dve", lambda e: e.tensor_tensor(
                    out=yo[:], in0=self.bank(3).rearrange("p (h d) -> p h d", d=HP),
                    in1=eacum[:, g * 8:(g + 1) * 8].unsqueeze(2).to_broadcast([128, 8, HP]), op=ALU.mult),
                    reads=self.bv(3) + [eacum], writes=[yo])
                f.op("dve", lambda e: e.tensor_tensor(
                    out=y[:, g * 8:(g + 1) * 8, :], in0=self.bank(6).rearrange("p (h d) -> p h d", d=HP),
                    in1=yo[:], op=ALU.add), reads=self.bv(6) + [yo], writes=[V(y, g)])
                f.mm(self.bank(7), bm_[:, g * 128:(g + 1) * 128], xdec[:, g * 8:(g + 1) * 8, :].rearrange("p h d -> p (h d)"),
                     True, True, [bm_, xdec], self.bv(7))
                f.op("dve", lambda e: e.tensor_tensor(
                    out=ST[:, g, :].rearrange("p (h d) -> p h d", d=HP),
                    in0=ST[:, g, :].rearrange("p (h d) -> p h d", d=HP),
                    in1=ealast[:, g * 8:(g + 1) * 8].unsqueeze(2).to_broadcast([128, 8, HP]), op=ALU.mult),
                    reads=[V(ST, g), ealast], writes=[V(ST, g)])
                f.op("dve", lambda e: e.tensor_tensor(out=ST[:, g, :], in0=ST[:, g, :], in1=self.bank(7), op=ALU.add),
                     reads=[V(ST, g)] + self.bv(7), writes=[V(ST, g)])
            f.op("dve", lambda e: e.tensor_tensor(out=xdt[:], in0=x_[:],
                                                  in1=Dbc[:].unsqueeze(2).to_broadcast([128, NH, HP]), op=ALU.mult),
                 reads=[x_, Dbc], writes=[xdt])
            f.op("dve", lambda e: e.tensor_tensor(out=y[:], in0=y[:], in1=xdt[:], op=ALU.add),
                 reads=[y, xdt], writes=[y])
            f.op("act", lambda e: e.activation(sz[:], z_[:], AF.Silu), reads=[z_], writes=[sz])
            yf = y[:].rearrange("p h d -> p (h d)")
            f.op("dve", lambda e: e.tensor_tensor(out=sz[:], in0=sz[:], in1=yf, op=ALU.mult), reads=[sz, y], writes=[sz])
            for g in range(2):
                f.op("act", lambda e: e.activation(junk[:], sz[:, g * 512:(g + 1) * 512], AF.Square,
                                                   accum_out=ss[:, g:g + 1]), reads=[sz], writes=[junk, V(ss, g)])
            f.op("act", lambda e: e.activation(ss[:, 2:4], ss[:, 0:2], AF.Sqrt, bias=self.epsln[:, 1:2], scale=1.0 / 512),
                 reads=[ss, self.epsln], writes=[ss])
            f.op("dve", lambda e: e.reciprocal(ss[:, 2:4], ss[:, 2:4]), reads=[ss], writes=[ss])
            yo_ = yout[c % 2]
            for g in range(2):
                f.op("dve", lambda e: e.scalar_tensor_tensor(out=yo_[:, g * 512:(g + 1) * 512], in0=sz[:, g * 512:(g + 1) * 512],
                                                             scalar=ss[:, 2 + g:3 + g], in1=NGb[:, g * 512:(g + 1) * 512],
                                                             op0=ALU.mult, op1=ALU.mult),
                     reads=[sz, ss, NGb], writes=[yo_])
            f.dma("sp", self.MIX[tok, 0:D], yo_[:], reads=[yo_], writes=[V(self.MIX, ("ssd", c))])

    def phase_a2(self, l):
        f, TS, TPU, NU = self.f, self.TS, self.TPU, self.NU
        PS = self.PS
        NW = N_IN - O_QL
        oQL, oCKV, oQI, oKI, oWI, oQM = 0, O_CKV - O_QL, O_QI - O_QL, O_KI - O_QL, O_WI - O_QL, O_QM - O_QL
        W2 = f.sb("a2_W", [128, 8, NW])
        WK = f.sb("a2_WK", [128, 8, 128])
        for kc in range(8):
            f.dma("sp", W2[:, kc, :], self.w_in[l, kc * 128:(kc + 1) * 128, O_QL:N_IN], writes=[V(W2, kc)])
            for hf in range(2):
                f.dma("sp", WK[:, kc, hf * 64:(hf + 1) * 64], self.w_in[l, kc * 128:(kc + 1) * 128, O_KI:O_KI + 64],
                      writes=[V(WK, (kc, hf))])
        W2b = f.sb("a2_Wb", [128, 8, NW], BF16)
        for kc in range(8):
            f.op("pool", lambda e: e.tensor_copy(W2b[:, kc, :], W2[:, kc, :]), reads=[V(W2, kc)], writes=[V(W2b, kc)])
        KVG = self.bcast_load("a2_kvg", self.kv_norm_g[l, :], DLAT)
        hT = f.sb("a2_hT", [128, 8, TS])
        hTb = f.sb("a2_hTb", [128, 8, TS], BF16)
        htiles = [f.sb("a2_h%d" % i, [128, D]) for i in range(2)]
        qst = f.sb("a2_qst", [128, TPU, 8, 128], BF16)
        qist = f.sb("a2_qist", [128, 2, TS]); kist = f.sb("a2_kist", [128, TS])
        qmst = f.sb("a2_qmst", [128, 4, TS]); ctst = f.sb("a2_ctst", [128, TS], BF16)
        cvb = [f.sb("a2_cvb%d" % i, [128, 129], BF16) for i in range(2)]
        cv = [f.sb("a2_cv%d" % i, [128, 129]) for i in range(2)]
        wit = [f.sb("a2_wi%d" % i, [128, 4]) for i in range(2)]
        ss = f.sb("a2_ss", [128, 4]); junk = f.sb("a2_junk", [128, 128])
        for c_ in cv:
            f.op("dve", lambda e: e.memset(c_[:, 128:129], 1.0), writes=[V(c_, "one")])
        sc_lat = DLAT ** -0.5
        sc_mem = MEM_D ** -0.5
        nb = 0
        for u in range(NU):
            self.load_hT(hT, u, htiles, hTb)
            tsl = slice(u * TS, (u + 1) * TS)

            def fm_chunk(wcols, dst, scale, wr, lowp=False):
                nonlocal nb
                bk = 2 + nb % 2
                nb += 1
                hsrc = hTb if lowp else hT
                for kc in range(8):
                    f.mm(PS[:, bk * 512: bk * 512 + TS], wcols(kc), hsrc[:, kc, :], kc == 0, kc == 7,
                         [hsrc, W2, W2b, WK], self.bv(bk))
                src = PS[:, bk * 512: bk * 512 + TS]
                if len(dst.shape) == 3:
                    src = src.rearrange("p (i t) -> p i t", t=128)
                if scale is None:
                    f.op("act", lambda e: e.copy(dst, src), reads=self.bv(bk), writes=wr)
                else:
                    f.op("act", lambda e: e.activation(dst, src, AF.Copy, scale=scale), reads=self.bv(bk), writes=wr)

            for h in range(8):
                fm_chunk(lambda kc: W2b[:, kc, oQL + h * 128: oQL + (h + 1) * 128],
                         qst[:, :, h, :], sc_lat, [V(qst, h)], lowp=True)
            f.dma("sp", self.QL[:, u * TPU:(u + 1) * TPU, :], qst[:].rearrange("p i h t -> p i (h t)"),
                  reads=[qst], writes=[V(self.QL, u)])
            for j in range(2):
                fm_chunk(lambda kc: W2[:, kc, oQI + j * 128: oQI + (j + 1) * 128], qist[:, j, :], None, [V(qist, j)])
            f.dma("sp", self.QI[:, :, tsl], qist[:], reads=[qist], writes=[V(self.QI, u)])
            fm_chunk(lambda kc: WK[:, kc, :], kist[:], None, [kist])
            f.dma("sp", self.KI[:, tsl], kist[:], reads=[kist], writes=[V(self.KI, u)])
            for h in range(4):
                fm_chunk(lambda kc: W2b[:, kc, oQM + h * 128: oQM + (h + 1) * 128], qmst[:, h, :], sc_mem, [V(qmst, h)], lowp=True)
            f.dma("sp", self.QM[:, :, tsl], qmst[:], reads=[qmst], writes=[V(self.QM, u)])
            for i in range(TPU):
                ti = u * TPU + i
                tok = slice(ti * 128, (ti + 1) * 128)
                c_, w_ = cv[ti % 2], wit[ti % 2]
                for kc in range(8):
                    f.mm(PS[:, 4 * 512: 4 * 512 + 128], hTb[:, kc, i * 128:(i + 1) * 128], W2b[:, kc, oCKV:oCKV + 128],
                         kc == 0, kc == 7, [hTb, W2b], self.bv(4))
                for kc in range(8):
                    f.mm(PS[:, 5 * 512: 5 * 512 + 4], hT[:, kc, i * 128:(i + 1) * 128], W2[:, kc, oWI:oWI + 4],
                         kc == 0, kc == 7, [hT, W2], self.bv(5))
                f.op("act", lambda e: e.copy(w_[:], PS[:, 5 * 512: 5 * 512 + 4]), reads=self.bv(5), writes=[w_])
                f.dma("sp", self.WI[tok, :], w_[:], reads=[w_], writes=[V(self.WI, ti)])
                f.op("act", lambda e: e.activation(junk[:], PS[:, 4 * 512: 4 * 512 + 128], AF.Square, accum_out=ss[:, 0:1]),
                     reads=self.bv(4), writes=[junk, ss])
                f.op("act", lambda e: e.activation(ss[:, 1:2], ss[:, 0:1], AF.Sqrt, bias=self.epsln[:, 1:2], scale=1.0 / DLAT),
                     reads=[ss, self.epsln], writes=[ss])
                f.op("dve", lambda e: e.reciprocal(ss[:, 2:3], ss[:, 1:2]), reads=[ss], writes=[ss])
                f.op("dve", lambda e: e.scalar_tensor_tensor(out=c_[:, 0:128], in0=PS[:, 4 * 512: 4 * 512 + 128],
                                                             scalar=ss[:, 2:3], in1=KVG[:], op0=ALU.mult, op1=ALU.mult),
                     reads=self.bv(4) + [ss, KVG], writes=[V(c_, "c")])
                cb_ = cvb[ti % 2]
                f.op("act", lambda e: e.copy(cb_[:], c_[:]), reads=[c_], writes=[cb_])
                f.dma("sp", self.CV[tok, :], cb_[:], reads=[cb_], writes=[V(self.CV, ti)])
                f.tr(PS[:, 6 * 512: 6 * 512 + 128], c_[:, 0:128], self.ident[:], [V(c_, "c"), self.ident], self.bv(6))
                f.op("act", lambda e: e.copy(ctst[:, i * 128:(i + 1) * 128], PS[:, 6 * 512: 6 * 512 + 128]),
                     reads=self.bv(6), writes=[V(ctst, i)])
            f.dma("sp", self.CT[:, tsl], ctst[:], reads=[ctst], writes=[V(self.CT, u)])

    def phase_dsa(self, l):
        f, S, NT, NIT, NSEL = self.f, self.S, self.NT, self.NIT, self.NSEL
        PS = self.PS
        CTr = f.sb("d_CT", [128, S], BF16); EPSP = f.sb("d_eps", [128, S])
        VAt = [f.sb("d_VA%d" % i, [128, 129], BF16) for i in range(4)]
        f.dma("sp", CTr[:], self.CT[:], reads=[self.CT], writes=[CTr])
        f.dma("sp", EPSP[:], self.cin["c_epspos"][:], writes=[EPSP])
        WUV = f.sb("d_wuv", [128, 8, 64])
        f.dma("sp", WUV[:], self.w_uv[l].rearrange("h c d -> c h d"), writes=[WUV])
        I4 = f.sb("d_i4", [128, 512], BF16); CBM = f.sb("d_cbm", [128, 128]); ONEI = f.sb("d_onei", [65, 128], BF16)
        NSL = f.sb("d_nsl", [33, 1024])
        f.dma("sp", NSL[:], self.cin["c_negsl33"][:], writes=[NSL])
        POW2 = f.sb("d_pow2", [128, 64])
        f.dma("sp", I4[:], self.cin["c_i4"][:], writes=[I4])
        f.dma("sp", CBM[:], self.cin["c_cbm"][:], writes=[CBM])
        f.dma("sp", ONEI[:], self.cin["c_onei"][:], writes=[ONEI])
        f.dma("sp", POW2[:], self.cin["c_pow2"][:], writes=[POW2])
        RB = [f.sb("d_rb%d" % i, [65, 1024], BF16) for i in range(2)]
        SL128 = f.sb("d_sl128", [1, 1024])
        f.dma("sp", SL128[:], self.cin["c_sl128"][:], writes=[SL128])
        for r in RB:
            f.dma("sp", r[:], self.cin["c_rbinit"][:], writes=[r])
        smrep = f.sb("d_smrep", [128, 33])
        srow = f.sb("d_srow", [33, 4, 128]); srow_i = f.sb("d_srowi", [33, 2, 128], I32)
        SC = f.sb("d_SC", [128, S]); MB = f.sb("d_MB", [128, S], BF16)
        Pt = [f.sb("d_P%d" % i, [128, 1024], BF16) for i in range(2)]
        QLb = [f.sb("d_ql%d" % i, [128, 1024], BF16) for i in range(2)]
        QIb = [f.sb("d_qi%d" % i, [128, 2, 128]) for i in range(2)]
        WIb = [f.sb("d_wi%d" % i, [128, 4]) for i in range(2)]
        KIs = [f.sb("d_ki%d" % i, [128, 512]) for i in range(2)]
        rl = [f.sb("d_rl%d" % i, [128, 512]) for i in range(2)]
        sm = f.sb("d_sm", [128, 16])
        hs = f.sb("d_hs", [128, 64])
        ctx = f.sb("d_ctx", [128, 8, 128]); ctxT = f.sb("d_ctxT", [128, 8, 128])
        rinv = f.sb("d_rinv", [128, 8]); yd = [f.sb("d_yd%d" % i, [128, 512]) for i in range(2)]
        ACC = PS[:, 4 * 512: 8 * 512].rearrange("p (h c) -> p h c", c=256)
        SCs = [SC, f.sb("d_SC1", [128, S])]
        junk = f.sb("d_junk", [128, S], BF16)
        nrl = 0
        nkt = 0

        def scores(b):
            nonlocal nrl
            SC = SCs[b % 2]
            t0 = b * 128
            te = t0 + 128
            qi, wi = QIb[b % 2], WIb[b % 2]
            f.dma("sp", qi[:], self.QI[:, :, t0:te], reads=[self.QI], writes=[qi])
            f.dma("sp", wi[:], self.WI[t0:te, :], reads=[self.WI], writes=[wi])
            nsl = (te + 511) // 512
            for si in range(nsl):
                c0 = si * 512
                ncol = min(512, te - c0)
                ks = KIs[si % 2]
                f.dma("sp", ks[:, 0:ncol], self.KI[:, c0:c0 + ncol], reads=[self.KI], writes=[ks])
                for hi in range(4):
                    bk = nrl % 2
                    r_ = rl[nrl % 2]
                    nrl += 1
                    pb = (hi % 2) * 64
                    f.mm(PS[:, bk * 512: bk * 512 + ncol], qi[pb:pb + 64, hi // 2, :], ks[pb:pb + 64, 0:ncol], True, True,
                         [qi, ks], self.bv(bk))
                    f.op("act", lambda e: e.activation(r_[:, 0:ncol], PS[:, bk * 512: bk * 512 + ncol], AF.Relu),
                         reads=self.bv(bk), writes=[r_])
                    prev = EPSP if hi == 0 else SC
                    f.op("dve", lambda e: e.scalar_tensor_tensor(out=SC[:, c0:c0 + ncol], in0=r_[:, 0:ncol],
                                                                 scalar=wi[:, hi:hi + 1], in1=prev[:, c0:c0 + ncol],
                                                                 op0=ALU.mult, op1=ALU.add),
                         reads=[r_, wi, EPSP, SC], writes=[SC])

        def bisect(b):
            SC = SCs[b % 2]
            t0 = b * 128
            te = t0 + 128
            f.op("dve", lambda e: e.tensor_reduce(out=sm[:, 0:1], in_=SC[:, 0:te], axis=AX.X, op=ALU.min),
                 reads=[SC], writes=[V(sm, 0)])
            f.op("dve", lambda e: e.tensor_tensor(out=SC[:, t0:te], in0=SC[:, t0:te], in1=CBM[:], op=ALU.add),
                 reads=[SC, CBM], writes=[SC])
            f.op("dve", lambda e: e.tensor_reduce(out=sm[:, 1:2], in_=SC[:, 0:te], axis=AX.X, op=ALU.max),
                 reads=[SC], writes=[V(sm, 1)])
            f.op("dve", lambda e: e.tensor_tensor(out=sm[:, 7:8], in0=sm[:, 1:2], in1=sm[:, 0:1], op=ALU.subtract),
                 reads=[V(sm, 0), V(sm, 1)], writes=[V(sm, 7)])
            f.op("dve", lambda e: e.tensor_scalar(sm[:, 7:8], sm[:, 7:8], 1.001, 1e-6, op0=ALU.mult, op1=ALU.add),
                 reads=[V(sm, 7)], writes=[V(sm, 7)])
            f.op("dve", lambda e: e.tensor_scalar(hs[:, 0:NIT], POW2[:, 0:NIT], sm[:, 7:8], None, op0=ALU.mult),
                 reads=[POW2, V(sm, 7)], writes=[hs])
            f.op("dve", lambda e: e.tensor_copy(sm[:, 2:3], sm[:, 0:1]), reads=[V(sm, 0)], writes=[V(sm, 2)])
            for it in range(NIT):
                f.op("dve", lambda e: e.tensor_tensor(out=sm[:, 3:4], in0=sm[:, 2:3], in1=hs[:, it:it + 1], op=ALU.add),
                     reads=[V(sm, 2), hs], writes=[V(sm, 3)])
                f.op("dve", lambda e: e.tensor_scalar(junk[:, 0:te], SC[:, 0:te], sm[:, 3:4], 0.0, op0=ALU.is_ge, op1=ALU.add,
                                                      accum_out=sm[:, 4:5]), reads=[SC, V(sm, 3)], writes=[junk, V(sm, 4)])
                f.op("dve", lambda e: e.tensor_scalar(sm[:, 5:6], sm[:, 4:5], float(NSEL) - 0.5, hs[:, it:it + 1],
                                                      op0=ALU.is_ge, op1=ALU.mult), reads=[V(sm, 4), hs], writes=[V(sm, 5)])
                f.op("dve", lambda e: e.tensor_tensor(out=sm[:, 2:3], in0=sm[:, 2:3], in1=sm[:, 5:6], op=ALU.add),
                     reads=[V(sm, 2), V(sm, 5)], writes=[V(sm, 2)])
            f.op("dve", lambda e: e.tensor_scalar(MB[:, 0:te], SC[:, 0:te], sm[:, 2:3], NEG, op0=ALU.is_lt, op1=ALU.mult),
                 reads=[SC, V(sm, 2)], writes=[MB])
            f.op("dve", lambda e: e.tensor_tensor(out=SC[:, 0:te], in0=MB[:, 0:te], in1=EPSP[:, 0:te], op=ALU.subtract),
                 reads=[EPSP, MB], writes=[SC])
            f.op("dve", lambda e: e.tensor_reduce(out=sm[:, 6:7], in_=SC[:, 0:te], axis=AX.X, op=ALU.max),
                 reads=[SC], writes=[V(sm, 6)])
            f.op("dve", lambda e: e.tensor_copy(smrep[:], sm[:, 6:7].to_broadcast([128, 33])), reads=[V(sm, 6)], writes=[smrep])
            f.tr(PS[0:33, 0:128], smrep[:], self.ident[:], [smrep, self.ident], self.bv(0))
            f.op("dve", lambda e: e.tensor_scalar(srow[:, 0, :], PS[0:33, 0:128], 2.0 ** 30, None, op0=ALU.mult),
                 reads=self.bv(0), writes=[V(srow, 0)])
            f.op("dve", lambda e: e.tensor_copy(srow_i[:, 0, :], srow[:, 0, :]), reads=[V(srow, 0)], writes=[V(srow_i, 0)])
            f.op("dve", lambda e: e.tensor_scalar(srow_i[:, 1, :], srow_i[:, 0, :], 127, None, op0=ALU.bitwise_and),
                 reads=[V(srow_i, 0)], writes=[V(srow_i, 1)])
            f.op("dve", lambda e: e.tensor_copy(srow[:, 1, :], srow_i[:, 1, :]), reads=[V(srow_i, 1)], writes=[V(srow, 1)])
            f.op("dve", lambda e: e.tensor_tensor(out=srow[:, 2, :], in0=srow[:, 0, :], in1=srow[:, 1, :], op=ALU.subtract),
                 reads=[V(srow, 0), V(srow, 1)], writes=[V(srow, 2)])
            rb0 = RB[nkt % 2]
            f.op("dve", lambda e: e.tensor_tensor(out=rb0[0:1, :].rearrange("p (h t) -> p h t", t=128),
                                                  in0=NSL[0:1, :].rearrange("p (h t) -> p h t", t=128),
                                                  in1=srow[0:1, 2, :].unsqueeze(1).to_broadcast([1, 8, 128]), op=ALU.mult),
                 reads=[NSL, V(srow, 2)], writes=[V(rb0, 0)])
            for r_ in RB:
                f.op("dve", lambda e: e.tensor_tensor(out=r_[32:33, :].rearrange("p (h t) -> p h t", t=128),
                                                      in0=NSL[32:33, :].rearrange("p (h t) -> p h t", t=128),
                                                      in1=srow[32:33, 1, :].unsqueeze(1).to_broadcast([1, 8, 128]), op=ALU.mult),
                     reads=[NSL, V(srow, 1)], writes=[V(r_, 32)])

        def attention(b):
            nonlocal nkt
            t0 = b * 128
            te = t0 + 128
            ql = QLb[b % 2]
            f.dma("sp", ql[:], self.QL[:, b, :], reads=[self.QL], writes=[ql])
            for j in range(b + 1):
                rb = RB[nkt % 2]
                p_ = Pt[nkt % 2]
                lb = 2
                nkt += 1
                va = VAt[nkt % 4]
                f.dma("sp", va[:], self.CV[j * 128:(j + 1) * 128, :], reads=[self.CV], writes=[va])
                if j > 0:
                    rbp = RB[(nkt - 2) % 2]
                    f.op("pool", lambda e: e.tensor_tensor(out=rb[0:1, :], in0=rbp[0:1, :], in1=SL128[:], op=ALU.add),
                         reads=[V(rbp, 0), SL128], writes=[V(rb, 0)])
                for g in range(2):
                    o_ = self.bank(lb + g)
                    f.mm(o_, CTr[:, j * 128:(j + 1) * 128], ql[:, g * 512:(g + 1) * 512], True, False, [CTr, ql], self.bv(lb + g))
                    f.mm(o_, ONEI[:], rb[:, g * 512:(g + 1) * 512], False, False, [ONEI, rb], self.bv(lb + g))
                    f.mm(o_, MB[:, j * 128:(j + 1) * 128], I4[:], False, True, [MB, I4], self.bv(lb + g))
                    f.op("act", lambda e: e.activation(p_[:, g * 512:(g + 1) * 512], o_, AF.Exp),
                         reads=self.bv(lb + g), writes=[V(p_, g)])
                for h in range(8):
                    f.mm(ACC[:, h, 0:129], p_[:, h * 128:(h + 1) * 128], va[:], (j == 0 and h % 2 == 0), j == b,
                         [V(p_, h // 4), va], self.bv(4 + h // 2))
            f.op("dve", lambda e: e.reciprocal(rinv[:], ACC[:, :, 128]), reads=self.bv(4, 4), writes=[rinv])
            f.op("dve", lambda e: e.tensor_tensor(out=ctx[:], in0=ACC[:, :, 0:128],
                                                  in1=rinv[:].unsqueeze(2).to_broadcast([128, 8, 128]), op=ALU.mult),
                 reads=self.bv(4, 4) + [rinv], writes=[ctx])
            self.transposes(lambda c: ctx[:, c, :], 8, lambda c0, n: ctxT[:, c0:c0 + n, :], 0, [ctx], [ctxT])
            for h in range(8):
                f.mm(PS[:, 2 * 512 + h * 64: 2 * 512 + (h + 1) * 64], ctxT[:, h, :], WUV[:, h, :], True, True,
                     [ctxT, WUV], self.bv(2))
            y_ = yd[b % 2]
            f.op("act", lambda e: e.copy(y_[:], self.bank(2)), reads=self.bv(2), writes=[y_])
            f.dma("sp", self.MIX[t0:te, D:D + 512], y_[:], reads=[y_], writes=[V(self.MIX, ("dsa", b))])


        scores(0)
        for b in range(NT):
            bisect(b)
            if b + 1 < NT:
                scores(b + 1)
            attention(b)

    def phase_mem(self, l):
        f, NT = self.f, self.NT
        PS = self.PS
        WKm = f.sb("m_wk", [128, 8, 512]); WVm = f.sb("m_wv", [128, 8, 512])
        f.dma("sp", WKm[:], self.w_mem_k[l].rearrange("(c p) n -> p c n", p=128), writes=[WKm])
        f.dma("sp", WVm[:], self.w_mem_v[l].rearrange("(c p) n -> p c n", p=128), writes=[WVm])
        memKT = f.sb("m_kT", [128, 4, MEM_LEN]); memV = f.sb("m_v", [128, 2, 512])
        self.memT = f.sb("m_memT", [128, 8, MEM_LEN])
        mt_ = f.sb("m_mem", [128, D])
        for mi in range(MEM_LEN // 128):
            f.dma("sp", mt_[:], self.mem_in[mi * 128:(mi + 1) * 128, :], writes=[mt_])
            self.transposes(lambda c: mt_[:, c * 128:(c + 1) * 128], 8,
                            lambda c0, n: self.memT[:, c0:c0 + n, mi * 128:(mi + 1) * 128], 0, [mt_], [self.memT])
        for h in range(4):
            for kc in range(8):
                f.mm(PS[:, 0:MEM_LEN], WKm[:, kc, h * 128:(h + 1) * 128], self.memT[:, kc, :], kc == 0, kc == 7,
                     [WKm, self.memT], self.bv(0))
            f.op("act", lambda e: e.copy(memKT[:, h, :], PS[:, 0:MEM_LEN]), reads=self.bv(0), writes=[memKT])
        for mt in range(2):
            for kc in range(8):
                f.mm(self.bank(1), self.memT[:, kc, mt * 128:(mt + 1) * 128], WVm[:, kc, :], kc == 0, kc == 7,
                     [WVm, self.memT], self.bv(1))
            f.op("act", lambda e: e.copy(memV[:, mt, :], self.bank(1)), reads=self.bv(1), writes=[memV])
        QMt = [f.sb("m_q%d" % i, [128, 4, 128]) for i in range(2)]
        Pm = f.sb("m_P", [128, MEM_LEN]); PT = f.sb("m_PT", [128, 2, 128])
        sm = f.sb("m_sm", [128, 4]); ym = [f.sb("m_y%d" % i, [128, 512]) for i in range(2)]
        for i in range(NT):
            q_, y_ = QMt[i % 2], ym[i % 2]
            tok = slice(i * 128, (i + 1) * 128)
            f.dma("sp", q_[:], self.QM[:, :, tok], reads=[self.QM], writes=[q_])
            for h in range(4):
                bk = 2 + h % 2
                f.mm(PS[:, bk * 512: bk * 512 + MEM_LEN], q_[:, h, :], memKT[:, h, :], True, True, [q_, memKT], self.bv(bk))
                f.op("dve", lambda e: e.tensor_reduce(out=sm[:, 0:1], in_=PS[:, bk * 512: bk * 512 + MEM_LEN], axis=AX.X,
                                                      op=ALU.max), reads=self.bv(bk), writes=[V(sm, 0)])
                f.op("dve", lambda e: e.tensor_scalar(sm[:, 1:2], sm[:, 0:1], -1.0, None, op0=ALU.mult),
                     reads=[V(sm, 0)], writes=[V(sm, 1)])
                f.op("act", lambda e: e.activation(Pm[:], PS[:, bk * 512: bk * 512 + MEM_LEN], AF.Exp, bias=sm[:, 1:2],
                                                   scale=1.0, accum_out=sm[:, 2:3]),
                     reads=self.bv(bk) + [V(sm, 1)], writes=[Pm, V(sm, 2)])
                f.op("dve", lambda e: e.reciprocal(sm[:, 3:4], sm[:, 2:3]), reads=[V(sm, 2)], writes=[V(sm, 3)])
                self.transposes(lambda c: Pm[:, c * 128:(c + 1) * 128], 2, lambda c0, n: PT[:, c0:c0 + n, :], 4, [Pm], [PT])
                for mt in range(2):
                    f.mm(PS[:, 6 * 512: 6 * 512 + 128], PT[:, mt, :], memV[:, mt, h * 128:(h + 1) * 128], mt == 0, mt == 1,
                         [PT, memV], self.bv(6))
                f.op("dve", lambda e: e.tensor_scalar(y_[:, h * 128:(h + 1) * 128], PS[:, 6 * 512: 6 * 512 + 128],
                                                      sm[:, 3:4], None, op0=ALU.mult),
                     reads=self.bv(6) + [V(sm, 3)], writes=[y_])
            f.dma("sp", self.MIX[tok, D + 512:D + 1024], y_[:], reads=[y_], writes=[V(self.MIX, ("mem", i))])

    def phase_outp(self, l):
        f, NT, CAP = self.f, self.NT, self.CAP
        PS = self.PS
        WO = f.sb("o_WO", [128, 16, D], BF16)
        WOs = [f.sb("o_WOs%d" % i, [128, D]) for i in range(2)]
        for kc in range(16):
            ws = WOs[kc % 2]
            f.dma("sp", ws[:], self.w_out[l, kc * 128:(kc + 1) * 128, :], writes=[ws])
            f.op("pool", lambda e: e.tensor_copy(WO[:, kc, :], ws[:]), reads=[ws], writes=[V(WO, kc)])
        RW = f.sb("o_RW", [128, 8, NE])
        f.dma("sp", RW[:], self.router_w[l].rearrange("(c p) n -> p c n", p=128), writes=[RW])
        RBb = self.bcast_load("o_rb", self.router_b[l, :], NE)
        g1 = self.bcast_load("o_g1", self.ln1_g[l, :], D)
        b1 = self.bcast_load("o_b1", self.ln1_b[l, :], D)
        ECAP = f.sb("o_ecap", [128, NE])
        f.dma("sp", ECAP[:], self.cin["c_eidx"][:], writes=[ECAP])
        f.op("dve", lambda e: e.tensor_scalar(ECAP[:], ECAP[:], float(CAP), None, op0=ALU.mult), reads=[ECAP], writes=[ECAP])
        base = f.sb("o_base", [128, NE])
        f.op("dve", lambda e: e.memset(base[:], 0.0), writes=[base])
        if l == 0:
            zt = f.sb("o_zero", [128, D])
            f.op("dve", lambda e: e.memset(zt[:], 0.0), writes=[zt])
            NR = CAP // 128
            for e_ in range(NE):
                f.dma("sp", self.XD[e_ * CAP:(e_ + 1) * CAP, :].rearrange("(r p) d -> p r d", p=128),
                      zt[:].unsqueeze(1).to_broadcast([128, NR, D]), reads=[zt], writes=[V(self.XD, ("z", e_))])
            f.barrier()
        mixt = [f.sb("o_mix%d" % i, [128, DMIX]) for i in range(2)]
        ht = [f.sb("o_h%d" % i, [128, D]) for i in range(2)]
        mixT = f.sb("o_mixT", [128, 16, 128], BF16)
        r_ = f.sb("o_r", [128, D]); scr = f.sb("o_scr", [128, D])
        h2t = [f.sb("o_h2%d" % i, [128, D]) for i in range(2)]
        h2T = f.sb("o_h2T", [128, 8, 128])
        stats = f.sb("o_st", [128, 12]); mv = f.sb("o_mv", [128, 4])
        lg = f.sb("o_lg", [128, NE]); m8 = f.sb("o_m8", [128, 8]); sel = f.sb("o_sel", [128, NE])
        e4 = f.sb("o_e4", [128, 4]); sm = f.sb("o_sm", [128, 4])
        g4 = [f.sb("o_g4%d" % i, [128, 4]) for i in range(2)]
        slotf = f.sb("o_slotf", [128, NE]); tmp = f.sb("o_tmp", [128, NE]); eq = f.sb("o_eq", [128, NE])
        junk = f.sb("o_junk", [128, NE])
        s4f = f.sb("o_s4f", [128, 4])
        s4i = [f.sb("o_s4i%d" % i, [128, 4], I32) for i in range(2)]
        for i in range(NT):
            tok = slice(i * 128, (i + 1) * 128)
            mx, h_, h2_, g4_, s4_ = mixt[i % 2], ht[i % 2], h2t[i % 2], g4[i % 2], s4i[i % 2]
            f.dma("sp", mx[:], self.MIX[tok, :], reads=[self.MIX], writes=[mx])
            f.dma("sp", h_[:], self.H[tok, :], reads=[V(self.H, i)], writes=[h_])
            self.transposes(lambda c: mx[:, c * 128:(c + 1) * 128], 16, lambda c0, n: mixT[:, c0:c0 + n, :], 0, [mx], [mixT])
            for sl in range(2):
                for kc in range(16):
                    f.mm(self.bank(2 + sl), mixT[:, kc, :], WO[:, kc, sl * 512:(sl + 1) * 512], kc == 0, kc == 15,
                         [mixT, V(WO, kc)], self.bv(2 + sl))
                f.op("dve", lambda e: e.scalar_tensor_tensor(out=r_[:, sl * 512:(sl + 1) * 512], in0=h_[:, sl * 512:(sl + 1) * 512],
                                                             scalar=float(ALPHA), in1=self.bank(2 + sl), op0=ALU.mult, op1=ALU.add),
                     reads=[h_] + self.bv(2 + sl), writes=[r_])
            self.ln_tile(r_, h2_, g1, b1, scr, stats, mv)
            f.dma("sp", self.H2[tok, :], h2_[:], reads=[h2_], writes=[V(self.H2, i)])
            self.transposes(lambda c: h2_[:, c * 128:(c + 1) * 128], 8, lambda c0, n: h2T[:, c0:c0 + n, :], 0, [h2_], [h2T])
            for kc in range(8):
                f.mm(PS[:, 4 * 512: 4 * 512 + NE], h2T[:, kc, :], RW[:, kc, :], kc == 0, kc == 7, [h2T, RW], self.bv(4))
            f.op("dve", lambda e: e.tensor_tensor(out=lg[:], in0=PS[:, 4 * 512: 4 * 512 + NE], in1=RBb[:], op=ALU.add),
                 reads=self.bv(4) + [RBb], writes=[lg])
            f.op("dve", lambda e: e.max(out=m8[:], in_=lg[:]), reads=[lg], writes=[m8])
            f.op("dve", lambda e: e.tensor_scalar(sel[:], lg[:], m8[:, 3:4], None, op0=ALU.is_ge), reads=[lg, m8], writes=[sel])
            f.op("dve", lambda e: e.tensor_scalar(sm[:, 0:1], m8[:, 0:1], -1.0, None, op0=ALU.mult), reads=[m8], writes=[V(sm, 0)])
            f.op("act", lambda e: e.activation(e4[:], m8[:, 0:4], AF.Exp, bias=sm[:, 0:1], scale=1.0, accum_out=sm[:, 1:2]),
                 reads=[m8, V(sm, 0)], writes=[e4, V(sm, 1)])
            f.op("dve", lambda e: e.reciprocal(sm[:, 2:3], sm[:, 1:2]), reads=[V(sm, 1)], writes=[V(sm, 2)])
            f.op("dve", lambda e: e.tensor_scalar(g4_[:], e4[:], sm[:, 2:3], None, op0=ALU.mult), reads=[e4, V(sm, 2)], writes=[g4_])
            f.dma("sp", self.GATE4[tok, :], g4_[:], reads=[g4_], writes=[V(self.GATE4, i)])
            f.mm(PS[:, 5 * 512: 5 * 512 + NE], self.cSU[:], sel[:], True, True, [self.cSU, sel], self.bv(5))
            f.mm(PS[:, 6 * 512: 6 * 512 + NE], self.ones[:], sel[:], True, True, [self.ones, sel], self.bv(6))
            f.op("dve", lambda e: e.tensor_tensor(out=tmp[:], in0=PS[:, 5 * 512: 5 * 512 + NE], in1=base[:], op=ALU.add),
                 reads=self.bv(5) + [base], writes=[tmp])
            f.op("dve", lambda e: e.tensor_tensor(out=slotf[:], in0=tmp[:], in1=ECAP[:], op=ALU.add), reads=[tmp, ECAP], writes=[slotf])
            f.op("dve", lambda e: e.tensor_scalar(tmp[:], tmp[:], float(CAP) - 0.5, 1.0e9, op0=ALU.is_ge, op1=ALU.mult),
                 reads=[tmp], writes=[tmp])
            f.op("dve", lambda e: e.tensor_tensor(out=slotf[:], in0=slotf[:], in1=tmp[:], op=ALU.add), reads=[slotf, tmp], writes=[slotf])
            f.op("dve", lambda e: e.tensor_tensor(out=base[:], in0=base[:], in1=PS[:, 6 * 512: 6 * 512 + NE], op=ALU.add),
                 reads=[base] + self.bv(6), writes=[base])
            for k in range(4):
                f.op("dve", lambda e: e.scalar_tensor_tensor(out=junk[:], in0=lg[:], scalar=m8[:, k:k + 1], in1=slotf[:],
                                                             op0=ALU.is_equal, op1=ALU.mult, accum_out=s4f[:, k:k + 1]),
                     reads=[lg, m8, slotf], writes=[junk, V(s4f, k)])
            f.op("dve", lambda e: e.tensor_copy(s4_[:], s4f[:]), reads=[s4f], writes=[s4_])
            f.dma("sp", self.SLOT4[tok, :], s4_[:], reads=[s4_], writes=[V(self.SLOT4, i)])
            for k in range(4):
                f.dma("pool", self.XD[:, :], h2_[:, :], reads=[h2_, s4_], writes=[V(self.XD, ("s", i, k))],
                      indirect=dict(out_offset=bass.IndirectOffsetOnAxis(ap=s4_[:, k:k + 1], axis=0), in_offset=None,
                                    bounds_check=self.bc_reg(), oob_is_err=False))

    def phase_moe(self, l):
        f, CAP = self.f, self.CAP
        PS = self.PS
        NR = CAP // 128
        BGall = f.sb("e_bgall", [128, 16, NE])
        with ExitStack() as tmpst:
            old_st = f.stack
            f.stack = tmpst
            bgl = f.sb("e_bgl", [NE, 2 * DFF])
            f.dma("sp", bgl[:], self.b_gu[l], writes=[bgl])
            for c in range(16):
                bk = c % 2
                f.tr(PS[0:128, bk * 512: bk * 512 + NE], bgl[:, c * 128:(c + 1) * 128], self.ident[0:NE, 0:NE],
                     [bgl, self.ident], self.bv(bk))
                f.op("act", lambda e: e.copy(BGall[:, c, :], PS[:, bk * 512: bk * 512 + NE]), reads=self.bv(bk), writes=[V(BGall, c)])
            f.barrier()
            f.stack = old_st
        XT = f.sb("e_XT", [128, 8, CAP], BF16); AT = f.sb("e_AT", [128, 8, CAP], BF16)
        WD = [f.sb("e_WD%d" % i, [128, 8, D], BF16) for i in range(2)]
        WDs = f.sb("e_WDs", [128, 8, D])
        WG = [f.sb("e_WG%d" % i, [128, 8, 256], BF16) for i in range(2)]
        WGs = [f.sb("e_WGs%d" % i, [128, 8, 256]) for i in range(2)]
        xr = [f.sb("e_xr%d" % i, [128, D]) for i in range(2)]
        yr = [f.sb("e_yr%d" % i, [128, D]) for i in range(2)]
        BD = [f.sb("e_BD%d" % i, [128, D]) for i in range(2)]
        tg = f.sb("e_tg", [128, 512]); tsg = f.sb("e_tsg", [128, 512]); tl = f.sb("e_tl", [128, 512])
        slabs = [(s0, min(512, CAP - s0)) for s0 in range(0, CAP, 512)]
        nwg = 0
        nps = 0
        nrow = 0
        for e_ in range(NE):
            wd, bd = WD[e_ % 2], BD[e_ % 2]
            f.dma("sp", bd[:], self.b_down[l, e_, :].partition_broadcast(128), writes=[bd])
            for r in range(NR):
                x_ = xr[nrow % 2]
                nrow += 1
                f.dma("sp", x_[:], self.XD[e_ * CAP + r * 128: e_ * CAP + (r + 1) * 128, :], reads=[self.XD], writes=[x_])
                self.transposes(lambda c: x_[:, c * 128:(c + 1) * 128], 8, lambda c0, n: XT[:, c0:c0 + n, r * 128:(r + 1) * 128],
                                0, [x_], [V(XT, r)])
            for j in range(8):
                if j == 3:
                    f.dma("sp", WDs[:], self.w_down[l, e_].rearrange("(c p) n -> p c n", p=128), writes=[WDs])
                if j == 5:
                    for hf in range(2):
                        f.op("act", lambda e: e.copy(wd[:, hf * 4:(hf + 1) * 4, :], WDs[:, hf * 4:(hf + 1) * 4, :]),
                             reads=[WDs], writes=[V(wd, hf)])
                wg = WG[nwg % 2]
                nwg += 1
                wgs = WGs[(nwg - 1) % 2]
                f.dma("sp", wgs[:, :, 0:128], self.w_gu[l, e_, :, j * 128:(j + 1) * 128].rearrange("(c p) n -> p c n", p=128),
                      writes=[V(wgs, 0)])
                f.dma("sp", wgs[:, :, 128:256],
                      self.w_gu[l, e_, :, DFF + j * 128: DFF + (j + 1) * 128].rearrange("(c p) n -> p c n", p=128),
                      writes=[V(wgs, 1)])
                f.op("pool", lambda e: e.tensor_copy(wg[:], wgs[:]), reads=[wgs], writes=[wg])
                for (s0, n) in slabs:
                    bg_, bl_ = 2 + (nps % 2) * 2, 3 + (nps % 2) * 2
                    nps += 1
                    for kc in range(8):
                        f.mm(PS[:, bg_ * 512: bg_ * 512 + n], wg[:, kc, 0:128], XT[:, kc, s0:s0 + n], kc == 0, kc == 7,
                             [wg, XT], self.bv(bg_))
                    for kc in range(8):
                        f.mm(PS[:, bl_ * 512: bl_ * 512 + n], wg[:, kc, 128:256], XT[:, kc, s0:s0 + n], kc == 0, kc == 7,
                             [wg, XT], self.bv(bl_))
                    f.op("dve", lambda e: e.tensor_scalar(tg[:, 0:n], PS[:, bg_ * 512: bg_ * 512 + n], BGall[:, j, e_:e_ + 1], LIM,
                                                          op0=ALU.add, op1=ALU.min), reads=self.bv(bg_) + [BGall], writes=[tg])
                    f.op("act", lambda e: e.activation(tsg[:, 0:n], tg[:, 0:n], AF.Sigmoid, scale=SW_ALPHA), reads=[tg], writes=[tsg])
                    f.op("dve", lambda e: e.tensor_scalar(tl[:, 0:n], PS[:, bl_ * 512: bl_ * 512 + n], BGall[:, 8 + j, e_:e_ + 1], LIM,
                                                          op0=ALU.add, op1=ALU.min), reads=self.bv(bl_) + [BGall], writes=[tl])
                    f.op("dve", lambda e: e.tensor_scalar(tl[:, 0:n], tl[:, 0:n], -LIM, 1.0, op0=ALU.max, op1=ALU.add),
                         reads=[tl], writes=[tl])
                    f.op("dve", lambda e: e.tensor_tensor(out=tg[:, 0:n], in0=tg[:, 0:n], in1=tsg[:, 0:n], op=ALU.mult),
                         reads=[tg, tsg], writes=[tg])
                    f.op("dve", lambda e: e.tensor_tensor(out=AT[:, j, s0:s0 + n], in0=tg[:, 0:n], in1=tl[:, 0:n], op=ALU.mult),
                         reads=[tg, tl], writes=[V(AT, j)])
            for r in range(NR):
                y_ = yr[r % 2]
                for sl in range(2):
                    for fc in range(8):
                        f.mm(self.bank(6 + sl), AT[:, fc, r * 128:(r + 1) * 128], wd[:, fc, sl * 512:(sl + 1) * 512],
                             fc == 0, fc == 7, [AT, wd], self.bv(6 + sl))
                    f.op("dve", lambda e: e.tensor_tensor(out=y_[:, sl * 512:(sl + 1) * 512], in0=self.bank(6 + sl),
                                                          in1=bd[:, sl * 512:(sl + 1) * 512], op=ALU.add),
                         reads=self.bv(6 + sl) + [bd], writes=[y_])
                f.dma("sp", self.YD[e_ * CAP + r * 128: e_ * CAP + (r + 1) * 128, :], y_[:], reads=[y_],
                      writes=[V(self.YD, (e_, r))])

    def phase_comb(self, l, last):
        f, NT, CAP = self.f, self.NT, self.CAP
        g2 = self.bcast_load("c_g2", self.ln2_g[l, :], D)
        b2 = self.bcast_load("c_b2", self.ln2_b[l, :], D)
        h2t = [f.sb("c_h2%d" % i, [128, D]) for i in range(2)]
        g4 = [f.sb("c_g4%d" % i, [128, 4]) for i in range(2)]
        s4 = [f.sb("c_s4%d" % i, [128, 4], I32) for i in range(2)]
        yk = [f.sb("c_yk%d" % i, [128, D]) for i in range(4)]
        acc = f.sb("c_acc", [128, D]); scr = f.sb("c_scr", [128, D])
        ot = [f.sb("c_o%d" % i, [128, D]) for i in range(2)]
        stats = f.sb("c_st", [128, 12]); mv = f.sb("c_mv", [128, 4])
        dst = self.out if last else self.H
        for i in range(NT):
            tok = slice(i * 128, (i + 1) * 128)
            h2_, g4_, s4_, o_ = h2t[i % 2], g4[i % 2], s4[i % 2], ot[i % 2]
            f.dma("sp", h2_[:], self.H2[tok, :], reads=[V(self.H2, i)], writes=[h2_])
            f.dma("sp", g4_[:], self.GATE4[tok, :], reads=[V(self.GATE4, i)], writes=[g4_])
            f.dma("sp", s4_[:], self.SLOT4[tok, :], reads=[V(self.SLOT4, i)], writes=[s4_])
            for k in range(4):
                y_ = yk[k]
                f.dma("pool", y_[:, :], self.YD[:, :], reads=[self.YD, s4_], writes=[y_],
                      indirect=dict(out_offset=None, in_offset=bass.IndirectOffsetOnAxis(ap=s4_[:, k:k + 1], axis=0),
                                    bounds_check=self.bc_reg(), oob_is_err=False))
                if k == 0:
                    f.op("dve", lambda e: e.tensor_scalar(acc[:], y_[:], g4_[:, 0:1], None, op0=ALU.mult),
                         reads=[y_, g4_], writes=[acc])
                else:
                    f.op("dve", lambda e: e.scalar_tensor_tensor(out=acc[:], in0=y_[:], scalar=g4_[:, k:k + 1], in1=acc[:],
                                                                 op0=ALU.mult, op1=ALU.add), reads=[y_, g4_, acc], writes=[acc])
            f.op("dve", lambda e: e.scalar_tensor_tensor(out=acc[:], in0=h2_[:], scalar=float(ALPHA), in1=acc[:],
                                                         op0=ALU.mult, op1=ALU.add), reads=[h2_, acc], writes=[acc])
            self.ln_tile(acc, o_, g2, b2, scr, stats, mv)
            f.dma("sp", dst[tok, :], o_[:], reads=[o_], writes=[V(dst, i)])


_WNAMES = ["w_in", "conv_w", "conv_b", "dt_bias", "a_log", "d_skip", "ssd_norm_g", "kv_norm_g", "w_uv",
           "w_mem_k", "w_mem_v", "w_out", "ln1_g", "ln1_b", "router_w", "router_b", "w_gu", "b_gu",
           "w_down", "b_down", "ln2_g", "ln2_b"]


def core_inputs(inp, b, S, L, consts=None):
    feed = {"x": np.ascontiguousarray(np.asarray(inp["x"])[b, :S]), "mem": np.ascontiguousarray(np.asarray(inp["mem"])[b]),
            "ln_in_g": np.asarray(inp["ln_in_g"]).reshape(1, D), "ln_in_b": np.asarray(inp["ln_in_b"]).reshape(1, D)}
    for k in _WNAMES:
        feed[k] = np.ascontiguousarray(np.asarray(inp[k])[:L])
    feed.update(consts if consts is not None else make_consts(S))
    return feed


SEQ = 8192
BATCH = 4
N_CORES = 8
CFG = dict(S=SEQ, depth=DEPTH_FULL, TS=512, CAP=1280, NIT=34, NSEL=256)


def kernel(**inputs):
    inputs = {k: np.asarray(v) for k, v in inputs.items()}
    mk = MK(CFG["S"], CFG["depth"], CFG["TS"], CFG["CAP"], CFG["NIT"], CFG["NSEL"])
    nc = mk.build()
    consts = make_consts(SEQ)
    shared = {k: np.ascontiguousarray(inputs[k]) for k in _WNAMES}
    shared["ln_in_g"] = inputs["ln_in_g"].reshape(1, D)
    shared["ln_in_b"] = inputs["ln_in_b"].reshape(1, D)
    shared.update(consts)
    in_maps = []
    for c in range(N_CORES):
        b = c % BATCH
        m = dict(shared)
        m["x"] = np.ascontiguousarray(inputs["x"][b])
        m["mem"] = np.ascontiguousarray(inputs["mem"][b])
        in_maps.append(m)
    res = run_bass_kernel_spmd(nc, in_maps, core_ids=list(range(N_CORES)))
    out = np.stack([np.asarray(res.results[b]["out"]) for b in range(BATCH)], axis=0)
    return out.astype(np.float32)
```

```python
import math
import numpy as np
from contextlib import ExitStack
import concourse.bass as bass
import concourse.mybir as mybir
from concourse.bass_utils import run_bass_kernel_spmd

F32 = mybir.dt.float32
BF16 = mybir.dt.bfloat16
I32 = mybir.dt.int32
U32 = mybir.dt.uint32
AF = mybir.ActivationFunctionType
ALU = mybir.AluOpType
AX = mybir.AxisListType

NDS = 24
NDS_SW = 8


class Buf:
    def __init__(self, t, name):
        self.t = t
        self.name = name
        self.w = {}
        self.r = {}

    def __getitem__(self, idx):
        return self.t[idx]


class V:
    def __init__(self, buf, tag=None):
        self.buf = buf
        self.tag = tag


def _norm(x):
    if isinstance(x, V):
        return x.buf, x.tag
    return x, None


class FW:
    def __init__(self, nc, stack, same_engine_sync=True):
        self.nc = nc
        self.stack = stack
        self.eng = {"pe": nc.tensor, "dve": nc.vector, "act": nc.scalar,
                    "pool": nc.gpsimd, "sp": nc.sync}
        self.sem = {k: stack.enter_context(nc.semaphore("s_" + k)) for k in self.eng}
        self.cnt = {k: 0 for k in self.eng}
        self.waited = {k: {} for k in self.eng}
        self.dsem = [stack.enter_context(nc.semaphore("d%d" % i)) for i in range(NDS)]
        self.dcnt = 0
        self.dsem_sw = [stack.enter_context(nc.semaphore("w%d" % i)) for i in range(NDS_SW)]
        self.dcnt_sw = 0
        self.ses = same_engine_sync
        self.ninstr = 0

    def sb(self, name, shape, dt=F32):
        self.nalloc = getattr(self, "nalloc", 0) + 1
        name = "%s_%d" % (name, self.nalloc)
        return Buf(self.stack.enter_context(self.nc.sbuf_tensor(name, list(shape), dt)), name)

    def ps(self, name, shape, dt=F32):
        return Buf(self.stack.enter_context(self.nc.psum_tensor(name, list(shape), dt)), name)

    def dram(self, name, shape, dt=F32, kind="Internal"):
        return Buf(self.nc.dram_tensor(name, list(shape), dt, kind=kind).ap(), name)

    def _deps(self, reads, writes):
        deps = []
        for x in reads:
            b, tag = _norm(x)
            for tg, tok in b.w.items():
                if tag is None or tg is None or tg == tag:
                    deps.append(tok)
        for x in writes:
            b, tag = _norm(x)
            for tg, tok in b.w.items():
                if tag is None or tg is None or tg == tag:
                    deps.append(tok)
            for tg, toks in b.r.items():
                if tag is None or tg is None or tg == tag:
                    deps.extend(toks)
        return deps

    def _wait(self, ek, deps, skip_same=False):
        e = self.eng[ek]
        need = {}
        for (sem, val, src) in deps:
            if src == ek and (skip_same or not self.ses):
                continue
            key = id(sem)
            if self.waited[ek].get(key, 0) >= val:
                continue
            if key not in need or need[key][1] < val:
                need[key] = (sem, val)
        for key, (sem, val) in need.items():
            e.wait_ge(sem, val)
            self.waited[ek][key] = val
            self.ninstr += 1

    def _record(self, tok, reads, writes):
        for x in reads:
            b, tag = _norm(x)
            lst = b.r.setdefault(tag, [])
            lst[:] = [t for t in lst if t[0] is not tok[0]] + [tok]
        for x in writes:
            b, tag = _norm(x)
            if tag is None:
                b.w = {None: tok}
                b.r = {}
            else:
                b.w[tag] = tok
                b.r[tag] = []

    def op(self, ek, fn, reads=(), writes=(), skip_same=False):
        self._wait(ek, self._deps(reads, writes), skip_same=skip_same)
        ins = fn(self.eng[ek])
        self.cnt[ek] += 1
        ins.then_inc(self.sem[ek], 1)
        tok = (self.sem[ek], self.cnt[ek], ek)
        self._record(tok, reads, writes)
        self.ninstr += 1
        return tok

    def dma(self, qk, out, in_, reads=(), writes=(), indirect=None, **kw):
        self._wait(qk, self._deps(reads, writes))
        if qk == "pool":
            i = self.dcnt_sw % NDS_SW
            rnd = self.dcnt_sw // NDS_SW
            self.dcnt_sw += 1
            sem = self.dsem_sw[i]
        else:
            i = self.dcnt % NDS
            rnd = self.dcnt // NDS
            self.dcnt += 1
            sem = self.dsem[i]
        if rnd > 0:
            key = id(sem)
            if self.waited[qk].get(key, 0) < 16 * rnd:
                self.eng[qk].wait_ge(sem, 16 * rnd)
                self.waited[qk][key] = 16 * rnd
        if indirect is None:
            self.eng[qk].dma_start(out=out, in_=in_, **kw).then_inc(sem, 16)
        else:
            self.eng[qk].indirect_dma_start(out=out, in_=in_, **indirect).then_inc(sem, 16)
        tok = (sem, 16 * (rnd + 1), "dma")
        self._record(tok, reads, writes)
        self.ninstr += 1
        return tok

    def barrier(self):
        toks = [(self.sem[k], self.cnt[k], k) for k in self.eng if self.cnt[k] > 0]
        for i in range(NDS):
            n = (self.dcnt - i + NDS - 1) // NDS
            if n > 0:
                toks.append((self.dsem[i], 16 * n, "dma"))
        for i in range(NDS_SW):
            n = (self.dcnt_sw - i + NDS_SW - 1) // NDS_SW
            if n > 0:
                toks.append((self.dsem_sw[i], 16 * n, "dma"))
        for ek in self.eng:
            self._wait(ek, [t for t in toks if t[2] != ek], skip_same=True)

    def mm(self, out, lhsT, rhs, start, stop, reads, writes):
        return self.op("pe", lambda e: e.matmul(out, lhsT, rhs, start=start, stop=stop,
                                                skip_group_check=True),
                       reads=reads, writes=writes, skip_same=True)

    def tr(self, out, in_, ident, reads, writes):
        return self.op("pe", lambda e: e.transpose(out, in_, ident), reads=reads, writes=writes,
                       skip_same=True)


D = 1024
DEPTH_FULL = 4
NH = 16
HP = 64
NG = 2
DST = 128
CONVW = 4
CONVD = D + 2 * NG * DST
DSA_H = 8
DLAT = 128
IDX_H = 4
IDX_D = 64
MEM_LEN = 256
MEM_H = 4
MEM_D = 128
DMIX = 2048
NE = 32
TOPK = 4
DFF = 1024
LIM = 7.0
SW_ALPHA = 1.702
ALPHA = (2 * DEPTH_FULL) ** 0.25
LN_EPS = 1e-5
RMS_EPS = 1e-6
O_Z, O_XBC, O_DT, O_QL, O_CKV, O_QI, O_KI, O_WI, O_QM, N_IN = (
    0, 1024, 2560, 2576, 3600, 3728, 3984, 4048, 4052, 4564)
NEG = -1.0e30
EPS_TIE = 2.0 ** -30


def make_consts(S):
    c = {}
    c["c_ident"] = np.eye(128, dtype=np.float32)
    k = np.arange(128)
    c["c_U"] = (k[:, None] <= k[None, :]).astype(np.float32)
    c["c_SL"] = (k[:, None] > k[None, :]).astype(np.float32)
    c["c_SU"] = (k[:, None] < k[None, :]).astype(np.float32)
    c["c_ones"] = np.ones((128, 128), np.float32)
    c["c_cbm"] = np.where(k[None, :] <= k[:, None], 0.0, NEG).astype(np.float32)
    c["c_epspos"] = np.broadcast_to((-EPS_TIE * np.arange(S, dtype=np.float64)).astype(np.float32)[None, :],
                                    (128, S)).copy()
    import ml_dtypes
    c["c_i4"] = np.tile(np.eye(128, dtype=np.float32), (1, 4)).astype(ml_dtypes.bfloat16)
    sl = (2.0 ** (-8.0 * np.arange(1, DSA_H + 1) / DSA_H)).astype(np.float32)
    c["c_sl128"] = np.repeat(sl * 128.0, 128)[None, :].astype(np.float32)
    onei = np.zeros((65, 128), np.float32)
    onei[0] = 1.0
    onei[32] = 1.0
    onei[64] = np.arange(128)
    c["c_onei"] = onei.astype(ml_dtypes.bfloat16)
    rbinit = np.zeros((65, 1024), np.float32)
    rbinit[64] = np.repeat(sl, 128)
    c["c_rbinit"] = rbinit.astype(ml_dtypes.bfloat16)
    c["c_negsl33"] = np.broadcast_to(np.repeat(-sl, 128)[None, :], (33, 1024)).astype(np.float32).copy()
    c["c_slrow"] = np.repeat(sl, 128)[None, :].astype(np.float32)
    c["c_pow2"] = np.broadcast_to((2.0 ** -(np.arange(64) + 1.0)).astype(np.float32)[None, :], (128, 64)).copy()
    c["c_eidx"] = np.broadcast_to(np.arange(NE, dtype=np.float32)[None, :], (128, NE)).copy()
    return c


class MK:
    def __init__(self, S, depth, TS, CAP, NIT, NSEL, stop_after=None):
        self.S, self.L, self.TS, self.CAP, self.NIT, self.NSEL = S, depth, TS, CAP, NIT, NSEL
        self.NT = S // 128
        self.NU = S // TS
        self.TPU = TS // 128
        self.stop_after = stop_after
        self.nc = bass.Bass("TRN2", target_bir_lowering=False)

    def din(self, name, shape, dt=F32):
        return Buf(self.nc.dram_tensor(name, list(shape), dt, kind="ExternalInput").ap(), name)

    def build(self):
        nc, S, L, NT, CAP = self.nc, self.S, self.L, self.NT, self.CAP
        din = self.din
        self.x_in = din("x", [S, D]); self.mem_in = din("mem", [MEM_LEN, D])
        self.ln_in_g = din("ln_in_g", [1, D]); self.ln_in_b = din("ln_in_b", [1, D])
        self.w_in = din("w_in", [L, D, N_IN])
        self.conv_w = din("conv_w", [L, CONVW, CONVD]); self.conv_b = din("conv_b", [L, CONVD])
        self.dt_bias = din("dt_bias", [L, NH]); self.a_log = din("a_log", [L, NH]); self.d_skip = din("d_skip", [L, NH])
        self.ssd_norm_g = din("ssd_norm_g", [L, D]); self.kv_norm_g = din("kv_norm_g", [L, DLAT])
        self.w_uv = din("w_uv", [L, DSA_H, DLAT, 64])
        self.w_mem_k = din("w_mem_k", [L, D, 512]); self.w_mem_v = din("w_mem_v", [L, D, 512])
        self.w_out = din("w_out", [L, DMIX, D])
        self.ln1_g = din("ln1_g", [L, D]); self.ln1_b = din("ln1_b", [L, D])
        self.router_w = din("router_w", [L, D, NE]); self.router_b = din("router_b", [L, NE])
        self.w_gu = din("w_gu", [L, NE, D, 2 * DFF]); self.b_gu = din("b_gu", [L, NE, 2 * DFF])
        self.w_down = din("w_down", [L, NE, DFF, D]); self.b_down = din("b_down", [L, NE, D])
        self.ln2_g = din("ln2_g", [L, D]); self.ln2_b = din("ln2_b", [L, D])
        self.cin = {}
        for k, v in make_consts(S).items():
            self.cin[k] = din(k, list(v.shape), BF16 if v.dtype.itemsize == 2 else F32)
        self.out = Buf(nc.dram_tensor("out", [S, D], F32, kind="ExternalOutput").ap(), "out")

        with ExitStack() as st:
            f = self.f = FW(nc, st)
            self.H = f.dram("H", [S, D]); self.H2 = f.dram("H2", [S, D])
            self.Z = f.dram("Z", [S, D]); self.DTs = f.dram("DTs", [S, NH])
            self.X = f.dram("X", [S, D]); self.BTM = f.dram("BTM", [S, 256])
            self.BT = f.dram("BT", [128, 2, S]); self.CTs = f.dram("CTs", [128, 2, S])
            self.QL = f.dram("QL", [128, NT, 1024], BF16); self.QI = f.dram("QI", [128, 2, S])
            self.KI = f.dram("KI", [128, S]); self.WI = f.dram("WI", [S, 4])
            self.CV = f.dram("CV", [S, 129], BF16); self.CT = f.dram("CT", [128, S], BF16)
            self.QM = f.dram("QM", [128, 4, S])
            self.MIX = f.dram("MIX", [S, DMIX])
            self.GATE4 = f.dram("GATE4", [S, 4]); self.SLOT4 = f.dram("SLOT4", [S, 4], I32)
            self.XD = f.dram("XD", [NE * CAP, D]); self.YD = f.dram("YD", [NE * CAP, D])

            self.ident = f.sb("ident", [128, 128]); self.cU = f.sb("cU", [128, 128])
            self.cSL = f.sb("cSL", [128, 128]); self.cSU = f.sb("cSU", [128, 128])
            self.ones = f.sb("ones", [128, 128])
            for sbt, nm in ((self.ident, "c_ident"), (self.cU, "c_U"), (self.cSL, "c_SL"),
                            (self.cSU, "c_SU"), (self.ones, "c_ones")):
                f.dma("sp", sbt[:], self.cin[nm][:], writes=[sbt])
            self.epsln = f.sb("epsln", [128, 4])
            f.op("dve", lambda e: e.memset(self.epsln[:, 0:1], LN_EPS), writes=[self.epsln])
            f.op("dve", lambda e: e.memset(self.epsln[:, 1:2], RMS_EPS), reads=[self.epsln], writes=[self.epsln])
            f.op("dve", lambda e: e.memset(self.epsln[:, 2:3], 1.0), reads=[self.epsln], writes=[self.epsln])
            self.PS = f.ps("PS", [128, 4096])

            stages = [("ln_in", lambda: self.phase_ln_in())]
            for l in range(L):
                stages += [("a1_%d" % l, lambda l=l: self.phase_a1(l)),
                           ("ssd_%d" % l, lambda l=l: self.phase_ssd(l)),
                           ("a2_%d" % l, lambda l=l: self.phase_a2(l)),
                           ("dsa_%d" % l, lambda l=l: self.phase_dsa(l)),
                           ("mem_%d" % l, lambda l=l: self.phase_mem(l)),
                           ("outp_%d" % l, lambda l=l: self.phase_outp(l)),
                           ("moe_%d" % l, lambda l=l: self.phase_moe(l)),
                           ("comb_%d" % l, lambda l=l: self.phase_comb(l, last=(l == L - 1)))]
            for name, fn in stages:
                with ExitStack() as ph:
                    old = f.stack
                    f.stack = ph
                    fn()
                    f.barrier()
                    f.stack = old
                if self.stop_after == name:
                    break
        return nc

    def bc_reg(self):
        if getattr(self, "_bc_reg", None) is None:
            self._bc_reg = self.nc.gpsimd.to_reg(NE * self.CAP - 1)
        return self._bc_reg

    def bank(self, i, n=1):
        return self.PS[:, i * 512:(i + n) * 512]

    def bv(self, i, n=1):
        return [V(self.PS, j) for j in range(i, i + n)]

    def ln_tile(self, src, dst, g_bc, b_bc, scr, stats, mv):
        f, epsln = self.f, self.epsln
        f.op("dve", lambda e: e.bn_stats(stats[:, 0:6], src[:, 0:512]), reads=[src], writes=[stats])
        f.op("dve", lambda e: e.bn_stats(stats[:, 6:12], src[:, 512:1024]), reads=[src, stats], writes=[stats])
        f.op("dve", lambda e: e.bn_aggr(mv[:, 0:2], stats[:, 0:12]), reads=[stats], writes=[mv])
        f.op("act", lambda e: e.activation(mv[:, 2:3], mv[:, 1:2], AF.Sqrt, bias=epsln[:, 0:1], scale=1.0),
             reads=[mv, epsln], writes=[mv])
        f.op("dve", lambda e: e.reciprocal(mv[:, 3:4], mv[:, 2:3]), reads=[mv], writes=[mv])
        f.op("dve", lambda e: e.tensor_scalar(scr[:], src[:], mv[:, 0:1], mv[:, 3:4],
                                              op0=ALU.subtract, op1=ALU.mult), reads=[src, mv], writes=[scr])
        f.op("dve", lambda e: e.tensor_tensor(out=scr[:], in0=scr[:], in1=g_bc[:], op=ALU.mult),
             reads=[scr, g_bc], writes=[scr])
        f.op("dve", lambda e: e.tensor_tensor(out=dst[:], in0=scr[:], in1=b_bc[:], op=ALU.add),
             reads=[scr, b_bc], writes=[dst])

    def transposes(self, src_fn, nchunks, dst_fn, pbank, rd, wr, evac="act", scale=None):
        f, PS = self.f, self.PS
        for gi, c0 in enumerate(range(0, nchunks, 4)):
            n = min(4, nchunks - c0)
            bk = pbank + gi % 2
            for c in range(c0, c0 + n):
                f.tr(PS[:, bk * 512 + (c - c0) * 128: bk * 512 + (c - c0 + 1) * 128],
                     src_fn(c), self.ident[:], reads=list(rd) + [self.ident], writes=self.bv(bk))
            dst = dst_fn(c0, n)
            srcp = PS[:, bk * 512: bk * 512 + n * 128].rearrange("p (c t) -> p c t", t=128)
            if scale is not None:
                f.op("act", lambda e: e.activation(dst, srcp, AF.Copy, scale=scale), reads=self.bv(bk), writes=wr)
            elif evac == "act":
                f.op("act", lambda e: e.copy(dst, srcp), reads=self.bv(bk), writes=wr)
            else:
                f.op("dve", lambda e: e.tensor_copy(dst, srcp), reads=self.bv(bk), writes=wr)

    def bcast_load(self, name, src_row_ap, n):
        t = self.f.sb(name, [128, n])
        self.f.dma("sp", t[:], src_row_ap.partition_broadcast(128), writes=[t])
        return t

    def phase_ln_in(self):
        f, NT = self.f, self.NT
        g_bc = self.bcast_load("p0_g", self.ln_in_g[0, :], D)
        b_bc = self.bcast_load("p0_b", self.ln_in_b[0, :], D)
        xt = [f.sb("p0_x%d" % i, [128, D]) for i in range(2)]
        ot = [f.sb("p0_o%d" % i, [128, D]) for i in range(2)]
        scr = f.sb("p0_scr", [128, D]); stats = f.sb("p0_st", [128, 12]); mv = f.sb("p0_mv", [128, 4])
        for i in range(NT):
            a, o = xt[i % 2], ot[i % 2]
            f.dma("sp", a[:], self.x_in[i * 128:(i + 1) * 128, :], writes=[a])
            self.ln_tile(a, o, g_bc, b_bc, scr, stats, mv)
            f.dma("sp", self.H[i * 128:(i + 1) * 128, :], o[:], reads=[o], writes=[V(self.H, i)])

    def load_hT(self, hT, u, htiles, hTb=None):
        f = self.f
        for i in range(self.TPU):
            ti = u * self.TPU + i
            a = htiles[ti % 2]
            f.dma("sp", a[:], self.H[ti * 128:(ti + 1) * 128, :], reads=[V(self.H, ti)], writes=[a])
            self.transposes(lambda c: a[:, c * 128:(c + 1) * 128], 8,
                            lambda c0, n: hT[:, c0:c0 + n, i * 128:(i + 1) * 128], 0, [a], [hT])
            if hTb is not None:
                f.op("pool", lambda e: e.tensor_copy(hTb[:, :, i * 128:(i + 1) * 128], hT[:, :, i * 128:(i + 1) * 128]),
                     reads=[hT], writes=[hTb])

    def phase_a1(self, l):
        f, TS, TPU, NU = self.f, self.TS, self.TPU, self.NU
        NW = O_QL
        W1 = f.sb("a1_W", [128, 8, NW], BF16)
        W1s = [f.sb("a1_Ws%d" % i, [128, NW]) for i in range(2)]
        for kc in range(8):
            ws = W1s[kc % 2]
            f.dma("sp", ws[:], self.w_in[l, kc * 128:(kc + 1) * 128, 0:NW], writes=[ws])
            f.op("pool", lambda e: e.tensor_copy(W1[:, kc, :], ws[:]), reads=[ws], writes=[V(W1, kc)])
        CW = f.sb("a1_cw", [128, 12, 4]); CBs = f.sb("a1_cb", [128, 12])
        for j in range(4):
            f.dma("sp", CW[:, :, j], self.conv_w[l, j, :].rearrange("(c p) -> p c", p=128), writes=[V(CW, j)],
                  allow_slow_non_contiguous=True)
        f.dma("sp", CBs[:], self.conv_b[l, :].rearrange("(c p) -> p c", p=128), writes=[CBs],
              allow_slow_non_contiguous=True)
        dtb = self.bcast_load("a1_dtb", self.dt_bias[l, :], NH)
        hT = f.sb("a1_hT", [128, 8, TS], BF16)
        htiles = [f.sb("a1_h%d" % i, [128, D]) for i in range(2)]
        xpre = f.sb("a1_xpre", [128, 12, TS + 3])
        xc = f.sb("a1_xc", [128, 12, TS])
        acc = f.sb("a1_acc", [128, TS])
        zt = [f.sb("a1_z%d" % i, [128, D]) for i in range(2)]
        dtt = f.sb("a1_dt", [128, NH]); dte = f.sb("a1_dte", [128, NH])
        xtm = [f.sb("a1_xtm%d" % i, [128, D + 256]) for i in range(2)]
        f.op("dve", lambda e: e.memset(xpre[:, :, 0:3], 0.0), writes=[xpre])
        for u in range(NU):
            self.load_hT(hT, u, htiles)
            for i in range(TPU):
                ti = u * TPU + i
                z = zt[ti % 2]
                for sl in range(2):
                    bk = 2 + sl
                    for kc in range(8):
                        f.mm(self.bank(bk), hT[:, kc, i * 128:(i + 1) * 128], W1[:, kc, sl * 512:(sl + 1) * 512],
                             kc == 0, kc == 7, [hT, V(W1, kc)], self.bv(bk))
                    f.op("act", lambda e: e.copy(z[:, sl * 512:(sl + 1) * 512], self.bank(bk)),
                         reads=self.bv(bk), writes=[z])
                f.dma("sp", self.Z[ti * 128:(ti + 1) * 128, :], z[:], reads=[z], writes=[V(self.Z, ti)])
                for kc in range(8):
                    f.mm(self.PS[:, 4 * 512:4 * 512 + NH], hT[:, kc, i * 128:(i + 1) * 128],
                         W1[:, kc, O_DT:O_DT + NH], kc == 0, kc == 7, [hT, V(W1, kc)], self.bv(4))
                f.op("dve", lambda e: e.tensor_tensor(out=dte[:], in0=self.PS[:, 4 * 512:4 * 512 + NH], in1=dtb[:],
                                                      op=ALU.add), reads=self.bv(4) + [dtb], writes=[dte])
                f.op("act", lambda e: e.activation(dte[:], dte[:], AF.Exp), reads=[dte], writes=[dte])
                f.op("act", lambda e: e.activation(dtt[:], dte[:], AF.Ln, bias=self.epsln[:, 2:3], scale=1.0),
                     reads=[dte, self.epsln], writes=[dtt])
                f.dma("sp", self.DTs[ti * 128:(ti + 1) * 128, :], dtt[:], reads=[dtt], writes=[V(self.DTs, ti)])
            for cc in range(12):
                bk = 5 + cc % 2
                for kc in range(8):
                    f.mm(self.PS[:, bk * 512: bk * 512 + TS], W1[:, kc, O_XBC + cc * 128: O_XBC + (cc + 1) * 128],
                         hT[:, kc, :], kc == 0, kc == 7, [hT, V(W1, kc)], self.bv(bk))
                f.op("act", lambda e: e.copy(xpre[:, cc, 3:3 + TS], self.PS[:, bk * 512: bk * 512 + TS]),
                     reads=self.bv(bk), writes=[V(xpre, cc)])
                f.op("dve", lambda e: e.tensor_scalar(acc[:], xpre[:, cc, 0:TS], CW[:, cc, 0:1], CBs[:, cc:cc + 1],
                                                      op0=ALU.mult, op1=ALU.add),
                     reads=[V(xpre, cc), CW, CBs], writes=[acc])
                for j in range(1, 4):
                    f.op("dve", lambda e: e.scalar_tensor_tensor(out=acc[:], in0=xpre[:, cc, j:j + TS],
                                                                 scalar=CW[:, cc, j:j + 1], in1=acc[:],
                                                                 op0=ALU.mult, op1=ALU.add),
                         reads=[V(xpre, cc), CW, acc], writes=[acc])
                f.op("act", lambda e: e.activation(xc[:, cc, :], acc[:], AF.Silu), reads=[acc], writes=[V(xc, cc)])
                f.op("dve", lambda e: e.tensor_copy(xpre[:, cc, 0:3], xpre[:, cc, TS:TS + 3]),
                     reads=[V(xpre, cc)], writes=[V(xpre, cc)])
            f.dma("sp", self.BT[:, :, u * TS:(u + 1) * TS], xc[:, 8:10, :], reads=[V(xc, 8), V(xc, 9)],
                  writes=[V(self.BT, u)])
            f.dma("sp", self.CTs[:, :, u * TS:(u + 1) * TS], xc[:, 10:12, :], reads=[V(xc, 10), V(xc, 11)],
                  writes=[V(self.CTs, u)])
            for i in range(TPU):
                ti = u * TPU + i
                xm = xtm[ti % 2]
                self.transposes(lambda c: xc[:, c, i * 128:(i + 1) * 128], 10,
                                lambda c0, n: xm[:, c0 * 128:(c0 + n) * 128].rearrange("p (c t) -> p c t", t=128),
                                0, [xc], [xm], evac="dve")
                f.dma("sp", self.X[ti * 128:(ti + 1) * 128, :], xm[:, 0:D], reads=[xm], writes=[V(self.X, ti)])
                f.dma("sp", self.BTM[ti * 128:(ti + 1) * 128, :], xm[:, D:D + 256], reads=[xm],
                      writes=[V(self.BTM, ti)])

    def phase_ssd(self, l):
        f, NT = self.f, self.NT
        PS = self.PS
        cU, cSL, ones = self.cU, self.cSL, self.ones
        Abc = self.bcast_load("s_A", self.a_log[l, :], NH)
        f.op("act", lambda e: e.activation(Abc[:], Abc[:], AF.Exp), reads=[Abc], writes=[Abc])
        f.op("dve", lambda e: e.tensor_scalar(Abc[:], Abc[:], -1.0, None, op0=ALU.mult), reads=[Abc], writes=[Abc])
        Dbc = self.bcast_load("s_D", self.d_skip[l, :], NH)
        NGb = self.bcast_load("s_ng", self.ssd_norm_g[l, :], D)
        ST = f.sb("s_ST", [128, 2, 512])
        f.op("dve", lambda e: e.memset(ST[:], 0.0), writes=[ST])
        xt = [f.sb("s_x%d" % i, [128, NH, HP]) for i in range(2)]
        zt = [f.sb("s_z%d" % i, [128, D]) for i in range(2)]
        dtt = [f.sb("s_dt%d" % i, [128, NH]) for i in range(2)]
        bct = [f.sb("s_bc%d" % i, [128, 4, 128]) for i in range(2)]
        btm = [f.sb("s_bm%d" % i, [128, 256]) for i in range(2)]
        a = f.sb("s_a", [128, NH]); acum = f.sb("s_acum", [128, NH]); eacum = f.sb("s_eacum", [128, NH])
        alast = f.sb("s_alast", [128, NH]); ealast = f.sb("s_ealast", [128, NH]); dend = f.sb("s_dend", [128, NH])
        xdt = f.sb("s_xdt", [128, NH, HP]); xdec = f.sb("s_xdec", [128, NH, HP])
        mCB = f.sb("s_mcb", [128, 128])
        Wa = [f.sb("s_wa%d" % i, [128, 4, 128]) for i in range(2)]
        LT = [f.sb("s_lt%d" % i, [128, 4, 128]) for i in range(2)]
        G = [f.sb("s_g%d" % i, [128, 4, 128]) for i in range(2)]
        y = f.sb("s_y", [128, NH, HP]); yo = f.sb("s_yo", [128, 8, HP])
        sz = f.sb("s_sz", [128, D]); ss = f.sb("s_ss", [128, 4]); junk = f.sb("s_junk", [128, 512])
        yout = [f.sb("s_yout%d" % i, [128, D]) for i in range(2)]
        for c in range(NT):
            x_, z_, d_, bc_, bm_ = xt[c % 2], zt[c % 2], dtt[c % 2], bct[c % 2], btm[c % 2]
            tok = slice(c * 128, (c + 1) * 128)
            f.dma("sp", x_[:].rearrange("p h d -> p (h d)"), self.X[tok, :], reads=[V(self.X, c)], writes=[x_])
            f.dma("sp", z_[:], self.Z[tok, :], reads=[V(self.Z, c)], writes=[z_])
            f.dma("sp", d_[:], self.DTs[tok, :], reads=[V(self.DTs, c)], writes=[d_])
            f.dma("sp", bc_[:, 0:2, :], self.BT[:, :, tok], reads=[self.BT], writes=[V(bc_, 0)])
            f.dma("sp", bc_[:, 2:4, :], self.CTs[:, :, tok], reads=[self.CTs], writes=[V(bc_, 1)])
            f.dma("sp", bm_[:], self.BTM[tok, :], reads=[V(self.BTM, c)], writes=[bm_])
            f.op("dve", lambda e: e.tensor_tensor(out=a[:], in0=d_[:], in1=Abc[:], op=ALU.mult),
                 reads=[d_, Abc], writes=[a])
            f.mm(PS[:, 0:NH], cU[:], a[:], True, True, [cU, a], self.bv(0))
            f.mm(PS[:, 512:512 + NH], ones[:], a[:], True, True, [ones, a], self.bv(1))
            f.op("dve", lambda e: e.tensor_copy(acum[:], PS[:, 0:NH]), reads=self.bv(0), writes=[acum])
            f.op("act", lambda e: e.activation(eacum[:], PS[:, 0:NH], AF.Exp), reads=self.bv(0), writes=[eacum])
            f.op("dve", lambda e: e.tensor_tensor(out=dend[:], in0=PS[:, 512:512 + NH], in1=acum[:], op=ALU.subtract),
                 reads=self.bv(1) + [acum], writes=[dend])
            f.op("act", lambda e: e.activation(dend[:], dend[:], AF.Exp), reads=[dend], writes=[dend])
            f.op("act", lambda e: e.activation(ealast[:], PS[:, 512:512 + NH], AF.Exp), reads=self.bv(1), writes=[ealast])
            f.op("dve", lambda e: e.tensor_tensor(out=xdt[:], in0=x_[:], in1=d_[:].unsqueeze(2).to_broadcast([128, NH, HP]),
                                                  op=ALU.mult), reads=[x_, d_], writes=[xdt])
            f.op("dve", lambda e: e.tensor_tensor(out=xdec[:], in0=xdt[:],
                                                  in1=dend[:].unsqueeze(2).to_broadcast([128, NH, HP]), op=ALU.mult),
                 reads=[xdt, dend], writes=[xdec])
            for g in range(2):
                f.mm(PS[:, 2 * 512:2 * 512 + 128], bc_[:, g, :], bc_[:, 2 + g, :], True, True, [bc_], self.bv(2))
                f.op("dve", lambda e: e.tensor_tensor(out=mCB[:], in0=PS[:, 2 * 512:2 * 512 + 128], in1=cU[:], op=ALU.mult),
                     reads=self.bv(2) + [cU], writes=[mCB])
                f.mm(self.bank(3), bc_[:, 2 + g, :], ST[:, g, :], True, True, [bc_, V(ST, g)], self.bv(3))
                for sg in range(2):
                    h0 = g * 8 + sg * 4
                    wa, lt, gg = Wa[sg], LT[sg], G[sg]
                    f.op("dve", lambda e: e.tensor_tensor(out=wa[:], in0=cSL[:].unsqueeze(1).to_broadcast([128, 4, 128]),
                                                          in1=a[:, h0:h0 + 4].unsqueeze(2).to_broadcast([128, 4, 128]),
                                                          op=ALU.mult), reads=[cSL, a], writes=[wa])
                    bk = 4 + sg
                    for hh in range(4):
                        f.mm(PS[:, bk * 512 + hh * 128: bk * 512 + (hh + 1) * 128], wa[:, hh, :], cU[:], True, True,
                             [wa, cU], self.bv(bk))
                    f.op("act", lambda e: e.activation(lt[:].rearrange("p h l -> p (h l)"), self.bank(bk), AF.Exp),
                         reads=self.bv(bk), writes=[lt])
                    f.op("dve", lambda e: e.tensor_tensor(out=gg[:], in0=lt[:],
                                                          in1=mCB[:].unsqueeze(1).to_broadcast([128, 4, 128]),
                                                          op=ALU.mult), reads=[lt, mCB], writes=[gg])
                    for hh in range(4):
                        h = h0 + hh
                        hl = sg * 4 + hh
                        f.mm(PS[:, 6 * 512 + hl * 64: 6 * 512 + (hl + 1) * 64], gg[:, hh, :], xdt[:, h, :], True, True,
                             [gg, xdt], self.bv(6))
                f.op("dve", lambda e: e.tensor_tensor(
                    out=yo[:], in0=self.bank(3).rearrange("p (h d) -> p h d", d=HP),
                    in1=eacum[:, g * 8:(g + 1) * 8].unsqueeze(2).to_broadcast([128, 8, HP]), op=ALU.mult),
                    reads=self.bv(3) + [eacum], writes=[yo])
                f.op("dve", lambda e: e.tensor_tensor(
                    out=y[:, g * 8:(g + 1) * 8, :], in0=self.bank(6).rearrange("p (h d) -> p h d", d=HP),
                    in1=yo[:], op=ALU.add), reads=self.bv(6) + [yo], writes=[V(y, g)])
                f.mm(self.bank(7), bm_[:, g * 128:(g + 1) * 128], xdec[:, g * 8:(g + 1) * 8, :].rearrange("p h d -> p (h d)"),
                     True, True, [bm_, xdec], self.bv(7))
                f.op("dve", lambda e: e.tensor_tensor(
                    out=ST[:, g, :].rearrange("p (h d) -> p h d", d=HP),
                    in0=ST[:, g, :].rearrange("p (h d) -> p h d", d=HP),
                    in1=ealast[:, g * 8:(g + 1) * 8].unsqueeze(2).to_broadcast([128, 8, HP]), op=ALU.mult),
                    reads=[V(ST, g), ealast], writes=[V(ST, g)])
                f.op("dve", lambda e: e.tensor_tensor(out=ST[:, g, :], in0=ST[:, g, :], in1=self.bank(7), op=ALU.add),
                     reads=[V(ST, g)] + self.bv(7), writes=[V(ST, g)])
            f.op("dve", lambda e: e.tensor_tensor(out=xdt[:], in0=x_[:],
                                                  in1=Dbc[:].unsqueeze(2).to_broadcast([128, NH, HP]), op=ALU.mult),
                 reads=[x_, Dbc], writes=[xdt])
            f.op("dve", lambda e: e.tensor_tensor(out=y[:], in0=y[:], in1=xdt[:], op=ALU.add),
                 reads=[y, xdt], writes=[y])
            f.op("act", lambda e: e.activation(sz[:], z_[:], AF.Silu), reads=[z_], writes=[sz])
            yf = y[:].rearrange("p h d -> p (h d)")
            f.op("dve", lambda e: e.tensor_tensor(out=sz[:], in0=sz[:], in1=yf, op=ALU.mult), reads=[sz, y], writes=[sz])
            for g in range(2):
                f.op("act", lambda e: e.activation(junk[:], sz[:, g * 512:(g + 1) * 512], AF.Square,
                                                   accum_out=ss[:, g:g + 1]), reads=[sz], writes=[junk, V(ss, g)])
            f.op("act", lambda e: e.activation(ss[:, 2:4], ss[:, 0:2], AF.Sqrt, bias=self.epsln[:, 1:2], scale=1.0 / 512),
                 reads=[ss, self.epsln], writes=[ss])
            f.op("dve", lambda e: e.reciprocal(ss[:, 2:4], ss[:, 2:4]), reads=[ss], writes=[ss])
            yo_ = yout[c % 2]
            for g in range(2):
                f.op("dve", lambda e: e.scalar_tensor_tensor(out=yo_[:, g * 512:(g + 1) * 512], in0=sz[:, g * 512:(g + 1) * 512],
                                                             scalar=ss[:, 2 + g:3 + g], in1=NGb[:, g * 512:(g + 1) * 512],
                                                             op0=ALU.mult, op1=ALU.mult),
                     reads=[sz, ss, NGb], writes=[yo_])
            f.dma("sp", self.MIX[tok, 0:D], yo_[:], reads=[yo_], writes=[V(self.MIX, ("ssd", c))])

    def phase_a2(self, l):
        f, TS, TPU, NU = self.f, self.TS, self.TPU, self.NU
        PS = self.PS
        NW = N_IN - O_QL
        oQL, oCKV, oQI, oKI, oWI, oQM = 0, O_CKV - O_QL, O_QI - O_QL, O_KI - O_QL, O_WI - O_QL, O_QM - O_QL
        W2 = f.sb("a2_W", [128, 8, NW])
        WK = f.sb("a2_WK", [128, 8, 128])
        for kc in range(8):
            f.dma("sp", W2[:, kc, :], self.w_in[l, kc * 128:(kc + 1) * 128, O_QL:N_IN], writes=[V(W2, kc)])
            for hf in range(2):
                f.dma("sp", WK[:, kc, hf * 64:(hf + 1) * 64], self.w_in[l, kc * 128:(kc + 1) * 128, O_KI:O_KI + 64],
                      writes=[V(WK, (kc, hf))])
        W2b = f.sb("a2_Wb", [128, 8, NW], BF16)
        for kc in range(8):
            f.op("pool", lambda e: e.tensor_copy(W2b[:, kc, :], W2[:, kc, :]), reads=[V(W2, kc)], writes=[V(W2b, kc)])
        KVG = self.bcast_load("a2_kvg", self.kv_norm_g[l, :], DLAT)
        hT = f.sb("a2_hT", [128, 8, TS])
        hTb = f.sb("a2_hTb", [128, 8, TS], BF16)
        htiles = [f.sb("a2_h%d" % i, [128, D]) for i in range(2)]
        qst = f.sb("a2_qst", [128, TPU, 8, 128], BF16)
        qist = f.sb("a2_qist", [128, 2, TS]); kist = f.sb("a2_kist", [128, TS])
        qmst = f.sb("a2_qmst", [128, 4, TS]); ctst = f.sb("a2_ctst", [128, TS], BF16)
        cvb = [f.sb("a2_cvb%d" % i, [128, 129], BF16) for i in range(2)]
        cv = [f.sb("a2_cv%d" % i, [128, 129]) for i in range(2)]
        wit = [f.sb("a2_wi%d" % i, [128, 4]) for i in range(2)]
        ss = f.sb("a2_ss", [128, 4]); junk = f.sb("a2_junk", [128, 128])
        for c_ in cv:
            f.op("dve", lambda e: e.memset(c_[:, 128:129], 1.0), writes=[V(c_, "one")])
        sc_lat = DLAT ** -0.5
        sc_mem = MEM_D ** -0.5
        nb = 0
        for u in range(NU):
            self.load_hT(hT, u, htiles, hTb)
            tsl = slice(u * TS, (u + 1) * TS)

            def fm_chunk(wcols, dst, scale, wr, lowp=False):
                nonlocal nb
                bk = 2 + nb % 2
                nb += 1
                hsrc = hTb if lowp else hT
                for kc in range(8):
                    f.mm(PS[:, bk * 512: bk * 512 + TS], wcols(kc), hsrc[:, kc, :], kc == 0, kc == 7,
                         [hsrc, W2, W2b, WK], self.bv(bk))
                src = PS[:, bk * 512: bk * 512 + TS]
                if len(dst.shape) == 3:
                    src = src.rearrange("p (i t) -> p i t", t=128)
                if scale is None:
                    f.op("act", lambda e: e.copy(dst, src), reads=self.bv(bk), writes=wr)
                else:
                    f.op("act", lambda e: e.activation(dst, src, AF.Copy, scale=scale), reads=self.bv(bk), writes=wr)

            for h in range(8):
                fm_chunk(lambda kc: W2b[:, kc, oQL + h * 128: oQL + (h + 1) * 128],
                         qst[:, :, h, :], sc_lat, [V(qst, h)], lowp=True)
            f.dma("sp", self.QL[:, u * TPU:(u + 1) * TPU, :], qst[:].rearrange("p i h t -> p i (h t)"),
                  reads=[qst], writes=[V(self.QL, u)])
            for j in range(2):
                fm_chunk(lambda kc: W2[:, kc, oQI + j * 128: oQI + (j + 1) * 128], qist[:, j, :], None, [V(qist, j)])
            f.dma("sp", self.QI[:, :, tsl], qist[:], reads=[qist], writes=[V(self.QI, u)])
            fm_chunk(lambda kc: WK[:, kc, :], kist[:], None, [kist])
            f.dma("sp", self.KI[:, tsl], kist[:], reads=[kist], writes=[V(self.KI, u)])
            for h in range(4):
                fm_chunk(lambda kc: W2b[:, kc, oQM + h * 128: oQM + (h + 1) * 128], qmst[:, h, :], sc_mem, [V(qmst, h)], lowp=True)
            f.dma("sp", self.QM[:, :, tsl], qmst[:], reads=[qmst], writes=[V(self.QM, u)])
            for i in range(TPU):
                ti = u * TPU + i
                tok = slice(ti * 128, (ti + 1) * 128)
                c_, w_ = cv[ti % 2], wit[ti % 2]
                for kc in range(8):
                    f.mm(PS[:, 4 * 512: 4 * 512 + 128], hTb[:, kc, i * 128:(i + 1) * 128], W2b[:, kc, oCKV:oCKV + 128],
                         kc == 0, kc == 7, [hTb, W2b], self.bv(4))
                for kc in range(8):
                    f.mm(PS[:, 5 * 512: 5 * 512 + 4], hT[:, kc, i * 128:(i + 1) * 128], W2[:, kc, oWI:oWI + 4],
                         kc == 0, kc == 7, [hT, W2], self.bv(5))
                f.op("act", lambda e: e.copy(w_[:], PS[:, 5 * 512: 5 * 512 + 4]), reads=self.bv(5), writes=[w_])
                f.dma("sp", self.WI[tok, :], w_[:], reads=[w_], writes=[V(self.WI, ti)])
                f.op("act", lambda e: e.activation(junk[:], PS[:, 4 * 512: 4 * 512 + 128], AF.Square, accum_out=ss[:, 0:1]),
                     reads=self.bv(4), writes=[junk, ss])
                f.op("act", lambda e: e.activation(ss[:, 1:2], ss[:, 0:1], AF.Sqrt, bias=self.epsln[:, 1:2], scale=1.0 / DLAT),
                     reads=[ss, self.epsln], writes=[ss])
                f.op("dve", lambda e: e.reciprocal(ss[:, 2:3], ss[:, 1:2]), reads=[ss], writes=[ss])
                f.op("dve", lambda e: e.scalar_tensor_tensor(out=c_[:, 0:128], in0=PS[:, 4 * 512: 4 * 512 + 128],
                                                             scalar=ss[:, 2:3], in1=KVG[:], op0=ALU.mult, op1=ALU.mult),
                     reads=self.bv(4) + [ss, KVG], writes=[V(c_, "c")])
                cb_ = cvb[ti % 2]
                f.op("act", lambda e: e.copy(cb_[:], c_[:]), reads=[c_], writes=[cb_])
                f.dma("sp", self.CV[tok, :], cb_[:], reads=[cb_], writes=[V(self.CV, ti)])
                f.tr(PS[:, 6 * 512: 6 * 512 + 128], c_[:, 0:128], self.ident[:], [V(c_, "c"), self.ident], self.bv(6))
                f.op("act", lambda e: e.copy(ctst[:, i * 128:(i + 1) * 128], PS[:, 6 * 512: 6 * 512 + 128]),
                     reads=self.bv(6), writes=[V(ctst, i)])
            f.dma("sp", self.CT[:, tsl], ctst[:], reads=[ctst], writes=[V(self.CT, u)])

    def phase_dsa(self, l):
        f, S, NT, NIT, NSEL = self.f, self.S, self.NT, self.NIT, self.NSEL
        PS = self.PS
        CTr = f.sb("d_CT", [128, S], BF16); EPSP = f.sb("d_eps", [128, S])
        VAt = [f.sb("d_VA%d" % i, [128, 129], BF16) for i in range(4)]
        f.dma("sp", CTr[:], self.CT[:], reads=[self.CT], writes=[CTr])
        f.dma("sp", EPSP[:], self.cin["c_epspos"][:], writes=[EPSP])
        WUV = f.sb("d_wuv", [128, 8, 64])
        f.dma("sp", WUV[:], self.w_uv[l].rearrange("h c d -> c h d"), writes=[WUV])
        I4 = f.sb("d_i4", [128, 512], BF16); CBM = f.sb("d_cbm", [128, 128]); ONEI = f.sb("d_onei", [65, 128], BF16)
        NSL = f.sb("d_nsl", [33, 1024])
        f.dma("sp", NSL[:], self.cin["c_negsl33"][:], writes=[NSL])
        POW2 = f.sb("d_pow2", [128, 64])
        f.dma("sp", I4[:], self.cin["c_i4"][:], writes=[I4])
        f.dma("sp", CBM[:], self.cin["c_cbm"][:], writes=[CBM])
        f.dma("sp", ONEI[:], self.cin["c_onei"][:], writes=[ONEI])
        f.dma("sp", POW2[:], self.cin["c_pow2"][:], writes=[POW2])
        RB = [f.sb("d_rb%d" % i, [65, 1024], BF16) for i in range(2)]
        SL128 = f.sb("d_sl128", [1, 1024])
        f.dma("sp", SL128[:], self.cin["c_sl128"][:], writes=[SL128])
        for r in RB:
            f.dma("sp", r[:], self.cin["c_rbinit"][:], writes=[r])
        smrep = f.sb("d_smrep", [128, 33])
        srow = f.sb("d_srow", [33, 4, 128]); srow_i = f.sb("d_srowi", [33, 2, 128], I32)
        SC = f.sb("d_SC", [128, S]); MB = f.sb("d_MB", [128, S], BF16)
        Pt = [f.sb("d_P%d" % i, [128, 1024], BF16) for i in range(2)]
        QLb = [f.sb("d_ql%d" % i, [128, 1024], BF16) for i in range(2)]
        QIb = [f.sb("d_qi%d" % i, [128, 2, 128]) for i in range(2)]
        WIb = [f.sb("d_wi%d" % i, [128, 4]) for i in range(2)]
        KIs = [f.sb("d_ki%d" % i, [128, 512]) for i in range(2)]
        rl = [f.sb("d_rl%d" % i, [128, 512]) for i in range(2)]
        sm = f.sb("d_sm", [128, 16])
        hs = f.sb("d_hs", [128, 64])
        ctx = f.sb("d_ctx", [128, 8, 128]); ctxT = f.sb("d_ctxT", [128, 8, 128])
        rinv = f.sb("d_rinv", [128, 8]); yd = [f.sb("d_yd%d" % i, [128, 512]) for i in range(2)]
        ACC = PS[:, 4 * 512: 8 * 512].rearrange("p (h c) -> p h c", c=256)
        SCs = [SC, f.sb("d_SC1", [128, S])]
        junk = f.sb("d_junk", [128, S], BF16)
        nrl = 0
        nkt = 0

        def scores(b):
            nonlocal nrl
            SC = SCs[b % 2]
            t0 = b * 128
            te = t0 + 128
            qi, wi = QIb[b % 2], WIb[b % 2]
            f.dma("sp", qi[:], self.QI[:, :, t0:te], reads=[self.QI], writes=[qi])
            f.dma("sp", wi[:], self.WI[t0:te, :], reads=[self.WI], writes=[wi])
            nsl = (te + 511) // 512
            for si in range(nsl):
                c0 = si * 512
                ncol = min(512, te - c0)
                ks = KIs[si % 2]
                f.dma("sp", ks[:, 0:ncol], self.KI[:, c0:c0 + ncol], reads=[self.KI], writes=[ks])
                for hi in range(4):
                    bk = nrl % 2
                    r_ = rl[nrl % 2]
                    nrl += 1
                    pb = (hi % 2) * 64
                    f.mm(PS[:, bk * 512: bk * 512 + ncol], qi[pb:pb + 64, hi // 2, :].bitcast(mybir.dt.float32r),
                         ks[pb:pb + 64, 0:ncol].bitcast(mybir.dt.float32r), True, True, [qi, ks], self.bv(bk))
                    f.op("act", lambda e: e.activation(r_[:, 0:ncol], PS[:, bk * 512: bk * 512 + ncol], AF.Relu),
                         reads=self.bv(bk), writes=[r_])
                    prev = EPSP if hi == 0 else SC
                    f.op("dve", lambda e: e.scalar_tensor_tensor(out=SC[:, c0:c0 + ncol], in0=r_[:, 0:ncol],
                                                                 scalar=wi[:, hi:hi + 1], in1=prev[:, c0:c0 + ncol],
                                                                 op0=ALU.mult, op1=ALU.add),
                         reads=[r_, wi, EPSP, SC], writes=[SC])

        def bisect(b):
            SC = SCs[b % 2]
            t0 = b * 128
            te = t0 + 128
            f.op("dve", lambda e: e.tensor_reduce(out=sm[:, 0:1], in_=SC[:, 0:te], axis=AX.X, op=ALU.min),
                 reads=[SC], writes=[V(sm, 0)])
            f.op("dve", lambda e: e.tensor_tensor(out=SC[:, t0:te], in0=SC[:, t0:te], in1=CBM[:], op=ALU.add),
                 reads=[SC, CBM], writes=[SC])
            f.op("dve", lambda e: e.tensor_reduce(out=sm[:, 1:2], in_=SC[:, 0:te], axis=AX.X, op=ALU.max),
                 reads=[SC], writes=[V(sm, 1)])
            f.op("dve", lambda e: e.tensor_tensor(out=sm[:, 7:8], in0=sm[:, 1:2], in1=sm[:, 0:1], op=ALU.subtract),
                 reads=[V(sm, 0), V(sm, 1)], writes=[V(sm, 7)])
            f.op("dve", lambda e: e.tensor_scalar(sm[:, 7:8], sm[:, 7:8], 1.001, 1e-6, op0=ALU.mult, op1=ALU.add),
                 reads=[V(sm, 7)], writes=[V(sm, 7)])
            f.op("dve", lambda e: e.tensor_scalar(hs[:, 0:NIT], POW2[:, 0:NIT], sm[:, 7:8], None, op0=ALU.mult),
                 reads=[POW2, V(sm, 7)], writes=[hs])
            f.op("dve", lambda e: e.tensor_copy(sm[:, 2:3], sm[:, 0:1]), reads=[V(sm, 0)], writes=[V(sm, 2)])
            for it in range(NIT):
                f.op("dve", lambda e: e.tensor_tensor(out=sm[:, 3:4], in0=sm[:, 2:3], in1=hs[:, it:it + 1], op=ALU.add),
                     reads=[V(sm, 2), hs], writes=[V(sm, 3)])
                f.op("dve", lambda e: e.tensor_scalar(junk[:, 0:te], SC[:, 0:te], sm[:, 3:4], 0.0, op0=ALU.is_ge, op1=ALU.add,
                                                      accum_out=sm[:, 4:5]), reads=[SC, V(sm, 3)], writes=[junk, V(sm, 4)])
                f.op("dve", lambda e: e.tensor_scalar(sm[:, 5:6], sm[:, 4:5], float(NSEL) - 0.5, hs[:, it:it + 1],
                                                      op0=ALU.is_ge, op1=ALU.mult), reads=[V(sm, 4), hs], writes=[V(sm, 5)])
                f.op("dve", lambda e: e.tensor_tensor(out=sm[:, 2:3], in0=sm[:, 2:3], in1=sm[:, 5:6], op=ALU.add),
                     reads=[V(sm, 2), V(sm, 5)], writes=[V(sm, 2)])
            f.op("dve", lambda e: e.tensor_scalar(MB[:, 0:te], SC[:, 0:te], sm[:, 2:3], NEG, op0=ALU.is_lt, op1=ALU.mult),
                 reads=[SC, V(sm, 2)], writes=[MB])
            f.op("dve", lambda e: e.tensor_tensor(out=SC[:, 0:te], in0=MB[:, 0:te], in1=EPSP[:, 0:te], op=ALU.subtract),
                 reads=[EPSP, MB], writes=[SC])
            f.op("dve", lambda e: e.tensor_reduce(out=sm[:, 6:7], in_=SC[:, 0:te], axis=AX.X, op=ALU.max),
                 reads=[SC], writes=[V(sm, 6)])
            f.op("dve", lambda e: e.tensor_copy(smrep[:], sm[:, 6:7].to_broadcast([128, 33])), reads=[V(sm, 6)], writes=[smrep])
            f.tr(PS[0:33, 0:128], smrep[:], self.ident[:], [smrep, self.ident], self.bv(0))
            f.op("dve", lambda e: e.tensor_scalar(srow[:, 0, :], PS[0:33, 0:128], 2.0 ** 30, None, op0=ALU.mult),
                 reads=self.bv(0), writes=[V(srow, 0)])
            f.op("dve", lambda e: e.tensor_copy(srow_i[:, 0, :], srow[:, 0, :]), reads=[V(srow, 0)], writes=[V(srow_i, 0)])
            f.op("dve", lambda e: e.tensor_scalar(srow_i[:, 1, :], srow_i[:, 0, :], 127, None, op0=ALU.bitwise_and),
                 reads=[V(srow_i, 0)], writes=[V(srow_i, 1)])
            f.op("dve", lambda e: e.tensor_copy(srow[:, 1, :], srow_i[:, 1, :]), reads=[V(srow_i, 1)], writes=[V(srow, 1)])
            f.op("dve", lambda e: e.tensor_tensor(out=srow[:, 2, :], in0=srow[:, 0, :], in1=srow[:, 1, :], op=ALU.subtract),
                 reads=[V(srow, 0), V(srow, 1)], writes=[V(srow, 2)])
            rb0 = RB[nkt % 2]
            f.op("dve", lambda e: e.tensor_tensor(out=rb0[0:1, :].rearrange("p (h t) -> p h t", t=128),
                                                  in0=NSL[0:1, :].rearrange("p (h t) -> p h t", t=128),
                                                  in1=srow[0:1, 2, :].unsqueeze(1).to_broadcast([1, 8, 128]), op=ALU.mult),
                 reads=[NSL, V(srow, 2)], writes=[V(rb0, 0)])
            for r_ in RB:
                f.op("dve", lambda e: e.tensor_tensor(out=r_[32:33, :].rearrange("p (h t) -> p h t", t=128),
                                                      in0=NSL[32:33, :].rearrange("p (h t) -> p h t", t=128),
                                                      in1=srow[32:33, 1, :].unsqueeze(1).to_broadcast([1, 8, 128]), op=ALU.mult),
                     reads=[NSL, V(srow, 1)], writes=[V(r_, 32)])

        def attention(b):
            nonlocal nkt
            t0 = b * 128
            te = t0 + 128
            ql = QLb[b % 2]
            f.dma("sp", ql[:], self.QL[:, b, :], reads=[self.QL], writes=[ql])
            for j in range(b + 1):
                rb = RB[nkt % 2]
                p_ = Pt[nkt % 2]
                lb = 2
                nkt += 1
                va = VAt[nkt % 4]
                f.dma("sp", va[:], self.CV[j * 128:(j + 1) * 128, :], reads=[self.CV], writes=[va])
                if j > 0:
                    rbp = RB[(nkt - 2) % 2]
                    f.op("pool", lambda e: e.tensor_tensor(out=rb[0:1, :], in0=rbp[0:1, :], in1=SL128[:], op=ALU.add),
                         reads=[V(rbp, 0), SL128], writes=[V(rb, 0)])
                for g in range(2):
                    o_ = self.bank(lb + g)
                    f.mm(o_, CTr[:, j * 128:(j + 1) * 128], ql[:, g * 512:(g + 1) * 512], True, False, [CTr, ql], self.bv(lb + g))
                    f.mm(o_, ONEI[:], rb[:, g * 512:(g + 1) * 512], False, False, [ONEI, rb], self.bv(lb + g))
                    f.mm(o_, MB[:, j * 128:(j + 1) * 128], I4[:], False, True, [MB, I4], self.bv(lb + g))
                    f.op("act", lambda e: e.activation(p_[:, g * 512:(g + 1) * 512], o_, AF.Exp),
                         reads=self.bv(lb + g), writes=[V(p_, g)])
                for h in range(8):
                    f.mm(ACC[:, h, 0:129], p_[:, h * 128:(h + 1) * 128], va[:], (j == 0 and h % 2 == 0), j == b,
                         [V(p_, h // 4), va], self.bv(4 + h // 2))
            f.op("dve", lambda e: e.reciprocal(rinv[:], ACC[:, :, 128]), reads=self.bv(4, 4), writes=[rinv])
            f.op("dve", lambda e: e.tensor_tensor(out=ctx[:], in0=ACC[:, :, 0:128],
                                                  in1=rinv[:].unsqueeze(2).to_broadcast([128, 8, 128]), op=ALU.mult),
                 reads=self.bv(4, 4) + [rinv], writes=[ctx])
            self.transposes(lambda c: ctx[:, c, :], 8, lambda c0, n: ctxT[:, c0:c0 + n, :], 0, [ctx], [ctxT])
            for h in range(8):
                f.mm(PS[:, 2 * 512 + h * 64: 2 * 512 + (h + 1) * 64], ctxT[:, h, :], WUV[:, h, :], True, True,
                     [ctxT, WUV], self.bv(2))
            y_ = yd[b % 2]
            f.op("act", lambda e: e.copy(y_[:], self.bank(2)), reads=self.bv(2), writes=[y_])
            f.dma("sp", self.MIX[t0:te, D:D + 512], y_[:], reads=[y_], writes=[V(self.MIX, ("dsa", b))])


        scores(0)
        for b in range(NT):
            bisect(b)
            if b + 1 < NT:
                scores(b + 1)
            attention(b)

    def phase_mem(self, l):
        f, NT = self.f, self.NT
        PS = self.PS
        WKm = f.sb("m_wk", [128, 8, 512]); WVm = f.sb("m_wv", [128, 8, 512])
        f.dma("sp", WKm[:], self.w_mem_k[l].rearrange("(c p) n -> p c n", p=128), writes=[WKm])
        f.dma("sp", WVm[:], self.w_mem_v[l].rearrange("(c p) n -> p c n", p=128), writes=[WVm])
        memKT = f.sb("m_kT", [128, 4, MEM_LEN]); memV = f.sb("m_v", [128, 2, 512])
        self.memT = f.sb("m_memT", [128, 8, MEM_LEN])
        mt_ = f.sb("m_mem", [128, D])
        for mi in range(MEM_LEN // 128):
            f.dma("sp", mt_[:], self.mem_in[mi * 128:(mi + 1) * 128, :], writes=[mt_])
            self.transposes(lambda c: mt_[:, c * 128:(c + 1) * 128], 8,
                            lambda c0, n: self.memT[:, c0:c0 + n, mi * 128:(mi + 1) * 128], 0, [mt_], [self.memT])
        for h in range(4):
            for kc in range(8):
                f.mm(PS[:, 0:MEM_LEN], WKm[:, kc, h * 128:(h + 1) * 128], self.memT[:, kc, :], kc == 0, kc == 7,
                     [WKm, self.memT], self.bv(0))
            f.op("act", lambda e: e.copy(memKT[:, h, :], PS[:, 0:MEM_LEN]), reads=self.bv(0), writes=[memKT])
        for mt in range(2):
            for kc in range(8):
                f.mm(self.bank(1), self.memT[:, kc, mt * 128:(mt + 1) * 128], WVm[:, kc, :], kc == 0, kc == 7,
                     [WVm, self.memT], self.bv(1))
            f.op("act", lambda e: e.copy(memV[:, mt, :], self.bank(1)), reads=self.bv(1), writes=[memV])
        QMt = [f.sb("m_q%d" % i, [128, 4, 128]) for i in range(2)]
        Pm = f.sb("m_P", [128, MEM_LEN]); PT = f.sb("m_PT", [128, 2, 128])
        sm = f.sb("m_sm", [128, 4]); ym = [f.sb("m_y%d" % i, [128, 512]) for i in range(2)]
        for i in range(NT):
            q_, y_ = QMt[i % 2], ym[i % 2]
            tok = slice(i * 128, (i + 1) * 128)
            f.dma("sp", q_[:], self.QM[:, :, tok], reads=[self.QM], writes=[q_])
            for h in range(4):
                bk = 2 + h % 2
                f.mm(PS[:, bk * 512: bk * 512 + MEM_LEN], q_[:, h, :], memKT[:, h, :], True, True, [q_, memKT], self.bv(bk))
                f.op("dve", lambda e: e.tensor_reduce(out=sm[:, 0:1], in_=PS[:, bk * 512: bk * 512 + MEM_LEN], axis=AX.X,
                                                      op=ALU.max), reads=self.bv(bk), writes=[V(sm, 0)])
                f.op("dve", lambda e: e.tensor_scalar(sm[:, 1:2], sm[:, 0:1], -1.0, None, op0=ALU.mult),
                     reads=[V(sm, 0)], writes=[V(sm, 1)])
                f.op("act", lambda e: e.activation(Pm[:], PS[:, bk * 512: bk * 512 + MEM_LEN], AF.Exp, bias=sm[:, 1:2],
                                                   scale=1.0, accum_out=sm[:, 2:3]),
                     reads=self.bv(bk) + [V(sm, 1)], writes=[Pm, V(sm, 2)])
                f.op("dve", lambda e: e.reciprocal(sm[:, 3:4], sm[:, 2:3]), reads=[V(sm, 2)], writes=[V(sm, 3)])
                self.transposes(lambda c: Pm[:, c * 128:(c + 1) * 128], 2, lambda c0, n: PT[:, c0:c0 + n, :], 4, [Pm], [PT])
                for mt in range(2):
                    f.mm(PS[:, 6 * 512: 6 * 512 + 128], PT[:, mt, :], memV[:, mt, h * 128:(h + 1) * 128], mt == 0, mt == 1,
                         [PT, memV], self.bv(6))
                f.op("dve", lambda e: e.tensor_scalar(y_[:, h * 128:(h + 1) * 128], PS[:, 6 * 512: 6 * 512 + 128],
                                                      sm[:, 3:4], None, op0=ALU.mult),
                     reads=self.bv(6) + [V(sm, 3)], writes=[y_])
            f.dma("sp", self.MIX[tok, D + 512:D + 1024], y_[:], reads=[y_], writes=[V(self.MIX, ("mem", i))])

    def phase_outp(self, l):
        f, NT, CAP = self.f, self.NT, self.CAP
        PS = self.PS
        WO = f.sb("o_WO", [128, 16, D], BF16)
        WOs = [f.sb("o_WOs%d" % i, [128, D]) for i in range(2)]
        for kc in range(16):
            ws = WOs[kc % 2]
            f.dma("sp", ws[:], self.w_out[l, kc * 128:(kc + 1) * 128, :], writes=[ws])
            f.op("pool", lambda e: e.tensor_copy(WO[:, kc, :], ws[:]), reads=[ws], writes=[V(WO, kc)])
        RW = f.sb("o_RW", [128, 8, NE])
        f.dma("sp", RW[:], self.router_w[l].rearrange("(c p) n -> p c n", p=128), writes=[RW])
        RBb = self.bcast_load("o_rb", self.router_b[l, :], NE)
        g1 = self.bcast_load("o_g1", self.ln1_g[l, :], D)
        b1 = self.bcast_load("o_b1", self.ln1_b[l, :], D)
        ECAP = f.sb("o_ecap", [128, NE])
        f.dma("sp", ECAP[:], self.cin["c_eidx"][:], writes=[ECAP])
        f.op("dve", lambda e: e.tensor_scalar(ECAP[:], ECAP[:], float(CAP), None, op0=ALU.mult), reads=[ECAP], writes=[ECAP])
        base = f.sb("o_base", [128, NE])
        f.op("dve", lambda e: e.memset(base[:], 0.0), writes=[base])
        if l == 0:
            zt = f.sb("o_zero", [128, D])
            f.op("dve", lambda e: e.memset(zt[:], 0.0), writes=[zt])
            NR = CAP // 128
            for e_ in range(NE):
                f.dma("sp", self.XD[e_ * CAP:(e_ + 1) * CAP, :].rearrange("(r p) d -> p r d", p=128),
                      zt[:].unsqueeze(1).to_broadcast([128, NR, D]), reads=[zt], writes=[V(self.XD, ("z", e_))])
            f.barrier()
        mixt = [f.sb("o_mix%d" % i, [128, DMIX]) for i in range(2)]
        ht = [f.sb("o_h%d" % i, [128, D]) for i in range(2)]
        mixT = f.sb("o_mixT", [128, 16, 128], BF16)
        r_ = f.sb("o_r", [128, D]); scr = f.sb("o_scr", [128, D])
        h2t = [f.sb("o_h2%d" % i, [128, D]) for i in range(2)]
        h2T = f.sb("o_h2T", [128, 8, 128])
        stats = f.sb("o_st", [128, 12]); mv = f.sb("o_mv", [128, 4])
        lg = f.sb("o_lg", [128, NE]); m8 = f.sb("o_m8", [128, 8]); sel = f.sb("o_sel", [128, NE])
        e4 = f.sb("o_e4", [128, 4]); sm = f.sb("o_sm", [128, 4])
        g4 = [f.sb("o_g4%d" % i, [128, 4]) for i in range(2)]
        slotf = f.sb("o_slotf", [128, NE]); tmp = f.sb("o_tmp", [128, NE]); eq = f.sb("o_eq", [128, NE])
        junk = f.sb("o_junk", [128, NE])
        s4f = f.sb("o_s4f", [128, 4])
        s4i = [f.sb("o_s4i%d" % i, [128, 4], I32) for i in range(2)]
        for i in range(NT):
            tok = slice(i * 128, (i + 1) * 128)
            mx, h_, h2_, g4_, s4_ = mixt[i % 2], ht[i % 2], h2t[i % 2], g4[i % 2], s4i[i % 2]
            f.dma("sp", mx[:], self.MIX[tok, :], reads=[self.MIX], writes=[mx])
            f.dma("sp", h_[:], self.H[tok, :], reads=[V(self.H, i)], writes=[h_])
            self.transposes(lambda c: mx[:, c * 128:(c + 1) * 128], 16, lambda c0, n: mixT[:, c0:c0 + n, :], 0, [mx], [mixT])
            for sl in range(2):
                for kc in range(16):
                    f.mm(self.bank(2 + sl), mixT[:, kc, :], WO[:, kc, sl * 512:(sl + 1) * 512], kc == 0, kc == 15,
                         [mixT, V(WO, kc)], self.bv(2 + sl))
                f.op("dve", lambda e: e.scalar_tensor_tensor(out=r_[:, sl * 512:(sl + 1) * 512], in0=h_[:, sl * 512:(sl + 1) * 512],
                                                             scalar=float(ALPHA), in1=self.bank(2 + sl), op0=ALU.mult, op1=ALU.add),
                     reads=[h_] + self.bv(2 + sl), writes=[r_])
            self.ln_tile(r_, h2_, g1, b1, scr, stats, mv)
            f.dma("sp", self.H2[tok, :], h2_[:], reads=[h2_], writes=[V(self.H2, i)])
            self.transposes(lambda c: h2_[:, c * 128:(c + 1) * 128], 8, lambda c0, n: h2T[:, c0:c0 + n, :], 0, [h2_], [h2T])
            for kc in range(8):
                f.mm(PS[:, 4 * 512: 4 * 512 + NE], h2T[:, kc, :], RW[:, kc, :], kc == 0, kc == 7, [h2T, RW], self.bv(4))
            f.op("dve", lambda e: e.tensor_tensor(out=lg[:], in0=PS[:, 4 * 512: 4 * 512 + NE], in1=RBb[:], op=ALU.add),
                 reads=self.bv(4) + [RBb], writes=[lg])
            f.op("dve", lambda e: e.max(out=m8[:], in_=lg[:]), reads=[lg], writes=[m8])
            f.op("dve", lambda e: e.tensor_scalar(sel[:], lg[:], m8[:, 3:4], None, op0=ALU.is_ge), reads=[lg, m8], writes=[sel])
            f.op("dve", lambda e: e.tensor_scalar(sm[:, 0:1], m8[:, 0:1], -1.0, None, op0=ALU.mult), reads=[m8], writes=[V(sm, 0)])
            f.op("act", lambda e: e.activation(e4[:], m8[:, 0:4], AF.Exp, bias=sm[:, 0:1], scale=1.0, accum_out=sm[:, 1:2]),
                 reads=[m8, V(sm, 0)], writes=[e4, V(sm, 1)])
            f.op("dve", lambda e: e.reciprocal(sm[:, 2:3], sm[:, 1:2]), reads=[V(sm, 1)], writes=[V(sm, 2)])
            f.op("dve", lambda e: e.tensor_scalar(g4_[:], e4[:], sm[:, 2:3], None, op0=ALU.mult), reads=[e4, V(sm, 2)], writes=[g4_])
            f.dma("sp", self.GATE4[tok, :], g4_[:], reads=[g4_], writes=[V(self.GATE4, i)])
            f.mm(PS[:, 5 * 512: 5 * 512 + NE], self.cSU[:], sel[:], True, True, [self.cSU, sel], self.bv(5))
            f.mm(PS[:, 6 * 512: 6 * 512 + NE], self.ones[:], sel[:], True, True, [self.ones, sel], self.bv(6))
            f.op("dve", lambda e: e.tensor_tensor(out=tmp[:], in0=PS[:, 5 * 512: 5 * 512 + NE], in1=base[:], op=ALU.add),
                 reads=self.bv(5) + [base], writes=[tmp])
            f.op("dve", lambda e: e.tensor_tensor(out=slotf[:], in0=tmp[:], in1=ECAP[:], op=ALU.add), reads=[tmp, ECAP], writes=[slotf])
            f.op("dve", lambda e: e.tensor_scalar(tmp[:], tmp[:], float(CAP) - 0.5, 1.0e9, op0=ALU.is_ge, op1=ALU.mult),
                 reads=[tmp], writes=[tmp])
            f.op("dve", lambda e: e.tensor_tensor(out=slotf[:], in0=slotf[:], in1=tmp[:], op=ALU.add), reads=[slotf, tmp], writes=[slotf])
            f.op("dve", lambda e: e.tensor_tensor(out=base[:], in0=base[:], in1=PS[:, 6 * 512: 6 * 512 + NE], op=ALU.add),
                 reads=[base] + self.bv(6), writes=[base])
            for k in range(4):
                f.op("dve", lambda e: e.scalar_tensor_tensor(out=junk[:], in0=lg[:], scalar=m8[:, k:k + 1], in1=slotf[:],
                                                             op0=ALU.is_equal, op1=ALU.mult, accum_out=s4f[:, k:k + 1]),
                     reads=[lg, m8, slotf], writes=[junk, V(s4f, k)])
            f.op("dve", lambda e: e.tensor_copy(s4_[:], s4f[:]), reads=[s4f], writes=[s4_])
            f.dma("sp", self.SLOT4[tok, :], s4_[:], reads=[s4_], writes=[V(self.SLOT4, i)])
            for k in range(4):
                f.dma("pool", self.XD[:, :], h2_[:, :], reads=[h2_, s4_], writes=[V(self.XD, ("s", i, k))],
                      indirect=dict(out_offset=bass.IndirectOffsetOnAxis(ap=s4_[:, k:k + 1], axis=0), in_offset=None,
                                    bounds_check=self.bc_reg(), oob_is_err=False))

    def phase_moe(self, l):
        f, CAP = self.f, self.CAP
        PS = self.PS
        NR = CAP // 128
        BGall = f.sb("e_bgall", [128, 16, NE])
        with ExitStack() as tmpst:
            old_st = f.stack
            f.stack = tmpst
            bgl = f.sb("e_bgl", [NE, 2 * DFF])
            f.dma("sp", bgl[:], self.b_gu[l], writes=[bgl])
            for c in range(16):
                bk = c % 2
                f.tr(PS[0:128, bk * 512: bk * 512 + NE], bgl[:, c * 128:(c + 1) * 128], self.ident[0:NE, 0:NE],
                     [bgl, self.ident], self.bv(bk))
                f.op("act", lambda e: e.copy(BGall[:, c, :], PS[:, bk * 512: bk * 512 + NE]), reads=self.bv(bk), writes=[V(BGall, c)])
            f.barrier()
            f.stack = old_st
        XT = f.sb("e_XT", [128, 8, CAP], BF16); AT = f.sb("e_AT", [128, 8, CAP], BF16)
        WD = [f.sb("e_WD%d" % i, [128, 8, D], BF16) for i in range(2)]
        WDs = f.sb("e_WDs", [128, 8, D])
        WG = [f.sb("e_WG%d" % i, [128, 8, 256], BF16) for i in range(2)]
        WGs = [f.sb("e_WGs%d" % i, [128, 8, 256]) for i in range(2)]
        xr = [f.sb("e_xr%d" % i, [128, D]) for i in range(2)]
        yr = [f.sb("e_yr%d" % i, [128, D]) for i in range(2)]
        BD = [f.sb("e_BD%d" % i, [128, D]) for i in range(2)]
        tg = f.sb("e_tg", [128, 512]); tsg = f.sb("e_tsg", [128, 512]); tl = f.sb("e_tl", [128, 512])
        slabs = [(s0, min(512, CAP - s0)) for s0 in range(0, CAP, 512)]
        nwg = 0
        nps = 0
        nrow = 0
        for e_ in range(NE):
            wd, bd = WD[e_ % 2], BD[e_ % 2]
            f.dma("sp", bd[:], self.b_down[l, e_, :].partition_broadcast(128), writes=[bd])
            for r in range(NR):
                x_ = xr[nrow % 2]
                nrow += 1
                f.dma("sp", x_[:], self.XD[e_ * CAP + r * 128: e_ * CAP + (r + 1) * 128, :], reads=[self.XD], writes=[x_])
                self.transposes(lambda c: x_[:, c * 128:(c + 1) * 128], 8, lambda c0, n: XT[:, c0:c0 + n, r * 128:(r + 1) * 128],
                                0, [x_], [V(XT, r)])
            for j in range(8):
                if j == 3:
                    f.dma("sp", WDs[:], self.w_down[l, e_].rearrange("(c p) n -> p c n", p=128), writes=[WDs])
                if j == 5:
                    for hf in range(2):
                        f.op("act", lambda e: e.copy(wd[:, hf * 4:(hf + 1) * 4, :], WDs[:, hf * 4:(hf + 1) * 4, :]),
                             reads=[WDs], writes=[V(wd, hf)])
                wg = WG[nwg % 2]
                nwg += 1
                wgs = WGs[(nwg - 1) % 2]
                f.dma("sp", wgs[:, :, 0:128], self.w_gu[l, e_, :, j * 128:(j + 1) * 128].rearrange("(c p) n -> p c n", p=128),
                      writes=[V(wgs, 0)])
                f.dma("sp", wgs[:, :, 128:256],
                      self.w_gu[l, e_, :, DFF + j * 128: DFF + (j + 1) * 128].rearrange("(c p) n -> p c n", p=128),
                      writes=[V(wgs, 1)])
                f.op("pool", lambda e: e.tensor_copy(wg[:], wgs[:]), reads=[wgs], writes=[wg])
                for (s0, n) in slabs:
                    bg_, bl_ = 2 + (nps % 2) * 2, 3 + (nps % 2) * 2
                    nps += 1
                    for kc in range(8):
                        f.mm(PS[:, bg_ * 512: bg_ * 512 + n], wg[:, kc, 0:128], XT[:, kc, s0:s0 + n], kc == 0, kc == 7,
                             [wg, XT], self.bv(bg_))
                    for kc in range(8):
                        f.mm(PS[:, bl_ * 512: bl_ * 512 + n], wg[:, kc, 128:256], XT[:, kc, s0:s0 + n], kc == 0, kc == 7,
                             [wg, XT], self.bv(bl_))
                    f.op("dve", lambda e: e.tensor_scalar(tg[:, 0:n], PS[:, bg_ * 512: bg_ * 512 + n], BGall[:, j, e_:e_ + 1], LIM,
                                                          op0=ALU.add, op1=ALU.min), reads=self.bv(bg_) + [BGall], writes=[tg])
                    f.op("act", lambda e: e.activation(tsg[:, 0:n], tg[:, 0:n], AF.Sigmoid, scale=SW_ALPHA), reads=[tg], writes=[tsg])
                    f.op("dve", lambda e: e.tensor_scalar(tl[:, 0:n], PS[:, bl_ * 512: bl_ * 512 + n], BGall[:, 8 + j, e_:e_ + 1], LIM,
                                                          op0=ALU.add, op1=ALU.min), reads=self.bv(bl_) + [BGall], writes=[tl])
                    f.op("dve", lambda e: e.tensor_scalar(tl[:, 0:n], tl[:, 0:n], -LIM, 1.0, op0=ALU.max, op1=ALU.add),
                         reads=[tl], writes=[tl])
                    f.op("dve", lambda e: e.tensor_tensor(out=tg[:, 0:n], in0=tg[:, 0:n], in1=tsg[:, 0:n], op=ALU.mult),
                         reads=[tg, tsg], writes=[tg])
                    f.op("dve", lambda e: e.tensor_tensor(out=AT[:, j, s0:s0 + n], in0=tg[:, 0:n], in1=tl[:, 0:n], op=ALU.mult),
                         reads=[tg, tl], writes=[V(AT, j)])
            for r in range(NR):
                y_ = yr[r % 2]
                for sl in range(2):
                    for fc in range(8):
                        f.mm(self.bank(6 + sl), AT[:, fc, r * 128:(r + 1) * 128], wd[:, fc, sl * 512:(sl + 1) * 512],
                             fc == 0, fc == 7, [AT, wd], self.bv(6 + sl))
                    f.op("dve", lambda e: e.tensor_tensor(out=y_[:, sl * 512:(sl + 1) * 512], in0=self.bank(6 + sl),
                                                          in1=bd[:, sl * 512:(sl + 1) * 512], op=ALU.add),
                         reads=self.bv(6 + sl) + [bd], writes=[y_])
                f.dma("sp", self.YD[e_ * CAP + r * 128: e_ * CAP + (r + 1) * 128, :], y_[:], reads=[y_],
                      writes=[V(self.YD, (e_, r))])

    def phase_comb(self, l, last):
        f, NT, CAP = self.f, self.NT, self.CAP
        g2 = self.bcast_load("c_g2", self.ln2_g[l, :], D)
        b2 = self.bcast_load("c_b2", self.ln2_b[l, :], D)
        h2t = [f.sb("c_h2%d" % i, [128, D]) for i in range(2)]
        g4 = [f.sb("c_g4%d" % i, [128, 4]) for i in range(2)]
        s4 = [f.sb("c_s4%d" % i, [128, 4], I32) for i in range(2)]
        yk = [f.sb("c_yk%d" % i, [128, D]) for i in range(4)]
        acc = f.sb("c_acc", [128, D]); scr = f.sb("c_scr", [128, D])
        ot = [f.sb("c_o%d" % i, [128, D]) for i in range(2)]
        stats = f.sb("c_st", [128, 12]); mv = f.sb("c_mv", [128, 4])
        dst = self.out if last else self.H
        for i in range(NT):
            tok = slice(i * 128, (i + 1) * 128)
            h2_, g4_, s4_, o_ = h2t[i % 2], g4[i % 2], s4[i % 2], ot[i % 2]
            f.dma("sp", h2_[:], self.H2[tok, :], reads=[V(self.H2, i)], writes=[h2_])
            f.dma("sp", g4_[:], self.GATE4[tok, :], reads=[V(self.GATE4, i)], writes=[g4_])
            f.dma("sp", s4_[:], self.SLOT4[tok, :], reads=[V(self.SLOT4, i)], writes=[s4_])
            for k in range(4):
                y_ = yk[k]
                f.dma("pool", y_[:, :], self.YD[:, :], reads=[self.YD, s4_], writes=[y_],
                      indirect=dict(out_offset=None, in_offset=bass.IndirectOffsetOnAxis(ap=s4_[:, k:k + 1], axis=0),
                                    bounds_check=self.bc_reg(), oob_is_err=False))
                if k == 0:
                    f.op("dve", lambda e: e.tensor_scalar(acc[:], y_[:], g4_[:, 0:1], None, op0=ALU.mult),
                         reads=[y_, g4_], writes=[acc])
                else:
                    f.op("dve", lambda e: e.scalar_tensor_tensor(out=acc[:], in0=y_[:], scalar=g4_[:, k:k + 1], in1=acc[:],
                                                                 op0=ALU.mult, op1=ALU.add), reads=[y_, g4_, acc], writes=[acc])
            f.op("dve", lambda e: e.scalar_tensor_tensor(out=acc[:], in0=h2_[:], scalar=float(ALPHA), in1=acc[:],
                                                         op0=ALU.mult, op1=ALU.add), reads=[h2_, acc], writes=[acc])
            self.ln_tile(acc, o_, g2, b2, scr, stats, mv)
            f.dma("sp", dst[tok, :], o_[:], reads=[o_], writes=[V(dst, i)])


_WNAMES = ["w_in", "conv_w", "conv_b", "dt_bias", "a_log", "d_skip", "ssd_norm_g", "kv_norm_g", "w_uv",
           "w_mem_k", "w_mem_v", "w_out", "ln1_g", "ln1_b", "router_w", "router_b", "w_gu", "b_gu",
           "w_down", "b_down", "ln2_g", "ln2_b"]


def core_inputs(inp, b, S, L, consts=None):
    feed = {"x": np.ascontiguousarray(np.asarray(inp["x"])[b, :S]), "mem": np.ascontiguousarray(np.asarray(inp["mem"])[b]),
            "ln_in_g": np.asarray(inp["ln_in_g"]).reshape(1, D), "ln_in_b": np.asarray(inp["ln_in_b"]).reshape(1, D)}
    for k in _WNAMES:
        feed[k] = np.ascontiguousarray(np.asarray(inp[k])[:L])
    feed.update(consts if consts is not None else make_consts(S))
    return feed


SEQ = 8192
BATCH = 4
N_CORES = 8
CFG = dict(S=SEQ, depth=DEPTH_FULL, TS=512, CAP=1280, NIT=34, NSEL=256)


def kernel(**inputs):
    inputs = {k: np.asarray(v) for k, v in inputs.items()}
    mk = MK(CFG["S"], CFG["depth"], CFG["TS"], CFG["CAP"], CFG["NIT"], CFG["NSEL"])
    nc = mk.build()
    consts = make_consts(SEQ)
    shared = {k: np.ascontiguousarray(inputs[k]) for k in _WNAMES}
    shared["ln_in_g"] = inputs["ln_in_g"].reshape(1, D)
    shared["ln_in_b"] = inputs["ln_in_b"].reshape(1, D)
    shared.update(consts)
    in_maps = []
    for c in range(N_CORES):
        b = c % BATCH
        m = dict(shared)
        m["x"] = np.ascontiguousarray(inputs["x"][b])
        m["mem"] = np.ascontiguousarray(inputs["mem"][b])
        in_maps.append(m)
    res = run_bass_kernel_spmd(nc, in_maps, core_ids=list(range(N_CORES)))
    out = np.stack([np.asarray(res.results[b]["out"]) for b in range(BATCH)], axis=0)
    return out.astype(np.float32)
```
